# Optimizing a Trainium2 kernel written in Bass

```python
import jax, jax.numpy as jnp
from jax import lax
import numpy as np

D_MODEL = 2048
BATCH = 2
SEQ = 16384
DEPTH = 1

F32 = jnp.float32
GRID_W = 64
CTX_LEN = 256
EPS = 1e-6
HEAD_DIM = 128
ATTN_HEADS = 8
ATTN_KV_HEADS = 2
ATTN_GROUP = ATTN_HEADS // ATTN_KV_HEADS
WINDOW = 128
BAND_BLOCK = 128
ROPE_THETA = 10000.0
HGRN_HEADS = 8
HGRN_DK = 128
HGRN_DV = 128
HGRN_CHUNK = 64
ATTN_WIDTH = ATTN_HEADS * HEAD_DIM
KV_WIDTH = ATTN_KV_HEADS * HEAD_DIM
HGRN_KW = HGRN_HEADS * HGRN_DK
HGRN_WIDTH = HGRN_HEADS * HGRN_DV
D_MIX = ATTN_WIDTH + HGRN_WIDTH
COL_CTX = 2 * KV_WIDTH + 2 * HGRN_KW + HGRN_WIDTH
N_IN_COLS = COL_CTX + ATTN_WIDTH + HGRN_KW + HGRN_WIDTH
PEER_HEADS = 8
PEER_NKEYS = 128
PEER_EXPERTS = PEER_NKEYS * PEER_NKEYS
PEER_DKEY = 256
PEER_TOPK = 16
PEER_TOKEN_BLOCK = 128

kernel_name = "hymba_hgrn2_swa_peer_dit_block"


def rms_norm(x, gain):
    xf = x.astype(F32)
    y = xf * lax.rsqrt(jnp.mean(xf * xf, axis=-1, keepdims=True) + EPS)
    return (y * gain.astype(F32)).astype(x.dtype)


def modulate(h, shift, scale):
    return h * (1 + scale) + shift


def to_heads(t, n_heads):
    return t.reshape(t.shape[0], t.shape[1], n_heads, t.shape[-1] // n_heads)


def split_projection(p, n_parts):
    sizes = (KV_WIDTH, KV_WIDTH, HGRN_KW, HGRN_KW, HGRN_WIDTH, ATTN_WIDTH, HGRN_KW, HGRN_WIDTH)[:n_parts]
    points = [int(s) for s in np.cumsum(sizes)[:-1]]
    return jnp.split(p, points, axis=-1)


def axial_rope_tables(n_tokens):
    rows = n_tokens // GRID_W
    row_pos = jnp.repeat(jnp.arange(rows, dtype=F32), GRID_W)
    col_pos = jnp.tile(jnp.arange(GRID_W, dtype=F32), rows)
    half = HEAD_DIM // 2
    inv_freq = jnp.power(ROPE_THETA, -jnp.arange(0, half, 2, dtype=F32) / half)
    ang_r = row_pos[:, None] * inv_freq
    ang_c = col_pos[:, None] * inv_freq
    return (jnp.cos(ang_r)[:, None], jnp.sin(ang_r)[:, None],
            jnp.cos(ang_c)[:, None], jnp.sin(ang_c)[:, None])


def rotate(x, cos, sin):
    x1, x2 = jnp.split(x, 2, axis=-1)
    return jnp.concatenate([x1 * cos - x2 * sin, x1 * sin + x2 * cos], axis=-1)


def apply_axial_rope(x, rope):
    cos_r, sin_r, cos_c, sin_c = rope
    xr, xc = jnp.split(x.astype(F32), 2, axis=-1)
    return jnp.concatenate([rotate(xr, cos_r, sin_r), rotate(xc, cos_c, sin_c)], axis=-1).astype(x.dtype)


def banded_attention_with_context(q, k, v, k_ctx, v_ctx, sink):
    B, T = q.shape[:2]
    nb = T // BAND_BLOCK
    scale = HEAD_DIM ** -0.5
    qb = q.reshape(B, nb, BAND_BLOCK, ATTN_KV_HEADS, ATTN_GROUP, HEAD_DIM)
    pad = ((0, 0), (BAND_BLOCK, BAND_BLOCK), (0, 0), (0, 0))

    def band(t):
        t = jnp.pad(t, pad).reshape(B, nb + 2, BAND_BLOCK, ATTN_KV_HEADS, HEAD_DIM)
        return jnp.concatenate([t[:, :-2], t[:, 1:-1], t[:, 2:]], axis=2)

    kb, vb = band(k), band(v)
    qi = jnp.arange(BAND_BLOCK)[:, None]
    ki = jnp.arange(3 * BAND_BLOCK)[None, :]
    rel = ki - BAND_BLOCK - qi
    key_pos = jnp.arange(nb)[:, None, None] * BAND_BLOCK + ki[None] - BAND_BLOCK
    valid = (jnp.abs(rel) <= WINDOW)[None] & (key_pos >= 0) & (key_pos < T)

    s_band = jnp.einsum('bnqhgd,bnkhd->bhgnqk', qb, kb, preferred_element_type=F32) * scale
    s_band = jnp.where(valid, s_band, -jnp.inf)
    s_ctx = jnp.einsum('bnqhgd,blhd->bhgnql', qb, k_ctx, preferred_element_type=F32) * scale
    sink_l = sink.astype(F32).reshape(ATTN_KV_HEADS, ATTN_GROUP)[None, :, :, None, None]
    m = jnp.maximum(jnp.maximum(s_band.max(-1), s_ctx.max(-1)), sink_l)
    p_band = jnp.exp(s_band - m[..., None])
    p_ctx = jnp.exp(s_ctx - m[..., None])
    denom = p_band.sum(-1) + p_ctx.sum(-1) + jnp.exp(sink_l - m)
    o = (jnp.einsum('bhgnqk,bnkhd->bnqhgd', p_band.astype(v.dtype), vb, preferred_element_type=F32)
         + jnp.einsum('bhgnql,blhd->bnqhgd', p_ctx.astype(v.dtype), v_ctx, preferred_element_type=F32))
    o = o / jnp.transpose(denom, (0, 3, 4, 1, 2))[..., None]
    return o.reshape(B, T, ATTN_WIDTH).astype(q.dtype)


def context_attention(q, k, v, sink):
    B, L = q.shape[:2]
    qg = q.reshape(B, L, ATTN_KV_HEADS, ATTN_GROUP, HEAD_DIM)
    s = jnp.einsum('blhgd,bmhd->bhglm', qg, k, preferred_element_type=F32) * (HEAD_DIM ** -0.5)
    sink_b = jnp.broadcast_to(sink.astype(F32).reshape(ATTN_KV_HEADS, ATTN_GROUP)[None, :, :, None, None],
                              s.shape[:-1] + (1,))
    p = jax.nn.softmax(jnp.concatenate([s, sink_b], axis=-1), axis=-1)[..., :-1]
    o = jnp.einsum('bhglm,bmhd->blhgd', p.astype(v.dtype), v, preferred_element_type=F32)
    return o.reshape(B, L, ATTN_WIDTH).astype(q.dtype)


def hgrn_gates(f_logit, lower_bound):
    f = lower_bound + (1 - lower_bound) * jax.nn.sigmoid(f_logit.astype(F32))
    return jnp.log(f), 1 - f


def scan_layout(t, n_heads):
    return to_heads(t, n_heads).transpose(0, 2, 1, 3)


def hgrn_chunk_scan(k, v, log_f, s0, q):
    B, H, T, _ = k.shape
    nc = T // HGRN_CHUNK
    with_out = q is not None

    def to_chunks(t):
        return jnp.moveaxis(t.astype(F32).reshape(B, H, nc, HGRN_CHUNK, t.shape[-1]), 2, 0)

    xs = (to_chunks(k), to_chunks(v), to_chunks(log_f), to_chunks(q)) if with_out else \
         (to_chunks(k), to_chunks(v), to_chunks(log_f))
    incl = jnp.tril(jnp.ones((HGRN_CHUNK, HGRN_CHUNK), dtype=bool))[:, :, None]

    def step(state, xc):
        kc, vc, lfc = xc[0], xc[1], xc[2]
        a = jnp.cumsum(lfc, axis=2)
        a_end = a[:, :, -1]
        new_state = (jnp.exp(a_end)[..., None] * state
                     + jnp.einsum('bhsk,bhsv->bhkv', kc * jnp.exp(a_end[:, :, None] - a), vc))
        if not with_out:
            return new_state, None
        qc = xc[3]
        inter = jnp.einsum('bhtk,bhkv->bhtv', qc * jnp.exp(a), state)
        decay = jnp.exp(jnp.where(incl, a[:, :, :, None, :] - a[:, :, None, :, :], -jnp.inf))
        scores = jnp.einsum('bhtk,bhtsk,bhsk->bhts', qc, decay, kc)
        return new_state, inter + jnp.einsum('bhts,bhsv->bhtv', scores, vc)

    state, ys = lax.scan(step, s0.astype(F32), xs)
    if not with_out:
        return state, None
    return state, jnp.moveaxis(ys, 0, 2).reshape(B, H, T, ys.shape[-1])


def hgrn_direction(k, v, log_f, s0, q, reverse):
    if reverse:
        k, v, log_f = jnp.flip(k, 2), jnp.flip(v, 2), jnp.flip(log_f, 2)
        q = None if q is None else jnp.flip(q, 2)
    state, o = hgrn_chunk_scan(k, v, log_f, s0, q)
    if o is not None and reverse:
        o = jnp.flip(o, 2)
    return state, o


def hgrn_output(o, g, gain):
    o = rms_norm(o.transpose(0, 2, 1, 3), gain)
    y = o * jax.nn.silu(to_heads(g, HGRN_HEADS).astype(F32))
    return y.reshape(y.shape[0], y.shape[1], HGRN_WIDTH).astype(g.dtype)


def mixing_sublayer(hx, hc, w_in, q_gain, k_gain, sink, lb_fwd, lb_bwd, o_gain, w_out, rope, with_ctx_out):
    B = hx.shape[0]
    k_x, v_x, ff_x, fb_x, i_x, qa_x, qh_x, g_x = split_projection(hx @ w_in, 8)
    ctx_cols = N_IN_COLS if with_ctx_out else COL_CTX
    parts_c = split_projection(hc @ w_in[:, :ctx_cols], 8 if with_ctx_out else 5)
    k_c, v_c, ff_c, fb_c, i_c = parts_c[0], parts_c[1], parts_c[2], parts_c[3], parts_c[4]

    q_xh = apply_axial_rope(rms_norm(to_heads(qa_x, ATTN_HEADS), q_gain), rope)
    k_xh = apply_axial_rope(rms_norm(to_heads(k_x, ATTN_KV_HEADS), k_gain), rope)
    k_ch = rms_norm(to_heads(k_c, ATTN_KV_HEADS), k_gain)
    v_xh, v_ch = to_heads(v_x, ATTN_KV_HEADS), to_heads(v_c, ATTN_KV_HEADS)
    attn_x = banded_attention_with_context(q_xh, k_xh, v_xh, k_ch, v_ch, sink)

    lf_fx, kf_x = hgrn_gates(ff_x, lb_fwd)
    lf_bx, kb_x = hgrn_gates(fb_x, lb_bwd)
    lf_fc, kf_c = hgrn_gates(ff_c, lb_fwd)
    lf_bc, kb_c = hgrn_gates(fb_c, lb_bwd)
    s0 = jnp.zeros((B, HGRN_HEADS, HGRN_DK, HGRN_DV), F32)
    q_c = scan_layout(parts_c[6], HGRN_HEADS) if with_ctx_out else None
    i_cs = scan_layout(i_c, HGRN_HEADS)
    sf_c, of_c = hgrn_direction(scan_layout(kf_c, HGRN_HEADS), i_cs, scan_layout(lf_fc, HGRN_HEADS), s0, q_c, False)
    sb_c, ob_c = hgrn_direction(scan_layout(kb_c, HGRN_HEADS), i_cs, scan_layout(lf_bc, HGRN_HEADS), s0, q_c, True)
    q_x = scan_layout(qh_x, HGRN_HEADS)
    i_xs = scan_layout(i_x, HGRN_HEADS)
    _, of_x = hgrn_direction(scan_layout(kf_x, HGRN_HEADS), i_xs, scan_layout(lf_fx, HGRN_HEADS), sf_c, q_x, False)
    _, ob_x = hgrn_direction(scan_layout(kb_x, HGRN_HEADS), i_xs, scan_layout(lf_bx, HGRN_HEADS), sb_c, q_x, True)
    hgrn_x = hgrn_output(of_x + ob_x, g_x, o_gain)

    mix_x = jnp.concatenate([attn_x, hgrn_x], axis=-1) @ w_out
    if not with_ctx_out:
        return mix_x, None
    q_ch = rms_norm(to_heads(parts_c[5], ATTN_HEADS), q_gain)
    attn_c = context_attention(q_ch, k_ch, v_ch, sink)
    hgrn_c = hgrn_output(of_c + ob_c, parts_c[7], o_gain)
    mix_c = jnp.concatenate([attn_c, hgrn_c], axis=-1) @ w_out
    return mix_x, mix_c


def peer_ffn(h, w_q, sub_keys, u, v):
    B, T, D = h.shape
    n = B * T
    hf = h.reshape(n, D)
    q = (hf @ w_q).reshape(n, PEER_HEADS, 2, PEER_DKEY // 2)
    s = jnp.einsum('nhpd,hpkd->nhpk', q, sub_keys, preferred_element_type=F32)
    s_top, i_top = lax.top_k(s, PEER_TOPK)
    cand = s_top[:, :, 0, :, None] + s_top[:, :, 1, None, :]
    cand_idx = i_top[:, :, 0, :, None] * PEER_NKEYS + i_top[:, :, 1, None, :]
    best, pos = lax.top_k(cand.reshape(n, PEER_HEADS, PEER_TOPK * PEER_TOPK), PEER_TOPK)
    idx = jnp.take_along_axis(cand_idx.reshape(n, PEER_HEADS, PEER_TOPK * PEER_TOPK), pos, axis=-1)
    gate = jax.nn.softmax(best, axis=-1)
    nblk = n // PEER_TOKEN_BLOCK

    def block(args):
        hb, ib, gb = args
        a = jnp.einsum('thkd,td->thk', u[ib], hb, preferred_element_type=F32)
        w = (gb * jax.nn.gelu(a, approximate=False)).astype(hb.dtype)
        return jnp.einsum('thk,thkd->td', w, v[ib])

    out = lax.map(block, (hf.reshape(nblk, PEER_TOKEN_BLOCK, D),
                          idx.reshape(nblk, PEER_TOKEN_BLOCK, PEER_HEADS, PEER_TOPK),
                          gate.reshape(nblk, PEER_TOKEN_BLOCK, PEER_HEADS, PEER_TOPK)))
    return out.reshape(B, T, D)


def setup_inputs(seed: int = 0) -> dict:
    key = jax.random.key(seed)
    ks = jax.random.split(key, 20)

    def nrm(k, shape, scale):
        return jax.random.normal(k, shape, F32) * scale

    return {
        "x": nrm(ks[0], (BATCH, SEQ, D_MODEL), 1.0),
        "c": nrm(ks[1], (BATCH, D_MODEL), 1.0),
        "ctx": nrm(ks[2], (BATCH, CTX_LEN, D_MODEL), 1.0),
        "c_ctx": nrm(ks[3], (D_MODEL,), 1.0),
        "w_ada": nrm(ks[4], (DEPTH, D_MODEL, 6 * D_MODEL), 0.02),
        "b_ada": nrm(ks[5], (DEPTH, 6 * D_MODEL), 0.02),
        "norm_mix": 1.0 + nrm(ks[6], (DEPTH, D_MODEL), 0.05),
        "norm_ffn": 1.0 + nrm(ks[7], (DEPTH, D_MODEL), 0.05),
        "w_in": nrm(ks[8], (DEPTH, D_MODEL, N_IN_COLS), D_MODEL ** -0.5),
        "q_norm": 1.0 + nrm(ks[9], (DEPTH, HEAD_DIM), 0.05),
        "k_norm": 1.0 + nrm(ks[10], (DEPTH, HEAD_DIM), 0.05),
        "attn_sink": nrm(ks[11], (DEPTH, ATTN_HEADS), 0.5),
        "hgrn_lb_logits": nrm(ks[12], (2, DEPTH + 1, HGRN_KW), 0.5),
        "hgrn_norm": 1.0 + nrm(ks[13], (DEPTH, HGRN_DV), 0.05),
        "w_out": nrm(ks[14], (DEPTH, D_MIX, D_MODEL), D_MIX ** -0.5),
        "peer_w_q": nrm(ks[15], (DEPTH, D_MODEL, PEER_HEADS * PEER_DKEY), D_MODEL ** -0.5),
        "peer_sub_keys": nrm(ks[16], (DEPTH, PEER_HEADS, 2, PEER_NKEYS, PEER_DKEY // 2), (PEER_DKEY // 2) ** -0.5),
        "peer_u": nrm(ks[17], (DEPTH, PEER_EXPERTS, D_MODEL), D_MODEL ** -0.5),
        "peer_v": nrm(ks[18], (DEPTH, PEER_EXPERTS, D_MODEL), 0.5),
    }


def reference(x, c, ctx, c_ctx, w_ada, b_ada, norm_mix, norm_ffn, w_in, q_norm, k_norm, attn_sink,
              hgrn_lb_logits, hgrn_norm, w_out, peer_w_q, peer_sub_keys, peer_u, peer_v):
    rope = axial_rope_tables(x.shape[1])
    lower_bounds = jnp.cumsum(jax.nn.softmax(hgrn_lb_logits.astype(F32), axis=1), axis=1)
    silu_c = jax.nn.silu(c)
    silu_cc = jax.nn.silu(c_ctx)
    for layer in range(DEPTH):
        last = layer == DEPTH - 1
        mod_x = (silu_c @ w_ada[layer] + b_ada[layer])[:, None, :]
        sh1, sc1, g1, sh2, sc2, g2 = jnp.split(mod_x, 6, axis=-1)
        n_ctx_mod = 2 if last else 6
        mod_c = silu_cc @ w_ada[layer][:, :n_ctx_mod * D_MODEL] + b_ada[layer][:n_ctx_mod * D_MODEL]
        mc = jnp.split(mod_c, n_ctx_mod, axis=-1)

        hx = modulate(rms_norm(x, norm_mix[layer]), sh1, sc1)
        hc = modulate(rms_norm(ctx, norm_mix[layer]), mc[0], mc[1])
        mix_x, mix_c = mixing_sublayer(hx, hc, w_in[layer], q_norm[layer], k_norm[layer], attn_sink[layer],
                                       lower_bounds[0, layer], lower_bounds[1, layer], hgrn_norm[layer],
                                       w_out[layer], rope, not last)
        x = x + g1 * mix_x
        hx2 = modulate(rms_norm(x, norm_ffn[layer]), sh2, sc2)
        x = x + g2 * peer_ffn(hx2, peer_w_q[layer], peer_sub_keys[layer], peer_u[layer], peer_v[layer])
        if not last:
            ctx = ctx + mc[2] * mix_c
            hc2 = modulate(rms_norm(ctx, norm_ffn[layer]), mc[3], mc[4])
            ctx = ctx + mc[5] * peer_ffn(hc2, peer_w_q[layer], peer_sub_keys[layer], peer_u[layer], peer_v[layer])
    return x
```

```python
import numpy as np
from contextlib import ExitStack
import concourse.bass as bass
import concourse.mybir as mybir
from concourse.bass_utils import run_bass_kernel_spmd

F32 = mybir.dt.float32
BF16 = mybir.dt.bfloat16
ALU = mybir.AluOpType
AF = mybir.ActivationFunctionType
AX = mybir.AxisListType

D = 2048
KC = 16
TOK = 4096
SEQ = 16384
G = 512
NT = G // 128
NG = TOK // G
EXT = NT + 2
SGT = 4
NCOL = 6656
C_K, C_V, C_FF, C_FB, C_I, C_QA, C_QH, C_GG = 0, 256, 512, 1536, 2560, 3584, 4608, 5632
EPS = 1e-6
NE = 16384
ENG = ['pe', 'act', 'dve', 'pool', 'sp']
CFG = dict(ng=NG, nec=NE // 128, nslot=3, nsub=TOK // (SGT * 128), mods=6)
PI = float(np.pi)


class Prog:
    def __init__(self, nc, es):
        self.nc = nc
        self.es = es
        self.ops = {e: [] for e in ENG}
        self.esem = {e: es.enter_context(nc.semaphore('sem_' + e)) for e in ENG if e != 'sp'}
        self.cnt = {e: 0 for e in ENG}
        self.known = {e: {} for e in ENG}
        self.bufs = {}
        self.dsem = {}
        self.final = []
        self.floor = {}
        self.nops = 0

    def _deps(self, reads, writes):
        deps = dict(self.floor)

        def add(s, v):
            if deps.get(s, 0) < v:
                deps[s] = v
        for k in reads:
            b = self.bufs.get(k)
            if b and b['w']:
                add(*b['w'])
        for k in writes:
            b = self.bufs.get(k)
            if b:
                if b['w']:
                    add(*b['w'])
                for s, v in b['r'].items():
                    add(s, v)
        return deps

    def _commit(self, reads, writes, ev):
        s, v = ev
        for k in reads:
            b = self.bufs.setdefault(k, {'w': None, 'r': {}})
            if b['r'].get(s, 0) < v:
                b['r'][s] = v
        for k in writes:
            self.bufs[k] = {'w': ev, 'r': {}}

    def _waits(self, eng, deps):
        waits = []
        kn = self.known[eng]
        for s, v in deps.items():
            if eng == 'pe' and s is self.esem['pe']:
                continue
            if kn.get(s, 0) < v:
                kn[s] = v
                waits.append((s, v))
        return waits

    def op(self, eng, fn, reads=(), writes=()):
        deps = self._deps(reads, writes)
        waits = self._waits(eng, deps)
        self.cnt[eng] += 1
        ev = (self.esem[eng], self.cnt[eng])
        self.ops[eng].append((waits, fn, ev[0], 1))
        self._commit(reads, writes, ev)
        self.nops += 1

    def dma(self, out, in_, reads=(), writes=(), key=None, eng='sp', final=False, **kw):
        if key is None:
            key = (list(writes) + list(reads))[0]
        if key not in self.dsem:
            self.dsem[key] = [self.es.enter_context(self.nc.semaphore('dsem%d' % len(self.dsem))), 0]
        ds = self.dsem[key]
        deps = self._deps(reads, writes)
        waits = self._waits(eng, deps)
        ds[1] += 16
        ev = (ds[0], ds[1])
        self.ops[eng].append((waits, lambda e: e.dma_start(out=out, in_=in_, **kw), ds[0], 16))
        self._commit(reads, writes, ev)
        self.nops += 1
        if final:
            self.final.append(ev)

    def barrier(self):
        for e in ENG:
            if e != 'sp' and self.cnt[e] > 0:
                self.floor[self.esem[e]] = self.cnt[e]
        for k, (s, v) in self.dsem.items():
            if v > 0:
                self.floor[s] = v

    def emit(self):
        nc = self.nc
        engmap = {'pe': 'tensor', 'act': 'scalar', 'dve': 'vector', 'pool': 'gpsimd', 'sp': 'sync'}
        with nc.Block() as block:
            for e in ENG:
                ops = self.ops[e]
                fin = self.final if e == 'sp' else []

                def body(eng, ops=ops, fin=fin):
                    for waits, fn, sem, inc in ops:
                        for s, v in waits:
                            eng.wait_ge(s, v)
                        fn(eng).then_inc(sem, inc)
                    for s, v in fin:
                        eng.wait_ge(s, v)
                getattr(block, engmap[e])(body)


class Arena:
    def __init__(self, ap, size, name):
        self.ap, self.size, self.off, self.name, self.n = ap, size, 0, name, 0

    def _shape(self, v, shape):
        if shape[0] < 128:
            v = v[0:shape[0]]
        if len(shape) == 2:
            return v
        if len(shape) == 3:
            return v.rearrange("p (a b) -> p a b", a=shape[1])
        return v.rearrange("p (a b c) -> p a b c", a=shape[1], b=shape[2])

    def _take(self, nf):
        assert self.off + nf <= self.size, (self.name, self.off, nf, self.size)
        v = self.ap[:, self.off:self.off + nf]
        self.off += nf
        self.n += 1
        return v, '%s_%d_%d' % (self.name, self.off, self.n)

    def f32(self, shape):
        n = int(np.prod(shape[1:]))
        v, k = self._take(n)
        return self._shape(v, shape), k

    def bf16(self, shape):
        n = int(np.prod(shape[1:]))
        v, k = self._take((n + 1) // 2)
        v = v.bitcast(BF16)[:, 0:n]
        return self._shape(v, shape), k


def build_nc():
    nc = bass.Bass("TRN2", target_bir_lowering=False)
    di = lambda n, s: nc.dram_tensor(n, s, F32, kind="ExternalInput").ap()
    xr = di("xr", [SEQ, D])
    ctxb = di("ctxb", [256, D])
    cvec = di("cvec", [2, D])
    w_ada = di("w_ada", [D, 6 * D])
    b_ada = di("b_ada", [6 * D])
    norm_mix = di("norm_mix", [D])
    norm_ffn = di("norm_ffn", [D])
    w_in = di("w_in", [D, NCOL])
    q_norm = di("q_norm", [128])
    k_norm = di("k_norm", [128])
    attn_sink = di("attn_sink", [8])
    lbl = di("lbl", [2, 2, 1024])
    hgrn_norm = di("hgrn_norm", [128])
    w_out = di("w_out", [D, D])
    w_q = di("w_q", [D, D])
    skd = di("sk", [16, 128, 128])
    pu = di("pu", [NE, D])
    pv = di("pv", [NE, D])
    flags = di("flags", [128, 32])
    outd = nc.dram_tensor("out", [TOK, D], F32, kind="ExternalOutput").ap()
    modd = nc.dram_tensor("modd", [8, D], F32, kind="Internal").ap()
    lbd = nc.dram_tensor("lbd", [2, 2, 1024], F32, kind="Internal").ap()
    x1s = nc.dram_tensor("x1s", [TOK, D], F32, kind=("ExternalOutput" if CFG.get("dbg") else "Internal")).ap()
    ssave = nc.dram_tensor("ssave", [NG, 128, 1024], F32, kind="Internal").ap()
    w_in_b = nc.dram_tensor("w_in_b", [D, NCOL], BF16, kind="Internal").ap()
    w_out_b = nc.dram_tensor("w_out_b", [D, D], BF16, kind="Internal").ap()
    w_q_b = nc.dram_tensor("w_q_b", [D, D], BF16, kind="Internal").ap()
    uTd = nc.dram_tensor("uTd", [NE // 128, 128, D], BF16, kind="Internal").ap()
    vd = nc.dram_tensor("vd", [NE // 128, 128, D], BF16, kind="Internal").ap()

    with ExitStack() as es:
        PERS = 29960
        PH = 23240
        pers_t = es.enter_context(nc.sbuf_tensor("pers", [128, PERS], F32))
        ph_t = es.enter_context(nc.sbuf_tensor("phase", [128, PH], F32))
        PS = [es.enter_context(nc.psum_tensor("ps%d" % i, [128, 512], F32)) for i in range(8)]
        p = Prog(nc, es)
        PA = Arena(pers_t[:], PERS, 'P')
        ph_n = [0]

        def new_phase():
            p.barrier()
            ph_n[0] += 1
            return Arena(ph_t[:], PH, 'H%d' % ph_n[0])

        psn = [0]

        def bank():
            i = psn[0] % 8
            psn[0] += 1
            return PS[i], 'psb%d' % i

        def bank_bf():
            t, k = bank()
            return t[:].bitcast(BF16), k

        val, kval = PA.f32([128, 128])
        vs, kvs = PA.f32([128, 128])
        vt, kvt = PA.f32([128, 128])
        p.op('pool', lambda e: e.iota(val, pattern=[[1, 128]], base=0, channel_multiplier=-1,
                                      allow_small_or_imprecise_dtypes=True), writes=[kval])
        p.op('pool', lambda e: e.iota(vs, pattern=[[0, 128]], base=0, channel_multiplier=1,
                                      allow_small_or_imprecise_dtypes=True), writes=[kvs])
        p.op('pool', lambda e: e.iota(vt, pattern=[[1, 128]], base=0, channel_multiplier=0,
                                      allow_small_or_imprecise_dtypes=True), writes=[kvt])

        def ts(out, in0, s1, s2, op0, op1=None, eng='dve', r=(), w=()):
            if op1 is None:
                p.op(eng, lambda e: e.tensor_scalar(out=out, in0=in0, scalar1=s1, scalar2=None, op0=op0), reads=r, writes=w)
            else:
                p.op(eng, lambda e: e.tensor_scalar(out=out, in0=in0, scalar1=s1, scalar2=s2, op0=op0, op1=op1), reads=r, writes=w)

        def tt(out, in0, in1, op, eng='dve', r=(), w=()):
            p.op(eng, lambda e: e.tensor_tensor(out=out, in0=in0, in1=in1, op=op), reads=r, writes=w)

        def stt(out, in0, sc, in1, op0, op1, eng='dve', r=(), w=()):
            p.op(eng, lambda e: e.scalar_tensor_tensor(out=out, in0=in0, scalar=sc, in1=in1, op0=op0, op1=op1), reads=r, writes=w)

        def act(out, in_, func, bias=None, scale=1.0, accum=None, r=(), w=()):
            kw = {}
            if bias is not None:
                kw['bias'] = bias
            if accum is not None:
                kw['accum_out'] = accum
            p.op('act', lambda e: e.activation(out=out, in_=in_, func=func, scale=scale, **kw), reads=r, writes=w)

        def cp(out, in_, eng='dve', r=(), w=()):
            if eng == 'act':
                p.op('act', lambda e: e.copy(out=out, in_=in_), reads=r, writes=w)
            else:
                p.op(eng, lambda e: e.tensor_copy(out=out, in_=in_), reads=r, writes=w)

        def mm(out, lhsT, rhs, start=True, stop=True, r=(), w=()):
            p.op('pe', lambda e: e.matmul(out, lhsT=lhsT, rhs=rhs, start=start, stop=stop), reads=r, writes=w)

        def tr(out, in_, idn, r=(), w=()):
            p.op('pe', lambda e: e.transpose(out, in_, idn), reads=r, writes=w)

        ident, kid = PA.f32([128, 128])
        ts(ident, val, 0.0, None, ALU.is_equal, r=[kval], w=[kid])
        identb, kidb = PA.bf16([128, 128])
        cp(identb, ident, r=[kid], w=[kidb])
        c_tmp, kct = PA.f32([128, 128])
        c_tmp2, kct2 = PA.f32([128, 128])
        BM, kbm = PA.f32([128, 128])
        ts(c_tmp, vs, 64.0, None, ALU.is_ge, r=[kvs], w=[kct])
        ts(c_tmp2, vt, 64.0, None, ALU.is_ge, r=[kvt], w=[kct2])
        tt(BM, c_tmp, c_tmp2, ALU.is_equal, r=[kct, kct2], w=[kbm])
        ind, kind = PA.f32([128, 2])
        cp(ind[:, 1:2], c_tmp[:, 0:1], r=[kct], w=[kind])
        ts(ind[:, 0:1], c_tmp[:, 0:1], -1.0, 1.0, ALU.mult, ALU.add, r=[kct, kind], w=[kind])
        TI, TX, TIb_, = {}, {}, {}
        kTI, kTX = {}, {}
        for d_, (opi, opx) in enumerate([(ALU.is_ge, ALU.is_lt), (ALU.is_le, ALU.is_gt)]):
            TI[d_], kTI[d_] = PA.f32([128, 128])
            TX[d_], kTX[d_] = PA.f32([128, 128])
            ts(TI[d_], val, 0.0, None, opi, r=[kval], w=[kTI[d_]])
            tt(TI[d_], TI[d_], BM, ALU.mult, r=[kTI[d_], kbm], w=[kTI[d_]])
            ts(TX[d_], val, 0.0, None, opx, r=[kval], w=[kTX[d_]])
            tt(TX[d_], TX[d_], BM, ALU.mult, r=[kTX[d_], kbm], w=[kTX[d_]])
        TXF, kTXF = {}, {}
        for d_, opx in enumerate([ALU.is_lt, ALU.is_gt]):
            TXF[d_], kTXF[d_] = PA.f32([128, 128])
            ts(TXF[d_], val, 0.0, None, opx, r=[kval], w=[kTXF[d_]])
        fl, kfl = PA.f32([128, 32])
        p.dma(fl, flags, writes=[kfl])
        maskP, kmP = PA.bf16([128, 128])
        maskN, kmN = PA.bf16([128, 128])
        maskP0, kmP0 = PA.bf16([128, 128])
        maskN0, kmN0 = PA.bf16([128, 128])
        ts(maskP, val, 0.0, None, ALU.is_le, r=[kval], w=[kmP])
        ts(maskN, val, 0.0, None, ALU.is_ge, r=[kval], w=[kmN])
        ts(maskP0, val, 0.0, fl[:, 8:9], ALU.is_le, ALU.mult, r=[kval, kfl], w=[kmP0])
        ts(maskN0, val, 0.0, fl[:, 9:10], ALU.is_ge, ALU.mult, r=[kval, kfl], w=[kmN0])
        ones_f, kof = PA.f32([128, 128])
        ones_b, kob = PA.bf16([128, 128])
        p.op('dve', lambda e: e.memset(ones_f, 1.0), writes=[kof])
        p.op('dve', lambda e: e.memset(ones_b, 1.0), writes=[kob])
        PermT, kpm = PA.f32([128, 128])
        p.op('pool', lambda e: e.iota(c_tmp.rearrange("p (a b) -> p a b", a=2), pattern=[[0, 2], [1, 64]], base=0, channel_multiplier=0,
                                      allow_small_or_imprecise_dtypes=True), reads=[kct], writes=[kct])
        ts(c_tmp, c_tmp, 32.0, None, ALU.is_lt, r=[kct], w=[kct])
        ts(c_tmp2, val, -32.0, None, ALU.is_equal, r=[kval], w=[kct2])
        tt(PermT, c_tmp2, c_tmp, ALU.mult, r=[kct, kct2], w=[kpm])
        ts(c_tmp, c_tmp, -1.0, 1.0, ALU.mult, ALU.add, r=[kct], w=[kct])
        ts(c_tmp2, val, 32.0, None, ALU.is_equal, r=[kval], w=[kct2])
        tt(c_tmp2, c_tmp2, c_tmp, ALU.mult, r=[kct, kct2], w=[kct2])
        tt(PermT, PermT, c_tmp2, ALU.add, r=[kpm, kct2], w=[kpm])
        small, ksm = PA.f32([128, 64])
        sgn = small[:, 0:1]
        invf = small[:, 1:2]
        pcol = vs[:, 0:1]
        ts(small[:, 30:31], pcol, 32.0, None, ALU.is_lt, r=[kvs, ksm], w=[ksm])
        ts(small[:, 31:32], pcol, 64.0, None, ALU.is_ge, r=[kvs, ksm], w=[ksm])
        ts(small[:, 32:33], pcol, 96.0, None, ALU.is_lt, r=[kvs, ksm], w=[ksm])
        tt(small[:, 2:3], small[:, 31:32], small[:, 32:33], ALU.mult, r=[ksm], w=[ksm])
        tt(small[:, 2:3], small[:, 2:3], small[:, 30:31], ALU.add, r=[ksm], w=[ksm])
        ts(sgn, small[:, 2:3], -2.0, 1.0, ALU.mult, ALU.add, r=[ksm], w=[ksm])
        ts(small[:, 33:34], pcol, 32.0, None, ALU.is_ge, r=[kvs, ksm], w=[ksm])
        ts(small[:, 34:35], pcol, 96.0, None, ALU.is_ge, r=[kvs, ksm], w=[ksm])
        tt(small[:, 33:34], small[:, 33:34], small[:, 31:32], ALU.add, r=[ksm], w=[ksm])
        tt(small[:, 33:34], small[:, 33:34], small[:, 34:35], ALU.add, r=[ksm], w=[ksm])
        stt(small[:, 3:4], small[:, 33:34], -32.0, pcol, ALU.mult, ALU.add, r=[ksm, kvs], w=[ksm])
        act(invf, small[:, 3:4], AF.Exp, scale=-float(np.log(10000.0)) / 32.0, r=[ksm], w=[ksm])
        qg = small[:, 4:5]
        kg = small[:, 5:6]
        p.dma(qg, q_norm.rearrange("(p o) -> p o", o=1), reads=[ksm], writes=[ksm])
        p.dma(kg, k_norm.rearrange("(p o) -> p o", o=1), reads=[ksm], writes=[ksm])
        esink = small[:, 8:16]
        p.dma(esink, attn_sink.partition_broadcast(128), reads=[ksm], writes=[ksm])
        act(esink, esink, AF.Exp, r=[ksm], w=[ksm])
        ogain, kog = PA.f32([128, 128])
        p.dma(ogain, hgrn_norm.partition_broadcast(128), writes=[kog])
        cols, kcols = PA.f32([128, 8, KC])
        nrm, knrm = PA.f32([128, 2, KC])
        p.dma(nrm[:, 0, :], norm_mix.rearrange("(c p) -> p c", p=128), writes=[knrm], allow_slow_non_contiguous=True)
        p.dma(nrm[:, 1, :], norm_ffn.rearrange("(c p) -> p c", p=128), reads=[knrm], writes=[knrm], allow_slow_non_contiguous=True)
        S_all, kS = PA.f32([128, 16, 128])
        kSh = [[kS + '_%d_%d' % (d_, h) for h in range(8)] for d_ in range(2)]
        Sb_all, _ = PA.bf16([128, 16, 128])
        kSb = [[kS + 'b_%d_%d' % (d_, h) for h in range(8)] for d_ in range(2)]
        S_ctx, kSc = PA.f32([128, 16, 128])
        kcT_c, kkc = PA.bf16([128, 2, 256])
        v_c, kvc = PA.bf16([128, 2, 2, 128])
        hT, khT = PA.bf16([128, KC, 768])
        wbf, kwbf = [], []
        wbfF = []
        for i in range(2):
            a, k = PA.bf16([128, KC * 640])
            wbfF.append(a)
            wbf.append(a.rearrange("p (a b) -> p a b", a=KC))
            kwbf.append(k)
        wstF, _ = PA.f32([128, 2048])
        wst, kwst = [], []
        for i in range(4):
            wst.append(wstF[:, i * 512:(i + 1) * 512])
            kwst.append('wst%d' % i)
        XS, kXS = PA.f32([128, D])
        xn, kxn = PA.bf16([128, D])
        sstat, kss = PA.f32([128, 8])
        wl = [0]

        WB = {}

        def load_w(dst, kdst, wd, col0, ncols, dcol=0):
            src = WB[id(wd)].rearrange("(kc p) c -> p kc c", p=128)[:, :, col0:col0 + ncols]
            p.dma(dst[:, :, dcol:dcol + ncols], src, writes=[kdst])

        def norm_T(rows_ap, ci, dstT, kdst, c0):
            p.dma(XS, rows_ap, writes=[kXS])
            p.op('dve', lambda e: e.memset(sstat[:, 0:1], 0.0), reads=[kss], writes=[kss])
            act(xn, XS, AF.Square, accum=sstat[:, 0:1], r=[kXS, kss], w=[kxn, kss])
            act(sstat[:, 1:2], sstat[:, 0:1], AF.Ln, bias=small[:, 20:21], scale=1.0 / D, r=[kss, ksm], w=[kss])
            act(sstat[:, 2:3], sstat[:, 1:2], AF.Exp, scale=-0.5, r=[kss], w=[kss])
            act(xn, XS, AF.Identity, scale=sstat[:, 2:3], r=[kXS, kss], w=[kxn])
            for half in range(2):
                pb, kpb = bank_bf()
                for q in range(8):
                    kc = half * 8 + q
                    tr(pb[:, q * 128:(q + 1) * 128], xn[:, kc * 128:(kc + 1) * 128], identb, r=[kxn, kidb], w=[kpb])
                for q in range(8):
                    kc = half * 8 + q
                    if q % 2 == 0:
                        act(dstT[:, kc, c0:c0 + 128], pb[:, q * 128:(q + 1) * 128], AF.Identity,
                            bias=cols[:, ci + 1, kc:kc + 1], scale=cols[:, ci, kc:kc + 1], r=[kpb, kcols], w=[kdst])
                    else:
                        ts(dstT[:, kc, c0:c0 + 128], pb[:, q * 128:(q + 1) * 128], cols[:, ci, kc:kc + 1],
                           cols[:, ci + 1, kc:kc + 1], ALU.mult, ALU.add, r=[kpb, kcols], w=[kdst])

        p.op('dve', lambda e: e.memset(small[:, 20:21], EPS), reads=[ksm], writes=[ksm])
        p.op('dve', lambda e: e.memset(small[:, 21:22], -PI), reads=[ksm], writes=[ksm])
        p.op('dve', lambda e: e.memset(small[:, 22:23], 1.0), reads=[ksm], writes=[ksm])

        H = new_phase()
        cv, kcv = H.f32([128, KC, 2])
        p.dma(cv[:, :, 0], cvec[0].rearrange("(c p) -> p c", p=128), writes=[kcv], allow_slow_non_contiguous=True)
        p.dma(cv[:, :, 1], cvec[1].rearrange("(c p) -> p c", p=128), reads=[kcv], writes=[kcv], allow_slow_non_contiguous=True)
        act(cv, cv, AF.Silu, r=[kcv], w=[kcv])
        rep, krep = H.f32([128, 2, KC, 128])
        for v_ in range(2):
            cp(rep[:, v_], cv[:, :, v_].unsqueeze(2).to_broadcast([128, KC, 128]), r=[kcv], w=[krep])
        brow, kbrow = H.f32([128, 512])
        mrow, kmrow = H.f32([128, 512])
        for mi in range(CFG['mods']):
            for cg in range(4):
                col0 = mi * D + cg * 512
                pb0, kpb0 = bank()
                pb1, kpb1 = bank()
                for kc in range(KC):
                    i = wl[0] % 4
                    wl[0] += 1
                    p.dma(wst[i][:, 0:512], w_ada[kc * 128:(kc + 1) * 128, col0:col0 + 512], writes=[kwst[i]])
                    mm(pb0[:], rep[:, 0, kc], wst[i][:, 0:512], start=(kc == 0), stop=(kc == KC - 1), r=[krep, kwst[i]], w=[kpb0])
                    if mi < 2:
                        mm(pb1[:], rep[:, 1, kc], wst[i][:, 0:512], start=(kc == 0), stop=(kc == KC - 1), r=[krep, kwst[i]], w=[kpb1])
                p.dma(brow, b_ada[col0:col0 + 512].partition_broadcast(128), writes=[kbrow])
                tt(mrow, pb0[:], brow, ALU.add, r=[kpb0, kbrow], w=[kmrow])
                p.dma(modd[mi:mi + 1, cg * 512:(cg + 1) * 512], mrow[0:1, :], reads=[kmrow], writes=['modd'], key=kmrow)
                if mi < 2:
                    tt(mrow, pb1[:], brow, ALU.add, r=[kpb1, kbrow], w=[kmrow])
                    p.dma(modd[6 + mi:7 + mi, cg * 512:(cg + 1) * 512], mrow[0:1, :], reads=[kmrow], writes=['modd'], key=kmrow)
        mc, kmc = H.f32([128, 8, KC])
        for mi in range(8):
            p.dma(mc[:, mi, :], modd[mi].rearrange("(c p) -> p c", p=128), reads=['modd', kmc], writes=[kmc], allow_slow_non_contiguous=True)
        for (ci, sci, shi, ni) in [(0, 1, 0, 0), (2, 4, 3, 1), (4, 7, 6, 0)]:
            stt(cols[:, ci, :], mc[:, sci, :], 1.0, nrm[:, ni, :], ALU.add, ALU.mult, r=[kmc, knrm, kcols], w=[kcols])
            cp(cols[:, ci + 1, :], mc[:, shi, :], r=[kmc, kcols], w=[kcols])
        lt_, klt = H.f32([1, 2, 2, 1024])
        p.dma(lt_, lbl.rearrange("(o a) b c -> o a b c", o=1), writes=[klt])
        lo_, klo = H.f32([1, 2, 2, 1024])
        tt(lo_[:, 0], lt_[:, :, 0, :], lt_[:, :, 1, :], ALU.subtract, r=[klt], w=[klo])
        act(lo_[:, 0], lo_[:, 0], AF.Sigmoid, r=[klo], w=[klo])
        ts(lo_[:, 1], lo_[:, 0], -1.0, 1.0, ALU.mult, ALU.add, r=[klo], w=[klo])
        p.dma(lbd.rearrange("(o a) b c -> o a b c", o=1), lo_, reads=[klo], writes=['lbd'], key=klo)

        H = new_phase()
        stg = [H.f32([128, D]) for _ in range(4)]
        ubr = [H.bf16([128, D]) for _ in range(2)]
        uTr = [H.bf16([128, KC, 128]) for _ in range(2)]
        vbr = [H.bf16([128, D]) for _ in range(2)]
        WB[id(w_in)], WB[id(w_out)], WB[id(w_q)] = w_in_b, w_out_b, w_q_b
        wn = 0
        for (wsrc, wdst, ncol_) in [(w_in, w_in_b, NCOL), (w_out, w_out_b, D), (w_q, w_q_b, D)]:
            for kc in range(KC):
                for c0 in range(0, ncol_, 2048):
                    n = min(2048, ncol_ - c0)
                    su, ksu = stg[wn % 4]
                    ub_, kub_ = (ubr + vbr)[wn % 4]
                    p.dma(su[:, 0:n], wsrc[kc * 128:(kc + 1) * 128, c0:c0 + n], writes=[ksu])
                    cp(ub_[:, 0:n], su[:, 0:n], eng=['pool', 'dve', 'act'][wn % 3], r=[ksu], w=[kub_])
                    p.dma(wdst[kc * 128:(kc + 1) * 128, c0:c0 + n], ub_[:, 0:n], reads=[kub_], writes=['wb'], key=kub_)
                    wn += 1
        for ec in range(CFG['nec']):
            i = ec % 2
            su, ksu = stg[i]
            sv, ksv = stg[2 + i]
            p.dma(su, pu[ec * 128:(ec + 1) * 128, :], writes=[ksu])
            p.dma(sv, pv[ec * 128:(ec + 1) * 128, :], writes=[ksv])
            ub_, kub_ = ubr[i]
            cp(ub_, su, eng='pool', r=[ksu], w=[kub_])
            vb_, kvb_ = vbr[i]
            cp(vb_, sv, eng='dve', r=[ksv], w=[kvb_])
            p.dma(vd[ec], vb_, reads=[kvb_], writes=['vd'], key=kvb_)
            uT_, kuT_ = uTr[i]
            for half in range(2):
                pt, kpt = bank_bf()
                for q in range(8):
                    kc = half * 8 + q
                    tr(pt[:, q * 128:(q + 1) * 128], ub_[:, kc * 128:(kc + 1) * 128], identb, r=[kub_, kidb], w=[kpt])
                cp(uT_[:, half * 8:(half + 1) * 8, :], pt[:, 0:1024].rearrange("p (a b) -> p a b", a=8), eng='act', r=[kpt], w=[kuT_])
            p.dma(uTd[ec].rearrange("p (a b) -> p a b", a=KC), uT_, reads=[kuT_], writes=['uTd'], key=kuT_)

        def qk_norm_rope(ps_ap, kps, n, gain, dst, kdst, A, cosv=None, sinv=None, ktab=None):
            sq, ksq = A['sq']
            kn_, kkn = A['kn']
            act(sq[:, 0:n], ps_ap, AF.Square, r=[kps], w=[ksq])
            pb, kpb = bank()
            mm(pb[:, 0:n], ones_f, sq[:, 0:n], r=[kof, ksq], w=[kpb])
            act(sq[:, 0:n], pb[:, 0:n], AF.Ln, bias=small[:, 20:21], scale=1.0 / 128, r=[kpb, ksm], w=[ksq])
            act(sq[:, 0:n], sq[:, 0:n], AF.Exp, scale=-0.5, r=[ksq], w=[ksq])
            if cosv is None:
                stt(dst, ps_ap, gain, sq[:, 0:n], ALU.mult, ALU.mult, r=[kps, ksq, ksm], w=[kdst])
                return
            stt(kn_[:, 0:n], ps_ap, gain, sq[:, 0:n], ALU.mult, ALU.mult, r=[kps, ksq, ksm], w=[kkn])
            pb2, kpb2 = bank()
            mm(pb2[:, 0:n], PermT, kn_[:, 0:n], r=[kpm, kkn], w=[kpb2])
            tt(sq[:, 0:n], pb2[:, 0:n], sinv, ALU.mult, r=[kpb2, ktab, ksq], w=[ksq])
            tt(kn_[:, 0:n], kn_[:, 0:n], cosv, ALU.mult, eng='pool', r=[kkn, ktab], w=[kkn])
            tt(dst, kn_[:, 0:n], sq[:, 0:n], ALU.add, r=[kkn, ksq], w=[kdst])

        def proj_fm(dstps, kps, wv, kw, wc0, src, ksrc, t0, n):
            for kc in range(KC):
                mm(dstps, wv[:, kc, wc0:wc0 + 128], src[:, kc, t0:t0 + n], start=(kc == 0), stop=(kc == KC - 1),
                   r=[kw, ksrc], w=[kps])

        def proj_tm(dstps, kps, wv, kw, wc0, ncols, src, ksrc, t0):
            for kc in range(KC):
                mm(dstps, src[:, kc, t0:t0 + 128], wv[:, kc, wc0:wc0 + ncols], start=(kc == 0), stop=(kc == KC - 1),
                   r=[kw, ksrc], w=[kps])

        def load_lba(H):
            lba, klba = H.f32([128, 2, 2, 1024])
            for a_ in range(2):
                for d_ in range(2):
                    p.dma(lba[:, a_, d_, :], lbd[a_, d_, :].partition_broadcast(128), reads=['lbd', klba], writes=[klba])
            return lba, klba

        def gate_math(sg, ksg, lb_b, oml_b, klba):
            act(sg, sg, AF.Ln, bias=small[:, 22:23], scale=1.0, r=[ksg, ksm], w=[ksg])
            act(sg, sg, AF.Exp, scale=-1.0, r=[ksg], w=[ksg])
            tt(sg, sg, oml_b, ALU.mult, r=[ksg, klba], w=[ksg])
            tt(sg, sg, lb_b, ALU.add, r=[ksg, klba], w=[ksg])

        def state_step(d_, h, lf, kk, iv, kin, A):
            S = S_all[:, d_ * 8 + h, :]
            pbm, kpbm = bank()
            mm(pbm[:, 0:128], TX[d_], lf, r=[kTX[d_]] + kin, w=[kpbm])
            mm(pbm[:, 128:130], lf, ind, r=[kind] + kin, w=[kpbm])
            Eb, kEb = A['Eb']
            ee, kee = A['ee']
            act(Eb, pbm[:, 0:128], AF.Exp, r=[kpbm], w=[kEb])
            act(ee, pbm[:, 128:130], AF.Exp, r=[kpbm], w=[kee])
            for c in ([0, 1] if d_ == 0 else [1, 0]):
                khz, kkhz = A['khz'][c]
                stt(khz, kk, ind[:, c:c + 1], Eb, ALU.mult, ALU.mult, r=[kEb, kind] + kin, w=[kkhz])
                pd, kpd = bank()
                mm(pd[:, 0:128], khz, iv, r=[kkhz] + kin, w=[kpd])
                stt(S, S, ee[:, c:c + 1], pd[:, 0:128], ALU.mult, ALU.add, r=[kSh[d_][h], kee, kpd], w=[kSh[d_][h]])

        def full_step(d_, h, lf, kk, iv, qTt, kin, ops_, kops, A):
            S = S_all[:, d_ * 8 + h, :]
            Sb = Sb_all[:, d_ * 8 + h, :]
            pa, kpa = bank()
            mm(pa[:, 0:128], lf, TI[d_], r=[kTI[d_]] + kin, w=[kpa])
            pbm, kpbm = bank()
            mm(pbm[:, 0:128], TX[d_], lf, r=[kTX[d_]] + kin, w=[kpbm])
            aTs, kaT = A['aTs']
            cp(aTs, pa[:, 0:128], eng='act', r=[kpa], w=[kaT])
            refc = [31, 95] if d_ == 0 else [32, 96]
            endc = [63, 127] if d_ == 0 else [0, 64]
            negr, knr = A['negr']
            Eq, kEq = A['Eq']
            Ek, kEk = A['Ek']
            Ea, kEa = A['Ea']
            ee, kee = A['ee']
            for c in range(2):
                ts(negr[:, c:c + 1], aTs[:, refc[c]:refc[c] + 1], -1.0, None, ALU.mult, r=[kaT, knr], w=[knr])
            for c in range(2):
                sl = slice(64 * c, 64 * c + 64)
                act(Eq[:, sl], pa[:, sl], AF.Exp, bias=negr[:, c:c + 1], scale=1.0, r=[kpa, knr, kEq], w=[kEq])
                act(Ek[:, sl], pa[:, sl], AF.Exp, bias=aTs[:, refc[c]:refc[c] + 1], scale=-1.0, r=[kpa, kaT, kEk], w=[kEk])
                act(ee[:, c:c + 1], aTs[:, endc[c]:endc[c] + 1], AF.Exp, r=[kaT, kee], w=[kee])
            act(Ea, pa[:, 0:128], AF.Exp, r=[kpa], w=[kEa])
            Eb, kEb = A['Eb']
            act(Eb, pbm[:, 0:128], AF.Exp, r=[kpbm], w=[kEb])
            pt, kpt = bank_bf()
            tr(pt[:, 0:128], kk, identb, r=[kidb] + kin, w=[kpt])
            ktT, kktT = A['ktT']
            qtT, kqtT = A['qtT']
            qhT, kqhT = A['qhT']
            tt(ktT, pt[:, 0:128], Ek, ALU.mult, r=[kpt, kEk], w=[kktT])
            tt(qtT, qTt, Eq, ALU.mult, eng='pool', r=[kEq] + kin, w=[kqtT])
            tt(qhT, qTt, Ea, ALU.mult, eng='pool', r=[kEa] + kin, w=[kqhT])
            psc, kpsc = bank()
            mm(psc[:, 0:128], ktT, qtT, r=[kktT, kqtT], w=[kpsc])
            scT, kscT = A['scT']
            tt(scT, psc[:, 0:128], TI[d_], ALU.mult, r=[kpsc, kTI[d_]], w=[kscT])
            for c in range(2):
                khz, kkhz = A['khz'][c]
                stt(khz, kk, ind[:, c:c + 1], Eb, ALU.mult, ALU.mult, r=[kEb, kind] + kin, w=[kkhz])
            mm(ops_, scT, iv, start=True, stop=False, r=[kscT] + kin, w=[kops])
            order = [0, 1] if d_ == 0 else [1, 0]
            for n_, c in enumerate(order):
                mm(ops_[64 * c:64 * c + 64, :], qhT[:, 64 * c:64 * c + 64], Sb, start=False, stop=True,
                   r=[kqhT, kSb[d_][h]], w=[kops])
                khz, kkhz = A['khz'][c]
                pd, kpd = bank()
                mm(pd[:, 0:128], khz, iv, r=[kkhz] + kin, w=[kpd])
                stt(S, S, ee[:, c:c + 1], pd[:, 0:128], ALU.mult, ALU.add, r=[kSh[d_][h], kee, kpd], w=[kSh[d_][h]])
                cp(Sb, S, eng='act', r=[kSh[d_][h]], w=[kSb[d_][h]])

        def step_front(d_, h, lf, kk, iv, qTt, kin, A):
            pa, kpa = bank()
            mm(pa[:, 0:128], lf, TI[d_], r=[kTI[d_]] + kin, w=[kpa])
            pbm, kpbm = bank()
            mm(pbm[:, 0:128], TX[d_], lf, r=[kTX[d_]] + kin, w=[kpbm])
            aTs, kaT = A['aTs']
            cp(aTs, pa[:, 0:128], eng='act', r=[kpa], w=[kaT])
            refc = [31, 95] if d_ == 0 else [32, 96]
            endc = [63, 127] if d_ == 0 else [0, 64]
            negr, knr = A['negr']
            Eq, kEq = A['Eq']
            Ek, kEk = A['Ek']
            Ea, kEa = A['Ea']
            ee, kee = A['ee']
            for c in range(2):
                ts(negr[:, c:c + 1], aTs[:, refc[c]:refc[c] + 1], -1.0, None, ALU.mult, r=[kaT, knr], w=[knr])
            for c in range(2):
                sl = slice(64 * c, 64 * c + 64)
                act(Eq[:, sl], pa[:, sl], AF.Exp, bias=negr[:, c:c + 1], scale=1.0, r=[kpa, knr, kEq], w=[kEq])
                act(Ek[:, sl], pa[:, sl], AF.Exp, bias=aTs[:, refc[c]:refc[c] + 1], scale=-1.0, r=[kpa, kaT, kEk], w=[kEk])
                act(ee[:, c:c + 1], aTs[:, endc[c]:endc[c] + 1], AF.Exp, r=[kaT, kee], w=[kee])
            act(Ea, pa[:, 0:128], AF.Exp, r=[kpa], w=[kEa])
            Eb, kEb = A['Eb']
            act(Eb, pbm[:, 0:128], AF.Exp, r=[kpbm], w=[kEb])
            pt, kpt = bank_bf()
            tr(pt[:, 0:128], kk, identb, r=[kidb] + kin, w=[kpt])
            ktT, kktT = A['ktT']
            qtT, kqtT = A['qtT']
            qhT, kqhT = A['qhT']
            tt(ktT, pt[:, 0:128], Ek, ALU.mult, r=[kpt, kEk], w=[kktT])
            tt(qtT, qTt, Eq, ALU.mult, eng='pool', r=[kEq] + kin, w=[kqtT])
            tt(qhT, qTt, Ea, ALU.mult, eng='pool', r=[kEa] + kin, w=[kqhT])
            psc, kpsc = bank()
            mm(psc[:, 0:128], ktT, qtT, r=[kktT, kqtT], w=[kpsc])
            scT, kscT = A['scT']
            tt(scT, psc[:, 0:128], TI[d_], ALU.mult, r=[kpsc, kTI[d_]], w=[kscT])
            for c in range(2):
                khz, kkhz = A['khz'][c]
                stt(khz, kk, ind[:, c:c + 1], Eb, ALU.mult, ALU.mult, r=[kEb, kind] + kin, w=[kkhz])

        def step_back(d_, h, iv, kin, ops_, kops, A):
            S = S_all[:, d_ * 8 + h, :]
            Sb = Sb_all[:, d_ * 8 + h, :]
            scT, kscT = A['scT']
            qhT, kqhT = A['qhT']
            ee, kee = A['ee']
            mm(ops_, scT, iv, start=True, stop=False, r=[kscT] + kin, w=[kops])
            order = [0, 1] if d_ == 0 else [1, 0]
            for n_, c in enumerate(order):
                mm(ops_[64 * c:64 * c + 64, :], qhT[:, 64 * c:64 * c + 64], Sb, start=False, stop=True,
                   r=[kqhT, kSb[d_][h]], w=[kops])
                khz, kkhz = A['khz'][c]
                pd, kpd = bank()
                mm(pd[:, 0:128], khz, iv, r=[kkhz] + kin, w=[kpd])
                stt(S, S, ee[:, c:c + 1], pd[:, 0:128], ALU.mult, ALU.add, r=[kSh[d_][h], kee, kpd], w=[kSh[d_][h]])
                cp(Sb, S, eng='act', r=[kSh[d_][h]], w=[kSb[d_][h]])

        def step_scratch(H):
            A = {}
            A['Eb'] = H.f32([128, 128])
            A['ee'] = H.f32([128, 2])
            A['khz'] = [H.bf16([128, 128]), H.bf16([128, 128])]
            A['aTs'] = H.f32([128, 128])
            A['negr'] = H.f32([128, 2])
            A['Eq'] = H.f32([128, 128])
            A['Ek'] = H.f32([128, 128])
            A['Ea'] = H.f32([128, 128])
            A['ktT'] = H.bf16([128, 128])
            A['qtT'] = H.bf16([128, 128])
            A['qhT'] = H.bf16([128, 128])
            A['scT'] = H.bf16([128, 128])
            return A

        def hg_scratch(H):
            A = {}
            A['sg'] = H.f32([128, 128])
            A['Eb'] = H.f32([128, 128])
            A['ee'] = H.f32([128, 2])
            A['khz'] = [H.bf16([128, 128]), H.bf16([128, 128])]
            A['aTs'] = H.f32([128, 128])
            A['negr'] = H.f32([128, 2])
            A['Eq'] = H.f32([128, 128])
            A['Ek'] = H.f32([128, 128])
            A['Ea'] = H.f32([128, 128])
            A['ktT'] = H.bf16([128, 128])
            A['qtT'] = H.bf16([128, 128])
            A['qhT'] = H.bf16([128, 128])
            A['scT'] = H.bf16([128, 128])
            A['sq'] = H.f32([128, 512])
            A['kn'] = H.f32([128, 512])
            return A

        def load_lbh(lbh, klbh, h):
            for a in range(2):
                for d_ in range(2):
                    p.dma(lbh[:, a, d_, :], lbd[a, d_, h * 128:(h + 1) * 128].partition_broadcast(128),
                          reads=['lbd', klbh], writes=[klbh])

        def state_heads(d_, ntiles, A):
            lba, klba = A['lba']
            lfb, klfb0 = A['lf']
            kkb, kkkb0 = A['kk']
            ivb, kivb0 = A['iv']
            sg, ksg = A['sgw']
            n_ = ntiles
            for h in range(8):
                S = S_all[:, d_ * 8 + h, :]
                klfb, kkkb, kivb = klfb0, kkkb0, kivb0
                w_ = wbf[h % 2]
                kw_ = kwbf[h % 2]
                load_w(w_, kw_, w_in, (C_FF if d_ == 0 else C_FB) + h * 128, 128, 0)
                load_w(w_, kw_, w_in, C_I + h * 128, 128, 128)
                pf, kpf = bank()
                pi_, kpi = bank()
                for t in range(ntiles):
                    for kc in range(KC):
                        mm(pf[:, t * 128:(t + 1) * 128], hT[:, kc, t * 128:(t + 1) * 128], w_[:, kc, 0:128], start=(kc == 0), stop=(kc == KC - 1),
                           r=[kw_, khT], w=[kpf])
                        mm(pi_[:, t * 128:(t + 1) * 128], hT[:, kc, t * 128:(t + 1) * 128], w_[:, kc, 128:256], start=(kc == 0), stop=(kc == KC - 1),
                           r=[kw_, khT], w=[kpi])
                sgv = sg[:, 0:n_, :]
                act(sgv, pf[:, 0:n_ * 128].rearrange("p (a b) -> p a b", a=n_), AF.Exp, scale=-1.0, r=[kpf], w=[ksg])
                hs = slice(h * 128, (h + 1) * 128)
                gate_math(sgv, ksg, lba[:, 0, d_, hs].unsqueeze(1).to_broadcast([128, n_, 128]),
                          lba[:, 1, d_, hs].unsqueeze(1).to_broadcast([128, n_, 128]), klba)
                act(lfb[:, 0:n_, :], sgv, AF.Ln, r=[ksg], w=[klfb])
                ts(kkb[:, 0:n_, :], sgv, -1.0, 1.0, ALU.mult, ALU.add, r=[ksg], w=[kkkb])
                cp(ivb[:, 0:n_, :], pi_[:, 0:n_ * 128].rearrange("p (a b) -> p a b", a=n_), r=[kpi], w=[kivb])
                klfb, kkkb, kivb = klfb0, kkkb0, kivb0
                pa, kpa = bank()
                for t in range(ntiles):
                    mm(pa[:, 0:1], lfb[:, t, :], ones_f[:, 0:1], start=(t == 0), stop=(t == ntiles - 1),
                       r=[klfb, kof], w=[kpa])
                ee, kee = A['ee']
                act(ee[:, 0:1], pa[:, 0:1], AF.Exp, r=[kpa], w=[kee])
                pd, kpd = bank()
                for t in range(ntiles):
                    others = [t2 for t2 in range(ntiles) if (t2 > t if d_ == 0 else t2 < t)]
                    pb, kpb = bank()
                    mm(pb[:, 0:128], TXF[d_], lfb[:, t, :], start=True, stop=(len(others) == 0),
                       r=[kTXF[d_], klfb], w=[kpb])
                    for q_, t2 in enumerate(others):
                        mm(pb[:, 0:128], ones_f, lfb[:, t2, :], start=False, stop=(q_ == len(others) - 1),
                           r=[kof, klfb], w=[kpb])
                    Eb, kEb = A['Ebr'][t % 2]
                    khz, kkhz = A['khz'][t % 2]
                    act(Eb, pb[:, 0:128], AF.Exp, r=[kpb], w=[kEb])
                    tt(khz, kkb[:, t, :], Eb, ALU.mult, r=[kEb, kkkb], w=[kkhz])
                    mm(pd[:, 0:128], khz, ivb[:, t, :], start=(t == 0), stop=(t == ntiles - 1), r=[kkhz, kivb], w=[kpd])
                stt(S, S, ee[:, 0:1], pd[:, 0:128], ALU.mult, ALU.add, r=[kSh[d_][h], kee, kpd], w=[kSh[d_][h]])

        def state_pass(d_, row0, ntiles, src_d, ci, H, A):
            for t in range(ntiles):
                norm_T(src_d[row0 + t * 128: row0 + (t + 1) * 128, :], ci, hT, khT, t * 128)
            state_heads(d_, ntiles, A)

        def sp_scratch(H, ntiles):
            A = hg_scratch(H)
            A['lba'] = load_lba(H)
            A['sgw'] = H.f32([128, ntiles, 128])
            A['lf'] = H.f32([128, ntiles, 128])
            A['kk'] = H.bf16([128, ntiles, 128])
            A['iv'] = H.bf16([128, ntiles, 128])
            A['Ebr'] = [A['Eb'], H.f32([128, 128])]
            return A

        H = new_phase()
        A = sp_scratch(H, 2)
        for t in range(2):
            norm_T(ctxb[t * 128:(t + 1) * 128, :], 4, hT, khT, t * 128)
        for kvh in range(2):
            load_w(wbf[0], kwbf[0], w_in, C_K + kvh * 128, 128, 0)
            load_w(wbf[0], kwbf[0], w_in, C_V + kvh * 128, 128, 128)
            pb, kpb = bank()
            proj_fm(pb[:, 0:256], kpb, wbf[0], kwbf[0], 0, hT, khT, 0, 256)
            qk_norm_rope(pb[:, 0:256], kpb, 256, kg, kcT_c[:, kvh, :], kkc, A)
            for t in range(2):
                pb, kpb = bank()
                proj_tm(pb[:, 0:128], kpb, wbf[0], kwbf[0], 128, 128, hT, khT, t * 128)
                cp(v_c[:, kvh, t, :], pb[:, 0:128], eng='act', r=[kpb], w=[kvc])
        p.op('dve', lambda e: e.memset(S_all, 0.0), reads=[k for kk_ in kSh for k in kk_], writes=[k for kk_ in kSh for k in kk_])
        for d_ in range(2):
            state_heads(d_, 2, A)
        allS = [k for kk_ in kSh for k in kk_]
        cp(S_ctx, S_all, r=allS, w=[kSc])

        def reset_S(d_, fcol):
            for h in range(8):
                S = S_all[:, d_ * 8 + h, :]
                tt(S, S, S_ctx[:, d_ * 8 + h, :], ALU.subtract, r=[kSh[d_][h], kSc], w=[kSh[d_][h]])
                stt(S, S, fl[:, fcol:fcol + 1], S_ctx[:, d_ * 8 + h, :], ALU.mult, ALU.add,
                    r=[kSh[d_][h], kSc, kfl], w=[kSh[d_][h]])

        for m in range(CFG['nslot']):
            H = new_phase()
            A = sp_scratch(H, SGT)
            reset_S(0, 16 + m)
            for sg_ in range(CFG['nsub']):
                state_pass(0, (m + 1) * TOK + sg_ * SGT * 128, SGT, xr, 0, H, A)
        reset_S(0, 16 + 3)
        for m in range(CFG['nslot']):
            H = new_phase()
            A = sp_scratch(H, SGT)
            reset_S(1, 20 + m)
            for sg_ in range(CFG['nsub'] - 1, -1, -1):
                state_pass(1, (3 - m) * TOK + sg_ * SGT * 128, SGT, xr, 0, H, A)
        reset_S(1, 20 + 3)
        H = new_phase()
        A = sp_scratch(H, NT)
        kbS = [k for k in kSh[1]]
        for g in range(NG - 1, -1, -1):
            p.dma(ssave[g].rearrange("p (h v) -> p h v", h=8), S_all[:, 8:16, :], reads=kbS, writes=['ssave%d' % g], key='ssv')
            if g > 0 and g < CFG['ng']:
                state_pass(1, g * G, NT, xr, 0, H, A)

        for g in range(CFG['ng']):
            H = new_phase()
            A = hg_scratch(H)
            catT, kcat = H.bf16([128, KC, G])
            p.dma(S_all[:, 8:16, :], ssave[g].rearrange("p (h v) -> p h v", h=8), reads=['ssave%d' % g], writes=kbS, key='ssl')
            for h in range(8):
                cp(Sb_all[:, h, :], S_all[:, h, :], eng='act', r=[kSh[0][h]], w=[kSb[0][h]])
                cp(Sb_all[:, 8 + h, :], S_all[:, 8 + h, :], eng='act', r=[kSh[1][h]], w=[kSb[1][h]])
            for t in range(EXT):
                r0 = (g * G - 128 + t * 128) % SEQ
                norm_T(xr[r0:r0 + 128, :], 0, hT, khT, t * 128)
            NX = EXT * 128
            cosT, kcos = H.f32([128, NX])
            sinT, ksin = H.f32([128, NX])
            rope_mark = H.off
            pos, kpos = H.f32([128, NX])
            nrow = NX // 64
            p.op('pool', lambda e, pos=pos, g=g: e.iota(pos[0:64, :].rearrange("p (a b) -> p a b", b=64), pattern=[[1, nrow], [0, 64]],
                                                    base=8 * g - 2, channel_multiplier=0, allow_small_or_imprecise_dtypes=True),
                 writes=[kpos])
            p.op('pool', lambda e, pos=pos: e.iota(pos[64:128, :].rearrange("p (a b) -> p a b", b=64), pattern=[[0, nrow], [1, 64]],
                                               base=0, channel_multiplier=0, allow_small_or_imprecise_dtypes=True),
                 reads=[kpos], writes=[kpos])
            ts(pos[0:64, :], pos[0:64, :], fl[0:64, 10:11], None, ALU.add, r=[kpos, kfl], w=[kpos])
            ts(pos, pos, invf, None, ALU.mult, r=[kpos, ksm], w=[kpos])
            tA, ktA = H.f32([128, NX])
            tB, ktB = H.f32([128, NX])
            tK, ktK = H.f32([128, NX])
            tKi = tK.bitcast(mybir.dt.int32)

            def sin_table(dst, kdst, shift):
                ts(tB, pos, shift, None, ALU.add, r=[kpos], w=[ktB])
                ts(tA, tB, 1.0 / (2 * PI), None, ALU.mult, r=[ktB], w=[ktA])
                cp(tKi, tA, r=[ktA], w=[ktK])
                cp(tA, tKi, r=[ktK], w=[ktA])
                stt(tB, tA, -2 * PI, tB, ALU.mult, ALU.add, r=[ktA, ktB], w=[ktB])
                ts(tA, tB, PI, None, ALU.is_gt, r=[ktB], w=[ktA])
                stt(tB, tA, -2 * PI, tB, ALU.mult, ALU.add, r=[ktA, ktB], w=[ktB])
                ts(tA, tB, -PI, None, ALU.is_lt, r=[ktB], w=[ktA])
                stt(tB, tA, 2 * PI, tB, ALU.mult, ALU.add, r=[ktA, ktB], w=[ktB])
                act(dst, tB, AF.Sin, r=[ktB], w=[kdst])
            sin_table(sinT, ksin, 0.0)
            ts(sinT, sinT, sgn, None, ALU.mult, r=[ksin, ksm], w=[ksin])
            sin_table(cosT, kcos, 0.5 * PI)
            ktab = kcos
            H.off = rope_mark
            p.barrier()
            kTn, kkTn = H.bf16([128, NX])
            Vt, kVt = H.bf16([128, EXT, 128])
            qTn, kqTn = H.bf16([128, G])
            PT, kPT = H.bf16([128, 5, 128])
            rden, krd = H.f32([128, 128])
            for kvh in range(2):
                load_w(wbf[0], kwbf[0], w_in, C_K + kvh * 128, 128, 0)
                load_w(wbf[0], kwbf[0], w_in, C_V + kvh * 128, 128, 128)
                for t0 in range(0, NX, 512):
                    n = min(512, NX - t0)
                    pb, kpb = bank()
                    proj_fm(pb[:, 0:n], kpb, wbf[0], kwbf[0], 0, hT, khT, t0, n)
                    qk_norm_rope(pb[:, 0:n], kpb, n, kg, kTn[:, t0:t0 + n], kkTn, A, cosT[:, t0:t0 + n], sinT[:, t0:t0 + n], ktab)
                for t in range(EXT):
                    pb, kpb = bank()
                    proj_tm(pb[:, 0:128], kpb, wbf[0], kwbf[0], 128, 128, hT, khT, t * 128)
                    cp(Vt[:, t, :], pb[:, 0:128], eng='act', r=[kpb], w=[kVt])
                for hq in range(4):
                    hh = kvh * 4 + hq
                    load_w(wbf[1], kwbf[1], w_in, C_QA + hh * 128, 128, 0)
                    pb, kpb = bank()
                    proj_fm(pb[:, 0:G], kpb, wbf[1], kwbf[1], 0, hT, khT, 128, G)
                    qk_norm_rope(pb[:, 0:G], kpb, G, qg, qTn, kqTn, A, cosT[:, 128:128 + G], sinT[:, 128:128 + G], ktab)
                    for qt in range(NT):
                        ps0, kps0 = bank()
                        ps1, kps1 = bank()
                        qs = qTn[:, qt * 128:(qt + 1) * 128]
                        for kb in range(3):
                            mm(ps0[:, kb * 128:(kb + 1) * 128], kTn[:, (qt + kb) * 128:(qt + kb + 1) * 128], qs,
                               r=[kkTn, kqTn], w=[kps0])
                        for cb in range(2):
                            mm(ps1[:, cb * 128:(cb + 1) * 128], kcT_c[:, kvh, cb * 128:(cb + 1) * 128], qs,
                               r=[kkc, kqTn], w=[kps1])
                        sc = float(128 ** -0.5)
                        act(PT[:, 0:3, :], ps0[:, 0:384].rearrange("p (a b) -> p a b", a=3), AF.Exp, scale=sc, r=[kps0], w=[kPT])
                        act(PT[:, 3:5, :], ps1[:, 0:256].rearrange("p (a b) -> p a b", a=2), AF.Exp, scale=sc, r=[kps1, kPT], w=[kPT])
                        mP = maskP0 if (g == 0 and qt == 0) else maskP
                        mN = maskN0 if (g == NG - 1 and qt == NT - 1) else maskN
                        tt(PT[:, 0, :], PT[:, 0, :], mP, ALU.mult, r=[kPT, kmP, kmP0], w=[kPT])
                        tt(PT[:, 2, :], PT[:, 2, :], mN, ALU.mult, eng='pool', r=[kPT, kmN, kmN0], w=[kPT])
                        po, kpo = bank()
                        for bi in range(5):
                            vv = Vt[:, qt + bi, :] if bi < 3 else v_c[:, kvh, bi - 3, :]
                            mm(po[:, 0:128], vv, PT[:, bi, :], start=(bi == 0), stop=(bi == 4), r=[kVt, kvc, kPT], w=[kpo])
                        for bi in range(5):
                            mm(po[:, 128:256], ones_b, PT[:, bi, :], start=(bi == 0), stop=(bi == 4), r=[kob, kPT], w=[kpo])
                        act(rden, po[:, 128:256], AF.Ln, bias=esink[:, hh:hh + 1], scale=1.0, r=[kpo, ksm], w=[krd])
                        act(rden, rden, AF.Exp, scale=-1.0, r=[krd], w=[krd])
                        tt(catT[:, hh, qt * 128:(qt + 1) * 128], po[:, 0:128], rden, ALU.mult, r=[kpo, krd], w=[kcat])
            lba, klba = load_lba(H)
            SA = [A, step_scratch(H)]
            lfb, klfb = H.f32([128, 2, NT, 128])
            kkb, kkkb = H.bf16([128, 2, NT, 128])
            sgw, ksgw = H.f32([128, 2, NT, 128])
            ivb, kivb = H.bf16([128, NT, 128])
            sgb, ksgb = H.f32([128, NT, 128])
            qTh, kqTh = H.bf16([128, G])
            ob, kob_ = H.f32([128, NT, 128])
            ycat, kyc = H.bf16([128, 128])
            osb, kosb = H.f32([128, 128])
            for h in range(8):
                w_ = wbf[h % 2]
                kw_ = kwbf[h % 2]
                for bi, c0 in enumerate([C_FF, C_FB, C_I, C_GG, C_QH]):
                    load_w(w_, kw_, w_in, c0 + h * 128, 128, bi * 128)
                pbs = [bank() for _ in range(4)]
                for t in range(NT):
                    for kc in range(KC):
                        for bi in range(4):
                            mm(pbs[bi][0][:, t * 128:(t + 1) * 128], hT[:, kc, (t + 1) * 128:(t + 2) * 128], w_[:, kc, bi * 128:(bi + 1) * 128],
                               start=(kc == 0), stop=(kc == KC - 1), r=[kw_, khT], w=[pbs[bi][1]])
                for d_ in range(2):
                    act(sgw[:, d_], pbs[d_][0][:, 0:G].rearrange("p (a b) -> p a b", a=NT), AF.Exp, scale=-1.0, r=[pbs[d_][1], ksgw], w=[ksgw])
                hs = slice(h * 128, (h + 1) * 128)
                gate_math(sgw, ksgw, lba[:, 0, :, hs].unsqueeze(2).to_broadcast([128, 2, NT, 128]),
                          lba[:, 1, :, hs].unsqueeze(2).to_broadcast([128, 2, NT, 128]), klba)
                act(lfb, sgw, AF.Ln, r=[ksgw], w=[klfb])
                ts(kkb, sgw, -1.0, 1.0, ALU.mult, ALU.add, r=[ksgw], w=[kkkb])
                cp(ivb, pbs[2][0][:, 0:G].rearrange("p (a b) -> p a b", a=NT), r=[pbs[2][1]], w=[kivb])
                act(sgb, pbs[3][0][:, 0:G].rearrange("p (a b) -> p a b", a=NT), AF.Exp, scale=-1.0, r=[pbs[3][1]], w=[ksgb])
                act(sgb, sgb, AF.Ln, bias=small[:, 22:23], scale=1.0, r=[ksgb, ksm], w=[ksgb])
                act(sgb, sgb, AF.Exp, scale=-1.0, r=[ksgb], w=[ksgb])
                tt(sgb, sgb, pbs[3][0][:, 0:G].rearrange("p (a b) -> p a b", a=NT), ALU.mult, r=[ksgb, pbs[3][1]], w=[ksgb])
                pq, kpq = bank()
                proj_fm(pq[:, 0:G], kpq, w_, kw_, 512, hT, khT, 128, G)
                cp(qTh, pq[:, 0:G], eng='act', r=[kpq], w=[kqTh])
                hsteps = [(1, t) for t in range(NT - 1, -1, -1)] + [(0, t) for t in range(NT)]
                kin_h = [klfb, kkkb, kivb, kqTh]

                def h_front(i, h=h):
                    d_, t = hsteps[i]
                    step_front(d_, h, lfb[:, d_, t, :], kkb[:, d_, t, :], ivb[:, t, :], qTh[:, t * 128:(t + 1) * 128], kin_h, SA[i % 2])

                def h_back(i, h=h):
                    d_, t = hsteps[i]
                    po, kpo = bank()
                    step_back(d_, h, ivb[:, t, :], kin_h, po[:, 0:128], kpo, SA[i % 2])
                    if d_ == 1:
                        cp(ob[:, t, :], po[:, 0:128], r=[kpo], w=[kob_ + str(t)])
                        return
                    tt(osb, po[:, 0:128], ob[:, t, :], ALU.add, r=[kpo, kob_ + str(t)], w=[kosb])
                    p.op('dve', lambda e: e.memset(sstat[:, 4:5], 0.0), reads=[kss], writes=[kss])
                    sq, ksq = A['sq']
                    act(sq[:, 0:128], osb, AF.Square, accum=sstat[:, 4:5], r=[kosb, kss], w=[ksq, kss])
                    act(sstat[:, 5:6], sstat[:, 4:5], AF.Ln, bias=small[:, 20:21], scale=1.0 / 128, r=[kss, ksm], w=[kss])
                    act(sstat[:, 6:7], sstat[:, 5:6], AF.Exp, scale=-0.5, r=[kss], w=[kss])
                    stt(osb, osb, sstat[:, 6:7], ogain, ALU.mult, ALU.mult, r=[kosb, kss, kog], w=[kosb])
                    tt(ycat, osb, sgb[:, t, :], ALU.mult, r=[kosb, ksgb], w=[kyc])
                    pt, kpt = bank_bf()
                    tr(pt[:, 0:128], ycat, identb, r=[kyc, kidb], w=[kpt])
                    cp(catT[:, 8 + h, t * 128:(t + 1) * 128], pt[:, 0:128], eng='act', r=[kpt], w=[kcat])
                h_front(0)
                for i in range(len(hsteps)):
                    if i + 1 < len(hsteps):
                        h_front(i + 1)
                    h_back(i)
            gbc, kgbc = XS, kXS
            p.dma(gbc, modd[2].partition_broadcast(128), reads=['modd'], writes=[kgbc])
            for cg in range(4):
                w_ = wbf[cg % 2]
                kw_ = kwbf[cg % 2]
                load_w(w_, kw_, w_out, cg * 512, 512, 0)
                for t in range(NT):
                    if cg == 0:
                        pass
                    pb, kpb = bank()
                    for kc in range(KC):
                        mm(pb[:], catT[:, kc, t * 128:(t + 1) * 128], w_[:, kc, 0:512], start=(kc == 0), stop=(kc == KC - 1),
                           r=[kcat, kw_], w=[kpb])
                    r0 = g * G + t * 128
                    xa, kxa = A['sq']
                    p.dma(xa, xr[r0:r0 + 128, cg * 512:(cg + 1) * 512], writes=[kxa])
                    tt(A['kn'][0], pb[:], gbc[:, cg * 512:(cg + 1) * 512], ALU.mult, r=[kpb, kgbc], w=[A['kn'][1]])
                    tt(A['kn'][0], A['kn'][0], xa, ALU.add, r=[A['kn'][1], kxa], w=[A['kn'][1]])
                    p.dma(x1s[r0:r0 + 128, cg * 512:(cg + 1) * 512], A['kn'][0], reads=[A['kn'][1]], writes=['x1s'], key=A['kn'][1])

            H = new_phase()
            for t in range(NT):
                r0 = g * G + t * 128
                norm_T(x1s[r0:r0 + 128, :], 2, hT, khT, t * 128)
            s1, ks1 = H.f32([128, NT, 8, 128])
            s2, ks2 = H.f32([128, NT, 8, 128])
            cdiag, kcd = H.bf16([128, NT, 8, 128])
            off_mark = H.off
            qTb, kqTb = H.f32([128, G])
            skT, kskT = H.f32([128, 128])
            t16a, kt16a = H.f32([128, 16])
            t16b, kt16b = H.f32([128, 16])
            scr, kscr = H.f32([128, 256])
            cand, kcand = H.f32([128, 256])
            best, kbest = H.f32([128, 16])
            pst, kpst = H.f32([128, 8])
            for hp in range(16):
                hd, half = hp // 2, hp % 2
                load_w(wbf[hp % 2], kwbf[hp % 2], w_q, hp * 128, 128, 0)
                pb, kpb = bank()
                proj_fm(pb[:, 0:G], kpb, wbf[hp % 2], kwbf[hp % 2], 0, hT, khT, 0, G)
                cp(qTb, pb[:, 0:G], eng='act', r=[kpb], w=[kqTb])
                p.dma(XS[:, 0:128], skd[hp], writes=[kXS])
                pt, kpt = bank()
                tr(pt[:, 0:128], XS[:, 0:128], ident, r=[kXS, kid], w=[kpt])
                cp(skT, pt[:, 0:128], r=[kpt], w=[kskT])
                dst = s1 if half == 0 else s2
                kd = ks1 if half == 0 else ks2
                for t in range(NT):
                    pb2, kpb2 = bank()
                    mm(pb2[:, 0:128], qTb[:, t * 128:(t + 1) * 128], skT, r=[kqTb, kskT], w=[kpb2])
                    cp(dst[:, t, hd, :], pb2[:, 0:128], eng='act', r=[kpb2], w=[kd])
            for t in range(NT):
                for hd in range(8):
                    for (src, t16) in [(s1, t16a), (s2, t16b)]:
                        kt16 = kt16a if t16 is t16a else kt16b
                        p.op('dve', lambda e, src=src, t16=t16, t=t, hd=hd: e.max(out=t16[:, 0:8], in_=src[:, t, hd, :]), reads=[ks1, ks2], writes=[kt16])
                        p.op('dve', lambda e, src=src, t16=t16, t=t, hd=hd: e.match_replace(out=scr[:, 0:128], in_to_replace=t16[:, 0:8], in_values=src[:, t, hd, :], imm_value=-1e30),
                             reads=[ks1, ks2, kt16], writes=[kscr])
                        p.op('dve', lambda e, t16=t16: e.max(out=t16[:, 8:16], in_=scr[:, 0:128]), reads=[kscr, kt16], writes=[kt16])
                    tt(cand.rearrange("p (a b) -> p a b", a=16), t16a.unsqueeze(2).to_broadcast([128, 16, 16]),
                       t16b.unsqueeze(1).to_broadcast([128, 16, 16]), ALU.add, r=[kt16a, kt16b], w=[kcand])
                    p.op('dve', lambda e: e.max(out=best[:, 0:8], in_=cand), reads=[kcand], writes=[kbest])
                    p.op('dve', lambda e: e.match_replace(out=scr, in_to_replace=best[:, 0:8], in_values=cand, imm_value=-1e30),
                         reads=[kcand, kbest], writes=[kscr])
                    p.op('dve', lambda e: e.max(out=best[:, 8:16], in_=scr), reads=[kscr, kbest], writes=[kbest])
                    p.op('dve', lambda e: e.tensor_reduce(out=pst[:, 0:1], in_=best, axis=AX.X, op=ALU.max), reads=[kbest, kpst], writes=[kpst])
                    p.op('dve', lambda e: e.tensor_reduce(out=pst[:, 1:2], in_=best, axis=AX.X, op=ALU.min), reads=[kbest, kpst], writes=[kpst])
                    ts(pst[:, 2:3], pst[:, 0:1], -1.0, None, ALU.mult, r=[kpst], w=[kpst])
                    p.op('dve', lambda e: e.memset(pst[:, 3:4], 0.0), reads=[kpst], writes=[kpst])
                    act(scr[:, 0:16], best, AF.Exp, bias=pst[:, 2:3], scale=1.0, accum=pst[:, 3:4], r=[kbest, kpst], w=[kscr, kpst])
                    tt(pst[:, 4:5], pst[:, 1:2], pst[:, 0:1], ALU.subtract, r=[kpst], w=[kpst])
                    act(pst[:, 4:5], pst[:, 4:5], AF.Exp, r=[kpst], w=[kpst])
                    p.op('dve', lambda e: e.reciprocal(out=pst[:, 5:6], in_=pst[:, 3:4]), reads=[kpst], writes=[kpst])
                    tt(pst[:, 4:5], pst[:, 4:5], pst[:, 5:6], ALU.mult, r=[kpst], w=[kpst])
                    ts(cdiag[:, t, hd, :], ident, pst[:, 4:5], None, ALU.mult, r=[kid, kpst], w=[kcd])
                    ts(s1[:, t, hd, :], s1[:, t, hd, :], pst[:, 1:2], None, ALU.subtract, r=[ks1, kpst], w=[ks1])
            H.off = off_mark
            p.barrier()
            acc, kacc = H.f32([128, NT, D])
            kaccs = [kacc + str(t) for t in range(NT)]
            p.op('pool', lambda e: e.memset(acc, 0.0), writes=kaccs)
            NB = 4
            uTv = [wbfF[0][:, i * 2048:(i + 1) * 2048].rearrange("p (a b) -> p a b", a=KC) for i in range(2)]
            kuTv = ['uTv0', 'uTv1']
            vbv = [wbfF[0][:, 4096 + i * 2048: 4096 + (i + 1) * 2048] for i in range(3)] + \
                  [wbfF[1][:, i * 2048:(i + 1) * 2048] for i in range(5)]
            kvbv = ['vbv%d' % i for i in range(8)]
            Lbr = [(XS[:, i * 1024:(i + 1) * 1024].rearrange("p (a b) -> p a b", a=8), 'Lbr%d' % i) for i in range(2)] + \
                  [(wstF[:, i * 1024:(i + 1) * 1024].rearrange("p (a b) -> p a b", a=8), 'Lbr%d' % (2 + i)) for i in range(2)]
            Xbr = [H.bf16([128, 8, 128]) for _ in range(4)]
            Xmr = [H.bf16([128, 8, 128]) for _ in range(4)]
            gelr = [(xn[:, i * 512:(i + 1) * 512], 'gelr%d' % i) for i in range(4)]
            kGTc = ['gtc%d' % i for i in range(8)]
            p.barrier()
            nblk = CFG['nec'] // NB
            cnt2 = [0]

            def s1_block(blk):
                for c in range(NB):
                    ec = blk * NB + c
                    uT_, kuT_ = uTv[ec % 2], kuTv[ec % 2]
                    vb_, kvb_ = vbv[ec % 8], kvbv[ec % 8]
                    p.dma(uT_, uTd[ec].rearrange("p (a b) -> p a b", a=KC), writes=[kuT_])
                    p.dma(vb_, vd[ec], writes=[kvb_])
                    pa, kpa = bank()
                    for kc in range(KC):
                        mm(pa[:, 0:G], uT_[:, kc, :], hT[:, kc, 0:G], start=(kc == 0), stop=(kc == KC - 1), r=[kuT_, khT], w=[kpa])
                    gel, kgel = gelr[c]
                    act(gel, pa[:, 0:G], AF.Gelu, r=[kpa], w=[kgel])
                for c in range(NB):
                    ec = blk * NB + c
                    gel, kgel = gelr[c]
                    pw, kpw = bank()
                    for t in range(NT):
                        j = cnt2[0] % 4
                        cnt2[0] += 1
                        Lb, kLb = Lbr[j]
                        Xb, kXb = Xbr[j]
                        Xm, kXm = Xmr[j]
                        tt(Lb, s2[:, t], s1[:, t, :, ec:ec + 1].to_broadcast([128, 8, 128]), ALU.add, eng=('pool' if t % 2 == 0 else 'dve'),
                           r=[ks1, ks2], w=[kLb])
                        act(Xb, Lb, AF.Exp, r=[kLb], w=[kXb])
                        stt(Xm, Lb, 0.0, Xb, ALU.is_ge, ALU.mult, r=[kLb, kXb], w=[kXm])
                        for hd in range(8):
                            mm(pw[:, t * 128:(t + 1) * 128], Xm[:, hd, :], cdiag[:, t, hd, :], start=(hd == 0), stop=(hd == 7),
                               r=[kXm, kcd], w=[kpw])
                    gi = (blk % 2) * NB + c
                    GTc = hT[:, 2 * gi:2 * gi + 2, 512:768]
                    tt(GTc, gel.rearrange("p (a b) -> p a b", a=2), pw[:, 0:G].rearrange("p (a b) -> p a b", a=2), ALU.mult,
                       r=[kgel, kpw], w=[kGTc[gi]])

            def s2_block(blk):
                for t in range(NT):
                    for dh in range(2):
                        pvb = []
                        for q in range(2):
                            pvv, kpvv = bank()
                            pvb.append((pvv, kpvv))
                            dg = dh * 2 + q
                            for c in range(NB):
                                ec = blk * NB + c
                                gi = (blk % 2) * NB + c
                                mm(pvv[:], hT[:, 2 * gi + t // 2, 512 + (t % 2) * 128: 512 + (t % 2) * 128 + 128],
                                   vbv[ec % 8][:, dg * 512:(dg + 1) * 512], start=(c == 0), stop=(c == NB - 1),
                                   r=[kGTc[gi], kvbv[ec % 8]], w=[kpvv])
                        for q in range(2):
                            dg = dh * 2 + q
                            pvv, kpvv = pvb[q]
                            tt(acc[:, t, dg * 512:(dg + 1) * 512], acc[:, t, dg * 512:(dg + 1) * 512], pvv[:], ALU.add,
                               r=[kacc + str(t), kpvv], w=[kacc + str(t)])
            for blk in range(nblk):
                s1_block(blk)
                if blk > 0:
                    s2_block(blk - 1)
            s2_block(nblk - 1)
            p.barrier()
            un = [XS, wstF]
            kun = [[kXS], list(kwst)]
            p.dma(un[1], modd[5].partition_broadcast(128), reads=['modd'], writes=kun[1])
            for t in range(NT):
                r0 = g * G + t * 128
                p.dma(XS, x1s[r0:r0 + 128, :], reads=['x1s'], writes=[kXS])
                tt(acc[:, t, :], acc[:, t, :], un[1], ALU.mult, r=[kacc + str(t)] + kun[1], w=[kacc + str(t)])
                tt(acc[:, t, :], acc[:, t, :], XS, ALU.add, r=[kacc + str(t), kXS], w=[kacc + str(t)])
                p.dma(outd[r0:r0 + 128, :], acc[:, t, :], reads=[kacc + str(t)], writes=['outd'], key='outk', final=True)
        print("ops recorded", p.nops, {e: len(v) for e, v in p.ops.items()}, "dsems", len(p.dsem))
        p.emit()
    return nc


def make_in_maps(inputs):
    x = np.asarray(inputs["x"], np.float32)
    f32 = lambda k: np.ascontiguousarray(np.asarray(inputs[k], np.float32))
    in_maps = []
    for core in range(8):
        b, j = core // 4, core % 4
        fl = np.zeros((128, 32), np.float32)
        for m in range(4):
            rf = 1.0 if (j - 3 + m) <= 0 else 0.0
            rb = 1.0 if (j + 3 - m) >= 3 else 0.0
            fl[:, 16 + m] = 1.0 - rf
            fl[:, 20 + m] = 1.0 - rb
        fl[:, 8] = 1.0 if j > 0 else 0.0
        fl[:, 9] = 1.0 if j < 3 else 0.0
        fl[:, 10] = float(j * 64)
        in_maps.append({
            "xr": np.ascontiguousarray(np.roll(x[b], -j * TOK, axis=0)),
            "ctxb": f32("ctx")[b],
            "cvec": np.ascontiguousarray(np.stack([f32("c")[b], f32("c_ctx")])),
            "w_ada": f32("w_ada")[0], "b_ada": f32("b_ada")[0],
            "norm_mix": f32("norm_mix")[0], "norm_ffn": f32("norm_ffn")[0],
            "w_in": f32("w_in")[0], "q_norm": f32("q_norm")[0], "k_norm": f32("k_norm")[0],
            "attn_sink": f32("attn_sink")[0], "lbl": f32("hgrn_lb_logits"),
            "hgrn_norm": f32("hgrn_norm")[0], "w_out": f32("w_out")[0], "w_q": f32("peer_w_q")[0],
            "sk": f32("peer_sub_keys")[0].reshape(16, 128, 128),
            "pu": f32("peer_u")[0], "pv": f32("peer_v")[0],
            "flags": fl,
        })
    return in_maps


def kernel(**inputs):
    nc = build_nc()
    in_maps = make_in_maps(inputs)
    res = run_bass_kernel_spmd(nc, in_maps, core_ids=list(range(8)))
    out = np.zeros((2, SEQ, D), np.float32)
    for core in range(8):
        b, j = core // 4, core % 4
        out[b, j * TOK:(j + 1) * TOK] = res.results[core]["out"]
    return out
```

```python
import numpy as np
from contextlib import ExitStack
import concourse.bass as bass
import concourse.mybir as mybir
from concourse.bass_utils import run_bass_kernel_spmd

F32 = mybir.dt.float32
BF16 = mybir.dt.bfloat16
ALU = mybir.AluOpType
AF = mybir.ActivationFunctionType
AX = mybir.AxisListType

D = 2048
KC = 16
TOK = 4096
SEQ = 16384
G = 512
NT = G // 128
NG = TOK // G
EXT = NT + 2
SGT = 4
NCOL = 6656
C_K, C_V, C_FF, C_FB, C_I, C_QA, C_QH, C_GG = 0, 256, 512, 1536, 2560, 3584, 4608, 5632
EPS = 1e-6
NE = 16384
ENG = ['pe', 'act', 'dve', 'pool', 'sp']
CFG = dict(ng=NG, nec=NE // 128, nslot=3, nsub=TOK // (SGT * 128), mods=6)
PI = float(np.pi)


class Prog:
    def __init__(self, nc, es):
        self.nc = nc
        self.es = es
        self.ops = {e: [] for e in ENG}
        self.esem = {e: es.enter_context(nc.semaphore('sem_' + e)) for e in ENG if e != 'sp'}
        self.cnt = {e: 0 for e in ENG}
        self.known = {e: {} for e in ENG}
        self.bufs = {}
        self.dsem = {}
        self.final = []
        self.floor = {}
        self.nops = 0

    def _deps(self, reads, writes):
        deps = dict(self.floor)

        def add(s, v):
            if deps.get(s, 0) < v:
                deps[s] = v
        for k in reads:
            b = self.bufs.get(k)
            if b and b['w']:
                add(*b['w'])
        for k in writes:
            b = self.bufs.get(k)
            if b:
                if b['w']:
                    add(*b['w'])
                for s, v in b['r'].items():
                    add(s, v)
        return deps

    def _commit(self, reads, writes, ev):
        s, v = ev
        for k in reads:
            b = self.bufs.setdefault(k, {'w': None, 'r': {}})
            if b['r'].get(s, 0) < v:
                b['r'][s] = v
        for k in writes:
            self.bufs[k] = {'w': ev, 'r': {}}

    def _waits(self, eng, deps):
        waits = []
        kn = self.known[eng]
        for s, v in deps.items():
            if eng == 'pe' and s is self.esem['pe']:
                continue
            if kn.get(s, 0) < v:
                kn[s] = v
                waits.append((s, v))
        return waits

    def op(self, eng, fn, reads=(), writes=()):
        deps = self._deps(reads, writes)
        waits = self._waits(eng, deps)
        self.cnt[eng] += 1
        ev = (self.esem[eng], self.cnt[eng])
        self.ops[eng].append((waits, fn, ev[0], 1))
        self._commit(reads, writes, ev)
        self.nops += 1

    def dma(self, out, in_, reads=(), writes=(), key=None, eng='sp', final=False, **kw):
        if key is None:
            key = (list(writes) + list(reads))[0]
        if key not in self.dsem:
            self.dsem[key] = [self.es.enter_context(self.nc.semaphore('dsem%d' % len(self.dsem))), 0]
        ds = self.dsem[key]
        deps = self._deps(reads, writes)
        waits = self._waits(eng, deps)
        ds[1] += 16
        ev = (ds[0], ds[1])
        self.ops[eng].append((waits, lambda e: e.dma_start(out=out, in_=in_, **kw), ds[0], 16))
        self._commit(reads, writes, ev)
        self.nops += 1
        if final:
            self.final.append(ev)

    def barrier(self):
        for e in ENG:
            if e != 'sp' and self.cnt[e] > 0:
                self.floor[self.esem[e]] = self.cnt[e]
        for k, (s, v) in self.dsem.items():
            if v > 0:
                self.floor[s] = v

    def emit(self):
        nc = self.nc
        engmap = {'pe': 'tensor', 'act': 'scalar', 'dve': 'vector', 'pool': 'gpsimd', 'sp': 'sync'}
        with nc.Block() as block:
            for e in ENG:
                ops = self.ops[e]
                fin = self.final if e == 'sp' else []

                def body(eng, ops=ops, fin=fin):
                    for waits, fn, sem, inc in ops:
                        for s, v in waits:
                            eng.wait_ge(s, v)
                        fn(eng).then_inc(sem, inc)
                    for s, v in fin:
                        eng.wait_ge(s, v)
                getattr(block, engmap[e])(body)


class Arena:
    def __init__(self, ap, size, name):
        self.ap, self.size, self.off, self.name, self.n = ap, size, 0, name, 0

    def _shape(self, v, shape):
        if shape[0] < 128:
            v = v[0:shape[0]]
        if len(shape) == 2:
            return v
        if len(shape) == 3:
            return v.rearrange("p (a b) -> p a b", a=shape[1])
        return v.rearrange("p (a b c) -> p a b c", a=shape[1], b=shape[2])

    def _take(self, nf):
        assert self.off + nf <= self.size, (self.name, self.off, nf, self.size)
        v = self.ap[:, self.off:self.off + nf]
        self.off += nf
        self.n += 1
        return v, '%s_%d_%d' % (self.name, self.off, self.n)

    def f32(self, shape):
        n = int(np.prod(shape[1:]))
        v, k = self._take(n)
        return self._shape(v, shape), k

    def bf16(self, shape):
        n = int(np.prod(shape[1:]))
        v, k = self._take((n + 1) // 2)
        v = v.bitcast(BF16)[:, 0:n]
        return self._shape(v, shape), k


def build_nc():
    nc = bass.Bass("TRN2", target_bir_lowering=False)
    di = lambda n, s: nc.dram_tensor(n, s, F32, kind="ExternalInput").ap()
    xr = di("xr", [SEQ, D])
    ctxb = di("ctxb", [256, D])
    cvec = di("cvec", [2, D])
    w_ada = di("w_ada", [D, 6 * D])
    b_ada = di("b_ada", [6 * D])
    norm_mix = di("norm_mix", [D])
    norm_ffn = di("norm_ffn", [D])
    w_in = di("w_in", [D, NCOL])
    q_norm = di("q_norm", [128])
    k_norm = di("k_norm", [128])
    attn_sink = di("attn_sink", [8])
    lbl = di("lbl", [2, 2, 1024])
    hgrn_norm = di("hgrn_norm", [128])
    w_out = di("w_out", [D, D])
    w_q = di("w_q", [D, D])
    skd = di("sk", [16, 128, 128])
    pu = di("pu", [NE, D])
    pv = di("pv", [NE, D])
    flags = di("flags", [128, 32])
    outd = nc.dram_tensor("out", [TOK, D], F32, kind="ExternalOutput").ap()
    modd = nc.dram_tensor("modd", [8, D], F32, kind="Internal").ap()
    lbd = nc.dram_tensor("lbd", [2, 2, 1024], F32, kind="Internal").ap()
    x1s = nc.dram_tensor("x1s", [TOK, D], F32, kind=("ExternalOutput" if CFG.get("dbg") else "Internal")).ap()
    ssave = nc.dram_tensor("ssave", [NG, 128, 1024], F32, kind="Internal").ap()
    w_in_b = nc.dram_tensor("w_in_b", [D, NCOL], BF16, kind="Internal").ap()
    w_out_b = nc.dram_tensor("w_out_b", [D, D], BF16, kind="Internal").ap()
    w_q_b = nc.dram_tensor("w_q_b", [D, D], BF16, kind="Internal").ap()
    uTd = nc.dram_tensor("uTd", [NE // 128, 128, D], BF16, kind="Internal").ap()
    vd = nc.dram_tensor("vd", [NE // 128, 128, D], BF16, kind="Internal").ap()

    with ExitStack() as es:
        PERS = 29960
        PH = 23240
        pers_t = es.enter_context(nc.sbuf_tensor("pers", [128, PERS], F32))
        ph_t = es.enter_context(nc.sbuf_tensor("phase", [128, PH], F32))
        PS = [es.enter_context(nc.psum_tensor("ps%d" % i, [128, 512], F32)) for i in range(8)]
        p = Prog(nc, es)
        PA = Arena(pers_t[:], PERS, 'P')
        ph_n = [0]

        def new_phase():
            p.barrier()
            ph_n[0] += 1
            return Arena(ph_t[:], PH, 'H%d' % ph_n[0])

        psn = [0]

        def bank():
            i = psn[0] % 8
            psn[0] += 1
            return PS[i], 'psb%d' % i

        def bank_bf():
            t, k = bank()
            return t[:].bitcast(BF16), k

        val, kval = PA.f32([128, 128])
        vs, kvs = PA.f32([128, 128])
        vt, kvt = PA.f32([128, 128])
        p.op('pool', lambda e: e.iota(val, pattern=[[1, 128]], base=0, channel_multiplier=-1,
                                      allow_small_or_imprecise_dtypes=True), writes=[kval])
        p.op('pool', lambda e: e.iota(vs, pattern=[[0, 128]], base=0, channel_multiplier=1,
                                      allow_small_or_imprecise_dtypes=True), writes=[kvs])
        p.op('pool', lambda e: e.iota(vt, pattern=[[1, 128]], base=0, channel_multiplier=0,
                                      allow_small_or_imprecise_dtypes=True), writes=[kvt])

        def ts(out, in0, s1, s2, op0, op1=None, eng='dve', r=(), w=()):
            if op1 is None:
                p.op(eng, lambda e: e.tensor_scalar(out=out, in0=in0, scalar1=s1, scalar2=None, op0=op0), reads=r, writes=w)
            else:
                p.op(eng, lambda e: e.tensor_scalar(out=out, in0=in0, scalar1=s1, scalar2=s2, op0=op0, op1=op1), reads=r, writes=w)

        def tt(out, in0, in1, op, eng='dve', r=(), w=()):
            p.op(eng, lambda e: e.tensor_tensor(out=out, in0=in0, in1=in1, op=op), reads=r, writes=w)

        def stt(out, in0, sc, in1, op0, op1, eng='dve', r=(), w=()):
            p.op(eng, lambda e: e.scalar_tensor_tensor(out=out, in0=in0, scalar=sc, in1=in1, op0=op0, op1=op1), reads=r, writes=w)

        def act(out, in_, func, bias=None, scale=1.0, accum=None, r=(), w=()):
            kw = {}
            if bias is not None:
                kw['bias'] = bias
            if accum is not None:
                kw['accum_out'] = accum
            p.op('act', lambda e: e.activation(out=out, in_=in_, func=func, scale=scale, **kw), reads=r, writes=w)

        def cp(out, in_, eng='dve', r=(), w=()):
            if eng == 'act':
                p.op('act', lambda e: e.copy(out=out, in_=in_), reads=r, writes=w)
            else:
                p.op(eng, lambda e: e.tensor_copy(out=out, in_=in_), reads=r, writes=w)

        def mm(out, lhsT, rhs, start=True, stop=True, r=(), w=()):
            p.op('pe', lambda e: e.matmul(out, lhsT=lhsT, rhs=rhs, start=start, stop=stop), reads=r, writes=w)

        def tr(out, in_, idn, r=(), w=()):
            p.op('pe', lambda e: e.transpose(out, in_, idn), reads=r, writes=w)

        ident, kid = PA.f32([128, 128])
        ts(ident, val, 0.0, None, ALU.is_equal, r=[kval], w=[kid])
        identb, kidb = PA.bf16([128, 128])
        cp(identb, ident, r=[kid], w=[kidb])
        c_tmp, kct = PA.f32([128, 128])
        c_tmp2, kct2 = PA.f32([128, 128])
        BM, kbm = PA.f32([128, 128])
        ts(c_tmp, vs, 64.0, None, ALU.is_ge, r=[kvs], w=[kct])
        ts(c_tmp2, vt, 64.0, None, ALU.is_ge, r=[kvt], w=[kct2])
        tt(BM, c_tmp, c_tmp2, ALU.is_equal, r=[kct, kct2], w=[kbm])
        ind, kind = PA.f32([128, 2])
        cp(ind[:, 1:2], c_tmp[:, 0:1], r=[kct], w=[kind])
        ts(ind[:, 0:1], c_tmp[:, 0:1], -1.0, 1.0, ALU.mult, ALU.add, r=[kct, kind], w=[kind])
        TI, TX, TIb_, = {}, {}, {}
        kTI, kTX = {}, {}
        for d_, (opi, opx) in enumerate([(ALU.is_ge, ALU.is_lt), (ALU.is_le, ALU.is_gt)]):
            TI[d_], kTI[d_] = PA.f32([128, 128])
            TX[d_], kTX[d_] = PA.f32([128, 128])
            ts(TI[d_], val, 0.0, None, opi, r=[kval], w=[kTI[d_]])
            tt(TI[d_], TI[d_], BM, ALU.mult, r=[kTI[d_], kbm], w=[kTI[d_]])
            ts(TX[d_], val, 0.0, None, opx, r=[kval], w=[kTX[d_]])
            tt(TX[d_], TX[d_], BM, ALU.mult, r=[kTX[d_], kbm], w=[kTX[d_]])
        TXF, kTXF = {}, {}
        for d_, opx in enumerate([ALU.is_lt, ALU.is_gt]):
            TXF[d_], kTXF[d_] = PA.f32([128, 128])
            ts(TXF[d_], val, 0.0, None, opx, r=[kval], w=[kTXF[d_]])
        fl, kfl = PA.f32([128, 32])
        p.dma(fl, flags, writes=[kfl])
        maskP, kmP = PA.bf16([128, 128])
        maskN, kmN = PA.bf16([128, 128])
        maskP0, kmP0 = PA.bf16([128, 128])
        maskN0, kmN0 = PA.bf16([128, 128])
        ts(maskP, val, 0.0, None, ALU.is_le, r=[kval], w=[kmP])
        ts(maskN, val, 0.0, None, ALU.is_ge, r=[kval], w=[kmN])
        ts(maskP0, val, 0.0, fl[:, 8:9], ALU.is_le, ALU.mult, r=[kval, kfl], w=[kmP0])
        ts(maskN0, val, 0.0, fl[:, 9:10], ALU.is_ge, ALU.mult, r=[kval, kfl], w=[kmN0])
        ones_f, kof = PA.f32([128, 128])
        ones_b, kob = PA.bf16([128, 128])
        p.op('dve', lambda e: e.memset(ones_f, 1.0), writes=[kof])
        p.op('dve', lambda e: e.memset(ones_b, 1.0), writes=[kob])
        PermT, kpm = PA.f32([128, 128])
        p.op('pool', lambda e: e.iota(c_tmp.rearrange("p (a b) -> p a b", a=2), pattern=[[0, 2], [1, 64]], base=0, channel_multiplier=0,
                                      allow_small_or_imprecise_dtypes=True), reads=[kct], writes=[kct])
        ts(c_tmp, c_tmp, 32.0, None, ALU.is_lt, r=[kct], w=[kct])
        ts(c_tmp2, val, -32.0, None, ALU.is_equal, r=[kval], w=[kct2])
        tt(PermT, c_tmp2, c_tmp, ALU.mult, r=[kct, kct2], w=[kpm])
        ts(c_tmp, c_tmp, -1.0, 1.0, ALU.mult, ALU.add, r=[kct], w=[kct])
        ts(c_tmp2, val, 32.0, None, ALU.is_equal, r=[kval], w=[kct2])
        tt(c_tmp2, c_tmp2, c_tmp, ALU.mult, r=[kct, kct2], w=[kct2])
        tt(PermT, PermT, c_tmp2, ALU.add, r=[kpm, kct2], w=[kpm])
        small, ksm = PA.f32([128, 64])
        sgn = small[:, 0:1]
        invf = small[:, 1:2]
        pcol = vs[:, 0:1]
        ts(small[:, 30:31], pcol, 32.0, None, ALU.is_lt, r=[kvs, ksm], w=[ksm])
        ts(small[:, 31:32], pcol, 64.0, None, ALU.is_ge, r=[kvs, ksm], w=[ksm])
        ts(small[:, 32:33], pcol, 96.0, None, ALU.is_lt, r=[kvs, ksm], w=[ksm])
        tt(small[:, 2:3], small[:, 31:32], small[:, 32:33], ALU.mult, r=[ksm], w=[ksm])
        tt(small[:, 2:3], small[:, 2:3], small[:, 30:31], ALU.add, r=[ksm], w=[ksm])
        ts(sgn, small[:, 2:3], -2.0, 1.0, ALU.mult, ALU.add, r=[ksm], w=[ksm])
        ts(small[:, 33:34], pcol, 32.0, None, ALU.is_ge, r=[kvs, ksm], w=[ksm])
        ts(small[:, 34:35], pcol, 96.0, None, ALU.is_ge, r=[kvs, ksm], w=[ksm])
        tt(small[:, 33:34], small[:, 33:34], small[:, 31:32], ALU.add, r=[ksm], w=[ksm])
        tt(small[:, 33:34], small[:, 33:34], small[:, 34:35], ALU.add, r=[ksm], w=[ksm])
        stt(small[:, 3:4], small[:, 33:34], -32.0, pcol, ALU.mult, ALU.add, r=[ksm, kvs], w=[ksm])
        act(invf, small[:, 3:4], AF.Exp, scale=-float(np.log(10000.0)) / 32.0, r=[ksm], w=[ksm])
        qg = small[:, 4:5]
        kg = small[:, 5:6]
        p.dma(qg, q_norm.rearrange("(p o) -> p o", o=1), reads=[ksm], writes=[ksm])
        p.dma(kg, k_norm.rearrange("(p o) -> p o", o=1), reads=[ksm], writes=[ksm])
        esink = small[:, 8:16]
        p.dma(esink, attn_sink.partition_broadcast(128), reads=[ksm], writes=[ksm])
        act(esink, esink, AF.Exp, r=[ksm], w=[ksm])
        ogain, kog = PA.f32([128, 128])
        p.dma(ogain, hgrn_norm.partition_broadcast(128), writes=[kog])
        cols, kcols = PA.f32([128, 8, KC])
        nrm, knrm = PA.f32([128, 2, KC])
        p.dma(nrm[:, 0, :], norm_mix.rearrange("(c p) -> p c", p=128), writes=[knrm], allow_slow_non_contiguous=True)
        p.dma(nrm[:, 1, :], norm_ffn.rearrange("(c p) -> p c", p=128), reads=[knrm], writes=[knrm], allow_slow_non_contiguous=True)
        S_all, kS = PA.f32([128, 16, 128])
        kSh = [[kS + '_%d_%d' % (d_, h) for h in range(8)] for d_ in range(2)]
        Sb_all, _ = PA.bf16([128, 16, 128])
        kSb = [[kS + 'b_%d_%d' % (d_, h) for h in range(8)] for d_ in range(2)]
        S_ctx, kSc = PA.f32([128, 16, 128])
        kcT_c, kkc = PA.bf16([128, 2, 256])
        v_c, kvc = PA.bf16([128, 2, 2, 128])
        hT, khT = PA.bf16([128, KC, 768])
        wbf, kwbf = [], []
        wbfF = []
        for i in range(2):
            a, k = PA.bf16([128, KC * 640])
            wbfF.append(a)
            wbf.append(a.rearrange("p (a b) -> p a b", a=KC))
            kwbf.append(k)
        wstF, _ = PA.f32([128, 2048])
        wst, kwst = [], []
        for i in range(4):
            wst.append(wstF[:, i * 512:(i + 1) * 512])
            kwst.append('wst%d' % i)
        XS, kXS = PA.f32([128, D])
        xn, kxn = PA.bf16([128, D])
        sstat, kss = PA.f32([128, 8])
        wl = [0]

        WB = {}

        def load_w(dst, kdst, wd, col0, ncols, dcol=0):
            src = WB[id(wd)].rearrange("(kc p) c -> p kc c", p=128)[:, :, col0:col0 + ncols]
            p.dma(dst[:, :, dcol:dcol + ncols], src, writes=[kdst])

        def norm_T(rows_ap, ci, dstT, kdst, c0):
            p.dma(XS, rows_ap, writes=[kXS])
            p.op('dve', lambda e: e.memset(sstat[:, 0:1], 0.0), reads=[kss], writes=[kss])
            act(xn, XS, AF.Square, accum=sstat[:, 0:1], r=[kXS, kss], w=[kxn, kss])
            act(sstat[:, 1:2], sstat[:, 0:1], AF.Ln, bias=small[:, 20:21], scale=1.0 / D, r=[kss, ksm], w=[kss])
            act(sstat[:, 2:3], sstat[:, 1:2], AF.Exp, scale=-0.5, r=[kss], w=[kss])
            act(xn, XS, AF.Identity, scale=sstat[:, 2:3], r=[kXS, kss], w=[kxn])
            for half in range(2):
                pb, kpb = bank_bf()
                for q in range(8):
                    kc = half * 8 + q
                    tr(pb[:, q * 128:(q + 1) * 128], xn[:, kc * 128:(kc + 1) * 128], identb, r=[kxn, kidb], w=[kpb])
                for q in range(8):
                    kc = half * 8 + q
                    if q % 2 == 0:
                        act(dstT[:, kc, c0:c0 + 128], pb[:, q * 128:(q + 1) * 128], AF.Identity,
                            bias=cols[:, ci + 1, kc:kc + 1], scale=cols[:, ci, kc:kc + 1], r=[kpb, kcols], w=[kdst])
                    else:
                        ts(dstT[:, kc, c0:c0 + 128], pb[:, q * 128:(q + 1) * 128], cols[:, ci, kc:kc + 1],
                           cols[:, ci + 1, kc:kc + 1], ALU.mult, ALU.add, r=[kpb, kcols], w=[kdst])

        p.op('dve', lambda e: e.memset(small[:, 20:21], EPS), reads=[ksm], writes=[ksm])
        p.op('dve', lambda e: e.memset(small[:, 21:22], -PI), reads=[ksm], writes=[ksm])
        p.op('dve', lambda e: e.memset(small[:, 22:23], 1.0), reads=[ksm], writes=[ksm])

        H = new_phase()
        cv, kcv = H.f32([128, KC, 2])
        p.dma(cv[:, :, 0], cvec[0].rearrange("(c p) -> p c", p=128), writes=[kcv], allow_slow_non_contiguous=True)
        p.dma(cv[:, :, 1], cvec[1].rearrange("(c p) -> p c", p=128), reads=[kcv], writes=[kcv], allow_slow_non_contiguous=True)
        act(cv, cv, AF.Silu, r=[kcv], w=[kcv])
        rep, krep = H.f32([128, 2, KC, 128])
        for v_ in range(2):
            cp(rep[:, v_], cv[:, :, v_].unsqueeze(2).to_broadcast([128, KC, 128]), r=[kcv], w=[krep])
        brow, kbrow = H.f32([128, 512])
        mrow, kmrow = H.f32([128, 512])
        for mi in range(CFG['mods']):
            for cg in range(4):
                col0 = mi * D + cg * 512
                pb0, kpb0 = bank()
                pb1, kpb1 = bank()
                for kc in range(KC):
                    i = wl[0] % 4
                    wl[0] += 1
                    p.dma(wst[i][:, 0:512], w_ada[kc * 128:(kc + 1) * 128, col0:col0 + 512], writes=[kwst[i]])
                    mm(pb0[:], rep[:, 0, kc], wst[i][:, 0:512], start=(kc == 0), stop=(kc == KC - 1), r=[krep, kwst[i]], w=[kpb0])
                    if mi < 2:
                        mm(pb1[:], rep[:, 1, kc], wst[i][:, 0:512], start=(kc == 0), stop=(kc == KC - 1), r=[krep, kwst[i]], w=[kpb1])
                p.dma(brow, b_ada[col0:col0 + 512].partition_broadcast(128), writes=[kbrow])
                tt(mrow, pb0[:], brow, ALU.add, r=[kpb0, kbrow], w=[kmrow])
                p.dma(modd[mi:mi + 1, cg * 512:(cg + 1) * 512], mrow[0:1, :], reads=[kmrow], writes=['modd'], key=kmrow)
                if mi < 2:
                    tt(mrow, pb1[:], brow, ALU.add, r=[kpb1, kbrow], w=[kmrow])
                    p.dma(modd[6 + mi:7 + mi, cg * 512:(cg + 1) * 512], mrow[0:1, :], reads=[kmrow], writes=['modd'], key=kmrow)
        mc, kmc = H.f32([128, 8, KC])
        for mi in range(8):
            p.dma(mc[:, mi, :], modd[mi].rearrange("(c p) -> p c", p=128), reads=['modd', kmc], writes=[kmc], allow_slow_non_contiguous=True)
        for (ci, sci, shi, ni) in [(0, 1, 0, 0), (2, 4, 3, 1), (4, 7, 6, 0)]:
            stt(cols[:, ci, :], mc[:, sci, :], 1.0, nrm[:, ni, :], ALU.add, ALU.mult, r=[kmc, knrm, kcols], w=[kcols])
            cp(cols[:, ci + 1, :], mc[:, shi, :], r=[kmc, kcols], w=[kcols])
        lt_, klt = H.f32([1, 2, 2, 1024])
        p.dma(lt_, lbl.rearrange("(o a) b c -> o a b c", o=1), writes=[klt])
        lo_, klo = H.f32([1, 2, 2, 1024])
        tt(lo_[:, 0], lt_[:, :, 0, :], lt_[:, :, 1, :], ALU.subtract, r=[klt], w=[klo])
        act(lo_[:, 0], lo_[:, 0], AF.Sigmoid, r=[klo], w=[klo])
        ts(lo_[:, 1], lo_[:, 0], -1.0, 1.0, ALU.mult, ALU.add, r=[klo], w=[klo])
        p.dma(lbd.rearrange("(o a) b c -> o a b c", o=1), lo_, reads=[klo], writes=['lbd'], key=klo)

        H = new_phase()
        stg = [H.f32([128, D]) for _ in range(4)]
        ubr = [H.bf16([128, D]) for _ in range(2)]
        uTr = [H.bf16([128, KC, 128]) for _ in range(2)]
        vbr = [H.bf16([128, D]) for _ in range(2)]
        WB[id(w_in)], WB[id(w_out)], WB[id(w_q)] = w_in_b, w_out_b, w_q_b
        wn = 0
        for (wsrc, wdst, ncol_) in [(w_in, w_in_b, NCOL), (w_out, w_out_b, D), (w_q, w_q_b, D)]:
            for kc in range(KC):
                for c0 in range(0, ncol_, 2048):
                    n = min(2048, ncol_ - c0)
                    su, ksu = stg[wn % 4]
                    ub_, kub_ = (ubr + vbr)[wn % 4]
                    p.dma(su[:, 0:n], wsrc[kc * 128:(kc + 1) * 128, c0:c0 + n], writes=[ksu])
                    cp(ub_[:, 0:n], su[:, 0:n], eng=['pool', 'dve', 'act'][wn % 3], r=[ksu], w=[kub_])
                    p.dma(wdst[kc * 128:(kc + 1) * 128, c0:c0 + n], ub_[:, 0:n], reads=[kub_], writes=['wb'], key=kub_)
                    wn += 1
        for ec in range(CFG['nec']):
            i = ec % 2
            su, ksu = stg[i]
            sv, ksv = stg[2 + i]
            p.dma(su, pu[ec * 128:(ec + 1) * 128, :], writes=[ksu])
            p.dma(sv, pv[ec * 128:(ec + 1) * 128, :], writes=[ksv])
            ub_, kub_ = ubr[i]
            cp(ub_, su, eng='pool', r=[ksu], w=[kub_])
            vb_, kvb_ = vbr[i]
            cp(vb_, sv, eng='dve', r=[ksv], w=[kvb_])
            p.dma(vd[ec], vb_, reads=[kvb_], writes=['vd'], key=kvb_)
            uT_, kuT_ = uTr[i]
            for half in range(2):
                pt, kpt = bank_bf()
                for q in range(8):
                    kc = half * 8 + q
                    tr(pt[:, q * 128:(q + 1) * 128], ub_[:, kc * 128:(kc + 1) * 128], identb, r=[kub_, kidb], w=[kpt])
                cp(uT_[:, half * 8:(half + 1) * 8, :], pt[:, 0:1024].rearrange("p (a b) -> p a b", a=8), eng='act', r=[kpt], w=[kuT_])
            p.dma(uTd[ec].rearrange("p (a b) -> p a b", a=KC), uT_, reads=[kuT_], writes=['uTd'], key=kuT_)

        def qk_norm_rope(ps_ap, kps, n, gain, dst, kdst, A, cosv=None, sinv=None, ktab=None):
            sq, ksq = A['sq']
            kn_, kkn = A['kn']
            act(sq[:, 0:n], ps_ap, AF.Square, r=[kps], w=[ksq])
            pb, kpb = bank()
            mm(pb[:, 0:n], ones_f, sq[:, 0:n], r=[kof, ksq], w=[kpb])
            act(sq[:, 0:n], pb[:, 0:n], AF.Ln, bias=small[:, 20:21], scale=1.0 / 128, r=[kpb, ksm], w=[ksq])
            act(sq[:, 0:n], sq[:, 0:n], AF.Exp, scale=-0.5, r=[ksq], w=[ksq])
            if cosv is None:
                stt(dst, ps_ap, gain, sq[:, 0:n], ALU.mult, ALU.mult, r=[kps, ksq, ksm], w=[kdst])
                return
            stt(kn_[:, 0:n], ps_ap, gain, sq[:, 0:n], ALU.mult, ALU.mult, r=[kps, ksq, ksm], w=[kkn])
            pb2, kpb2 = bank()
            mm(pb2[:, 0:n], PermT, kn_[:, 0:n], r=[kpm, kkn], w=[kpb2])
            tt(sq[:, 0:n], pb2[:, 0:n], sinv, ALU.mult, r=[kpb2, ktab, ksq], w=[ksq])
            tt(kn_[:, 0:n], kn_[:, 0:n], cosv, ALU.mult, eng='pool', r=[kkn, ktab], w=[kkn])
            tt(dst, kn_[:, 0:n], sq[:, 0:n], ALU.add, r=[kkn, ksq], w=[kdst])

        def proj_fm(dstps, kps, wv, kw, wc0, src, ksrc, t0, n):
            for kc in range(KC):
                mm(dstps, wv[:, kc, wc0:wc0 + 128], src[:, kc, t0:t0 + n], start=(kc == 0), stop=(kc == KC - 1),
                   r=[kw, ksrc], w=[kps])

        def proj_tm(dstps, kps, wv, kw, wc0, ncols, src, ksrc, t0):
            for kc in range(KC):
                mm(dstps, src[:, kc, t0:t0 + 128], wv[:, kc, wc0:wc0 + ncols], start=(kc == 0), stop=(kc == KC - 1),
                   r=[kw, ksrc], w=[kps])

        def load_lba(H):
            lba, klba = H.f32([128, 2, 2, 1024])
            for a_ in range(2):
                for d_ in range(2):
                    p.dma(lba[:, a_, d_, :], lbd[a_, d_, :].partition_broadcast(128), reads=['lbd', klba], writes=[klba])
            return lba, klba

        def gate_math(sg, ksg, lb_b, oml_b, klba):
            act(sg, sg, AF.Ln, bias=small[:, 22:23], scale=1.0, r=[ksg, ksm], w=[ksg])
            act(sg, sg, AF.Exp, scale=-1.0, r=[ksg], w=[ksg])
            tt(sg, sg, oml_b, ALU.mult, r=[ksg, klba], w=[ksg])
            tt(sg, sg, lb_b, ALU.add, r=[ksg, klba], w=[ksg])

        def state_step(d_, h, lf, kk, iv, kin, A):
            S = S_all[:, d_ * 8 + h, :]
            pbm, kpbm = bank()
            mm(pbm[:, 0:128], TX[d_], lf, r=[kTX[d_]] + kin, w=[kpbm])
            mm(pbm[:, 128:130], lf, ind, r=[kind] + kin, w=[kpbm])
            Eb, kEb = A['Eb']
            ee, kee = A['ee']
            act(Eb, pbm[:, 0:128], AF.Exp, r=[kpbm], w=[kEb])
            act(ee, pbm[:, 128:130], AF.Exp, r=[kpbm], w=[kee])
            for c in ([0, 1] if d_ == 0 else [1, 0]):
                khz, kkhz = A['khz'][c]
                stt(khz, kk, ind[:, c:c + 1], Eb, ALU.mult, ALU.mult, r=[kEb, kind] + kin, w=[kkhz])
                pd, kpd = bank()
                mm(pd[:, 0:128], khz, iv, r=[kkhz] + kin, w=[kpd])
                stt(S, S, ee[:, c:c + 1], pd[:, 0:128], ALU.mult, ALU.add, r=[kSh[d_][h], kee, kpd], w=[kSh[d_][h]])

        def full_step(d_, h, lf, kk, iv, qTt, kin, ops_, kops, A):
            S = S_all[:, d_ * 8 + h, :]
            Sb = Sb_all[:, d_ * 8 + h, :]
            pa, kpa = bank()
            mm(pa[:, 0:128], lf, TI[d_], r=[kTI[d_]] + kin, w=[kpa])
            pbm, kpbm = bank()
            mm(pbm[:, 0:128], TX[d_], lf, r=[kTX[d_]] + kin, w=[kpbm])
            aTs, kaT = A['aTs']
            cp(aTs, pa[:, 0:128], eng='act', r=[kpa], w=[kaT])
            refc = [31, 95] if d_ == 0 else [32, 96]
            endc = [63, 127] if d_ == 0 else [0, 64]
            negr, knr = A['negr']
            Eq, kEq = A['Eq']
            Ek, kEk = A['Ek']
            Ea, kEa = A['Ea']
            ee, kee = A['ee']
            for c in range(2):
                ts(negr[:, c:c + 1], aTs[:, refc[c]:refc[c] + 1], -1.0, None, ALU.mult, r=[kaT, knr], w=[knr])
            for c in range(2):
                sl = slice(64 * c, 64 * c + 64)
                act(Eq[:, sl], pa[:, sl], AF.Exp, bias=negr[:, c:c + 1], scale=1.0, r=[kpa, knr, kEq], w=[kEq])
                act(Ek[:, sl], pa[:, sl], AF.Exp, bias=aTs[:, refc[c]:refc[c] + 1], scale=-1.0, r=[kpa, kaT, kEk], w=[kEk])
                act(ee[:, c:c + 1], aTs[:, endc[c]:endc[c] + 1], AF.Exp, r=[kaT, kee], w=[kee])
            act(Ea, pa[:, 0:128], AF.Exp, r=[kpa], w=[kEa])
            Eb, kEb = A['Eb']
            act(Eb, pbm[:, 0:128], AF.Exp, r=[kpbm], w=[kEb])
            pt, kpt = bank_bf()
            tr(pt[:, 0:128], kk, identb, r=[kidb] + kin, w=[kpt])
            ktT, kktT = A['ktT']
            qtT, kqtT = A['qtT']
            qhT, kqhT = A['qhT']
            tt(ktT, pt[:, 0:128], Ek, ALU.mult, r=[kpt, kEk], w=[kktT])
            tt(qtT, qTt, Eq, ALU.mult, eng='pool', r=[kEq] + kin, w=[kqtT])
            tt(qhT, qTt, Ea, ALU.mult, eng='pool', r=[kEa] + kin, w=[kqhT])
            psc, kpsc = bank()
            mm(psc[:, 0:128], ktT, qtT, r=[kktT, kqtT], w=[kpsc])
            scT, kscT = A['scT']
            tt(scT, psc[:, 0:128], TI[d_], ALU.mult, r=[kpsc, kTI[d_]], w=[kscT])
            for c in range(2):
                khz, kkhz = A['khz'][c]
                stt(khz, kk, ind[:, c:c + 1], Eb, ALU.mult, ALU.mult, r=[kEb, kind] + kin, w=[kkhz])
            mm(ops_, scT, iv, start=True, stop=False, r=[kscT] + kin, w=[kops])
            order = [0, 1] if d_ == 0 else [1, 0]
            for n_, c in enumerate(order):
                mm(ops_[64 * c:64 * c + 64, :], qhT[:, 64 * c:64 * c + 64], Sb, start=False, stop=True,
                   r=[kqhT, kSb[d_][h]], w=[kops])
                khz, kkhz = A['khz'][c]
                pd, kpd = bank()
                mm(pd[:, 0:128], khz, iv, r=[kkhz] + kin, w=[kpd])
                stt(S, S, ee[:, c:c + 1], pd[:, 0:128], ALU.mult, ALU.add, r=[kSh[d_][h], kee, kpd], w=[kSh[d_][h]])
                cp(Sb, S, eng='act', r=[kSh[d_][h]], w=[kSb[d_][h]])

        def step_front(d_, h, lf, kk, iv, qTt, kin, A):
            pa, kpa = bank()
            mm(pa[:, 0:128], lf, TI[d_], r=[kTI[d_]] + kin, w=[kpa])
            pbm, kpbm = bank()
            mm(pbm[:, 0:128], TX[d_], lf, r=[kTX[d_]] + kin, w=[kpbm])
            aTs, kaT = A['aTs']
            cp(aTs, pa[:, 0:128], eng='act', r=[kpa], w=[kaT])
            refc = [31, 95] if d_ == 0 else [32, 96]
            endc = [63, 127] if d_ == 0 else [0, 64]
            negr, knr = A['negr']
            Eq, kEq = A['Eq']
            Ek, kEk = A['Ek']
            Ea, kEa = A['Ea']
            ee, kee = A['ee']
            for c in range(2):
                ts(negr[:, c:c + 1], aTs[:, refc[c]:refc[c] + 1], -1.0, None, ALU.mult, r=[kaT, knr], w=[knr])
            for c in range(2):
                sl = slice(64 * c, 64 * c + 64)
                act(Eq[:, sl], pa[:, sl], AF.Exp, bias=negr[:, c:c + 1], scale=1.0, r=[kpa, knr, kEq], w=[kEq])
                act(Ek[:, sl], pa[:, sl], AF.Exp, bias=aTs[:, refc[c]:refc[c] + 1], scale=-1.0, r=[kpa, kaT, kEk], w=[kEk])
                act(ee[:, c:c + 1], aTs[:, endc[c]:endc[c] + 1], AF.Exp, r=[kaT, kee], w=[kee])
            act(Ea, pa[:, 0:128], AF.Exp, r=[kpa], w=[kEa])
            Eb, kEb = A['Eb']
            act(Eb, pbm[:, 0:128], AF.Exp, r=[kpbm], w=[kEb])
            pt, kpt = bank_bf()
            tr(pt[:, 0:128], kk, identb, r=[kidb] + kin, w=[kpt])
            ktT, kktT = A['ktT']
            qtT, kqtT = A['qtT']
            qhT, kqhT = A['qhT']
            tt(ktT, pt[:, 0:128], Ek, ALU.mult, r=[kpt, kEk], w=[kktT])
            tt(qtT, qTt, Eq, ALU.mult, eng='pool', r=[kEq] + kin, w=[kqtT])
            tt(qhT, qTt, Ea, ALU.mult, eng='pool', r=[kEa] + kin, w=[kqhT])
            psc, kpsc = bank()
            mm(psc[:, 0:128], ktT, qtT, r=[kktT, kqtT], w=[kpsc])
            scT, kscT = A['scT']
            tt(scT, psc[:, 0:128], TI[d_], ALU.mult, r=[kpsc, kTI[d_]], w=[kscT])
            for c in range(2):
                khz, kkhz = A['khz'][c]
                stt(khz, kk, ind[:, c:c + 1], Eb, ALU.mult, ALU.mult, r=[kEb, kind] + kin, w=[kkhz])

        def step_back(d_, h, iv, kin, ops_, kops, A):
            S = S_all[:, d_ * 8 + h, :]
            Sb = Sb_all[:, d_ * 8 + h, :]
            scT, kscT = A['scT']
            qhT, kqhT = A['qhT']
            ee, kee = A['ee']
            mm(ops_, scT, iv, start=True, stop=False, r=[kscT] + kin, w=[kops])
            order = [0, 1] if d_ == 0 else [1, 0]
            for n_, c in enumerate(order):
                mm(ops_[64 * c:64 * c + 64, :], qhT[:, 64 * c:64 * c + 64], Sb, start=False, stop=True,
                   r=[kqhT, kSb[d_][h]], w=[kops])
                khz, kkhz = A['khz'][c]
                pd, kpd = bank()
                mm(pd[:, 0:128], khz, iv, r=[kkhz] + kin, w=[kpd])
                stt(S, S, ee[:, c:c + 1], pd[:, 0:128], ALU.mult, ALU.add, r=[kSh[d_][h], kee, kpd], w=[kSh[d_][h]])
                cp(Sb, S, eng='act', r=[kSh[d_][h]], w=[kSb[d_][h]])

        def step_scratch(H):
            A = {}
            A['Eb'] = H.f32([128, 128])
            A['ee'] = H.f32([128, 2])
            A['khz'] = [H.bf16([128, 128]), H.bf16([128, 128])]
            A['aTs'] = H.f32([128, 128])
            A['negr'] = H.f32([128, 2])
            A['Eq'] = H.f32([128, 128])
            A['Ek'] = H.f32([128, 128])
            A['Ea'] = H.f32([128, 128])
            A['ktT'] = H.bf16([128, 128])
            A['qtT'] = H.bf16([128, 128])
            A['qhT'] = H.bf16([128, 128])
            A['scT'] = H.bf16([128, 128])
            return A

        def hg_scratch(H):
            A = {}
            A['sg'] = H.f32([128, 128])
            A['Eb'] = H.f32([128, 128])
            A['ee'] = H.f32([128, 2])
            A['khz'] = [H.bf16([128, 128]), H.bf16([128, 128])]
            A['aTs'] = H.f32([128, 128])
            A['negr'] = H.f32([128, 2])
            A['Eq'] = H.f32([128, 128])
            A['Ek'] = H.f32([128, 128])
            A['Ea'] = H.f32([128, 128])
            A['ktT'] = H.bf16([128, 128])
            A['qtT'] = H.bf16([128, 128])
            A['qhT'] = H.bf16([128, 128])
            A['scT'] = H.bf16([128, 128])
            A['sq'] = H.f32([128, 512])
            A['kn'] = H.f32([128, 512])
            return A

        def load_lbh(lbh, klbh, h):
            for a in range(2):
                for d_ in range(2):
                    p.dma(lbh[:, a, d_, :], lbd[a, d_, h * 128:(h + 1) * 128].partition_broadcast(128),
                          reads=['lbd', klbh], writes=[klbh])

        def state_heads(d_, ntiles, A):
            lba, klba = A['lba']
            n_ = ntiles

            def quad_front(q):
                lfq, klfq = A['lfq'][q % 2]
                kkq, kkkq = A['kkq'][q % 2]
                ivq, kivq = A['ivq'][q % 2]
                sgq, ksgq = A['sgq'][q % 2]
                load_w(wbf[0], kwbf[0], w_in, (C_FF if d_ == 0 else C_FB) + q * 512, 512, 0)
                load_w(wbf[1], kwbf[1], w_in, C_I + q * 512, 512, 0)
                for t in range(ntiles):
                    pf, kpf = bank()
                    pi_, kpi = bank()
                    for kc in range(KC):
                        mm(pf[:], hT[:, kc, t * 128:(t + 1) * 128], wbf[0][:, kc, 0:512], start=(kc == 0), stop=(kc == KC - 1),
                           r=[kwbf[0], khT], w=[kpf])
                        mm(pi_[:], hT[:, kc, t * 128:(t + 1) * 128], wbf[1][:, kc, 0:512], start=(kc == 0), stop=(kc == KC - 1),
                           r=[kwbf[1], khT], w=[kpi])
                    act(sgq[:, t, :], pf[:], AF.Exp, scale=-1.0, r=[kpf, ksgq], w=[ksgq])
                    cp(ivq[:, t, :], pi_[:], r=[kpi, kivq], w=[kivq])
                sgv = sgq[:, 0:n_, :]
                qs = slice(q * 512, (q + 1) * 512)
                gate_math(sgv, ksgq, lba[:, 0, d_, qs].unsqueeze(1).to_broadcast([128, n_, 512]),
                          lba[:, 1, d_, qs].unsqueeze(1).to_broadcast([128, n_, 512]), klba)
                act(lfq[:, 0:n_, :], sgv, AF.Ln, r=[ksgq], w=[klfq])
                ts(kkq[:, 0:n_, :], sgv, -1.0, 1.0, ALU.mult, ALU.add, r=[ksgq], w=[kkkq])

            def head_tail(h):
                q, hq = h // 4, h % 4
                S = S_all[:, d_ * 8 + h, :]
                lfq, klfb = A['lfq'][q % 2]
                kkq, kkkb = A['kkq'][q % 2]
                ivq, kivb = A['ivq'][q % 2]
                hs = slice(hq * 128, (hq + 1) * 128)
                pa, kpa = bank()
                for t in range(ntiles):
                    mm(pa[:, 0:1], lfq[:, t, hs], ones_f[:, 0:1], start=(t == 0), stop=(t == ntiles - 1),
                       r=[klfb, kof], w=[kpa])
                ee, kee = A['ee2'][h % 2]
                act(ee[:, 0:1], pa[:, 0:1], AF.Exp, r=[kpa], w=[kee])
                pbs_ = []
                for t in range(ntiles):
                    others = [t2 for t2 in range(ntiles) if (t2 > t if d_ == 0 else t2 < t)]
                    pb, kpb = bank()
                    pbs_.append((pb, kpb))
                    mm(pb[:, 0:128], TXF[d_], lfq[:, t, hs], start=True, stop=(len(others) == 0),
                       r=[kTXF[d_], klfb], w=[kpb])
                    for q_, t2 in enumerate(others):
                        mm(pb[:, 0:128], ones_f, lfq[:, t2, hs], start=False, stop=(q_ == len(others) - 1),
                           r=[kof, klfb], w=[kpb])
                pd, kpd = bank()
                for t in range(ntiles):
                    pb, kpb = pbs_[t]
                    Eb, kEb = A['Ebr'][t % 2]
                    khz, kkhz = A['khz4'][t % 4]
                    act(Eb, pb[:, 0:128], AF.Exp, r=[kpb], w=[kEb])
                    tt(khz, kkq[:, t, hs], Eb, ALU.mult, r=[kEb, kkkb], w=[kkhz])
                for t in range(ntiles):
                    khz, kkhz = A['khz4'][t % 4]
                    mm(pd[:, 0:128], khz, ivq[:, t, hs], start=(t == 0), stop=(t == ntiles - 1), r=[kkhz, kivb], w=[kpd])
                stt(S, S, ee[:, 0:1], pd[:, 0:128], ALU.mult, ALU.add, r=[kSh[d_][h], kee, kpd], w=[kSh[d_][h]])

            quad_front(0)
            quad_front(1)
            for h in range(8):
                head_tail(h)

        def state_pass(d_, row0, ntiles, src_d, ci, H, A):
            for t in range(ntiles):
                norm_T(src_d[row0 + t * 128: row0 + (t + 1) * 128, :], ci, hT, khT, t * 128)
            state_heads(d_, ntiles, A)

        def sp_scratch(H, ntiles):
            A = hg_scratch(H)
            A['lba'] = load_lba(H)
            A['sgw'] = H.f32([128, ntiles, 128])
            A['lf'] = H.f32([128, ntiles, 128])
            A['kk'] = H.bf16([128, ntiles, 128])
            A['iv'] = H.bf16([128, ntiles, 128])
            A['Ebr'] = [A['Eb'], H.f32([128, 128])]
            A['lf2'] = [A['lf'], H.f32([128, ntiles, 128])]
            A['kk2'] = [A['kk'], H.bf16([128, ntiles, 128])]
            A['iv2'] = [A['iv'], H.bf16([128, ntiles, 128])]
            A['sg2'] = [A['sgw'], H.f32([128, ntiles, 128])]
            A['khz4'] = A['khz'] + [H.bf16([128, 128]), H.bf16([128, 128])]
            A['lfq'] = [H.f32([128, ntiles, 512]) for _ in range(2)]
            A['kkq'] = [H.bf16([128, ntiles, 512]) for _ in range(2)]
            A['ivq'] = [H.bf16([128, ntiles, 512]) for _ in range(2)]
            A['sgq'] = [H.f32([128, ntiles, 512]) for _ in range(2)]
            A['ee2'] = [A['ee'], H.f32([128, 2])]
            return A

        H = new_phase()
        A = sp_scratch(H, 2)
        for t in range(2):
            norm_T(ctxb[t * 128:(t + 1) * 128, :], 4, hT, khT, t * 128)
        for kvh in range(2):
            load_w(wbf[0], kwbf[0], w_in, C_K + kvh * 128, 128, 0)
            load_w(wbf[0], kwbf[0], w_in, C_V + kvh * 128, 128, 128)
            pb, kpb = bank()
            proj_fm(pb[:, 0:256], kpb, wbf[0], kwbf[0], 0, hT, khT, 0, 256)
            qk_norm_rope(pb[:, 0:256], kpb, 256, kg, kcT_c[:, kvh, :], kkc, A)
            for t in range(2):
                pb, kpb = bank()
                proj_tm(pb[:, 0:128], kpb, wbf[0], kwbf[0], 128, 128, hT, khT, t * 128)
                cp(v_c[:, kvh, t, :], pb[:, 0:128], eng='act', r=[kpb], w=[kvc])
        p.op('dve', lambda e: e.memset(S_all, 0.0), reads=[k for kk_ in kSh for k in kk_], writes=[k for kk_ in kSh for k in kk_])
        for d_ in range(2):
            state_heads(d_, 2, A)
        allS = [k for kk_ in kSh for k in kk_]
        cp(S_ctx, S_all, r=allS, w=[kSc])

        def reset_S(d_, fcol):
            for h in range(8):
                S = S_all[:, d_ * 8 + h, :]
                tt(S, S, S_ctx[:, d_ * 8 + h, :], ALU.subtract, r=[kSh[d_][h], kSc], w=[kSh[d_][h]])
                stt(S, S, fl[:, fcol:fcol + 1], S_ctx[:, d_ * 8 + h, :], ALU.mult, ALU.add,
                    r=[kSh[d_][h], kSc, kfl], w=[kSh[d_][h]])

        for m in range(CFG['nslot']):
            H = new_phase()
            A = sp_scratch(H, SGT)
            reset_S(0, 16 + m)
            for sg_ in range(CFG['nsub']):
                state_pass(0, (m + 1) * TOK + sg_ * SGT * 128, SGT, xr, 0, H, A)
        reset_S(0, 16 + 3)
        for m in range(CFG['nslot']):
            H = new_phase()
            A = sp_scratch(H, SGT)
            reset_S(1, 20 + m)
            for sg_ in range(CFG['nsub'] - 1, -1, -1):
                state_pass(1, (3 - m) * TOK + sg_ * SGT * 128, SGT, xr, 0, H, A)
        reset_S(1, 20 + 3)
        H = new_phase()
        A = sp_scratch(H, NT)
        kbS = [k for k in kSh[1]]
        for g in range(NG - 1, -1, -1):
            p.dma(ssave[g].rearrange("p (h v) -> p h v", h=8), S_all[:, 8:16, :], reads=kbS, writes=['ssave%d' % g], key='ssv')
            if g > 0 and g < CFG['ng']:
                state_pass(1, g * G, NT, xr, 0, H, A)

        for g in range(CFG['ng']):
            H = new_phase()
            A = hg_scratch(H)
            catT, kcat = H.bf16([128, KC, G])
            p.dma(S_all[:, 8:16, :], ssave[g].rearrange("p (h v) -> p h v", h=8), reads=['ssave%d' % g], writes=kbS, key='ssl')
            for h in range(8):
                cp(Sb_all[:, h, :], S_all[:, h, :], eng='act', r=[kSh[0][h]], w=[kSb[0][h]])
                cp(Sb_all[:, 8 + h, :], S_all[:, 8 + h, :], eng='act', r=[kSh[1][h]], w=[kSb[1][h]])
            for t in range(EXT):
                r0 = (g * G - 128 + t * 128) % SEQ
                norm_T(xr[r0:r0 + 128, :], 0, hT, khT, t * 128)
            NX = EXT * 128
            cosT, kcos = H.f32([128, NX])
            sinT, ksin = H.f32([128, NX])
            rope_mark = H.off
            pos, kpos = H.f32([128, NX])
            nrow = NX // 64
            p.op('pool', lambda e, pos=pos, g=g: e.iota(pos[0:64, :].rearrange("p (a b) -> p a b", b=64), pattern=[[1, nrow], [0, 64]],
                                                    base=8 * g - 2, channel_multiplier=0, allow_small_or_imprecise_dtypes=True),
                 writes=[kpos])
            p.op('pool', lambda e, pos=pos: e.iota(pos[64:128, :].rearrange("p (a b) -> p a b", b=64), pattern=[[0, nrow], [1, 64]],
                                               base=0, channel_multiplier=0, allow_small_or_imprecise_dtypes=True),
                 reads=[kpos], writes=[kpos])
            ts(pos[0:64, :], pos[0:64, :], fl[0:64, 10:11], None, ALU.add, r=[kpos, kfl], w=[kpos])
            ts(pos, pos, invf, None, ALU.mult, r=[kpos, ksm], w=[kpos])
            tA, ktA = H.f32([128, NX])
            tB, ktB = H.f32([128, NX])
            tK, ktK = H.f32([128, NX])
            tKi = tK.bitcast(mybir.dt.int32)

            def sin_table(dst, kdst, shift):
                ts(tB, pos, shift, None, ALU.add, r=[kpos], w=[ktB])
                ts(tA, tB, 1.0 / (2 * PI), None, ALU.mult, r=[ktB], w=[ktA])
                cp(tKi, tA, r=[ktA], w=[ktK])
                cp(tA, tKi, r=[ktK], w=[ktA])
                stt(tB, tA, -2 * PI, tB, ALU.mult, ALU.add, r=[ktA, ktB], w=[ktB])
                ts(tA, tB, PI, None, ALU.is_gt, r=[ktB], w=[ktA])
                stt(tB, tA, -2 * PI, tB, ALU.mult, ALU.add, r=[ktA, ktB], w=[ktB])
                ts(tA, tB, -PI, None, ALU.is_lt, r=[ktB], w=[ktA])
                stt(tB, tA, 2 * PI, tB, ALU.mult, ALU.add, r=[ktA, ktB], w=[ktB])
                act(dst, tB, AF.Sin, r=[ktB], w=[kdst])
            sin_table(sinT, ksin, 0.0)
            ts(sinT, sinT, sgn, None, ALU.mult, r=[ksin, ksm], w=[ksin])
            sin_table(cosT, kcos, 0.5 * PI)
            ktab = kcos
            H.off = rope_mark
            p.barrier()
            kTn, kkTn = H.bf16([128, NX])
            Vt, kVt = H.bf16([128, EXT, 128])
            qTn, kqTn = H.bf16([128, G])
            PT, kPT = H.bf16([128, 5, 128])
            rden, krd = H.f32([128, 128])
            for kvh in range(2):
                load_w(wbf[0], kwbf[0], w_in, C_K + kvh * 128, 128, 0)
                load_w(wbf[0], kwbf[0], w_in, C_V + kvh * 128, 128, 128)
                for t0 in range(0, NX, 512):
                    n = min(512, NX - t0)
                    pb, kpb = bank()
                    proj_fm(pb[:, 0:n], kpb, wbf[0], kwbf[0], 0, hT, khT, t0, n)
                    qk_norm_rope(pb[:, 0:n], kpb, n, kg, kTn[:, t0:t0 + n], kkTn, A, cosT[:, t0:t0 + n], sinT[:, t0:t0 + n], ktab)
                for t in range(EXT):
                    pb, kpb = bank()
                    proj_tm(pb[:, 0:128], kpb, wbf[0], kwbf[0], 128, 128, hT, khT, t * 128)
                    cp(Vt[:, t, :], pb[:, 0:128], eng='act', r=[kpb], w=[kVt])
                for hq in range(4):
                    hh = kvh * 4 + hq
                    load_w(wbf[1], kwbf[1], w_in, C_QA + hh * 128, 128, 0)
                    pb, kpb = bank()
                    proj_fm(pb[:, 0:G], kpb, wbf[1], kwbf[1], 0, hT, khT, 128, G)
                    qk_norm_rope(pb[:, 0:G], kpb, G, qg, qTn, kqTn, A, cosT[:, 128:128 + G], sinT[:, 128:128 + G], ktab)
                    for qt in range(NT):
                        ps0, kps0 = bank()
                        ps1, kps1 = bank()
                        qs = qTn[:, qt * 128:(qt + 1) * 128]
                        for kb in range(3):
                            mm(ps0[:, kb * 128:(kb + 1) * 128], kTn[:, (qt + kb) * 128:(qt + kb + 1) * 128], qs,
                               r=[kkTn, kqTn], w=[kps0])
                        for cb in range(2):
                            mm(ps1[:, cb * 128:(cb + 1) * 128], kcT_c[:, kvh, cb * 128:(cb + 1) * 128], qs,
                               r=[kkc, kqTn], w=[kps1])
                        sc = float(128 ** -0.5)
                        act(PT[:, 0:3, :], ps0[:, 0:384].rearrange("p (a b) -> p a b", a=3), AF.Exp, scale=sc, r=[kps0], w=[kPT])
                        act(PT[:, 3:5, :], ps1[:, 0:256].rearrange("p (a b) -> p a b", a=2), AF.Exp, scale=sc, r=[kps1, kPT], w=[kPT])
                        mP = maskP0 if (g == 0 and qt == 0) else maskP
                        mN = maskN0 if (g == NG - 1 and qt == NT - 1) else maskN
                        tt(PT[:, 0, :], PT[:, 0, :], mP, ALU.mult, r=[kPT, kmP, kmP0], w=[kPT])
                        tt(PT[:, 2, :], PT[:, 2, :], mN, ALU.mult, eng='pool', r=[kPT, kmN, kmN0], w=[kPT])
                        po, kpo = bank()
                        for bi in range(5):
                            vv = Vt[:, qt + bi, :] if bi < 3 else v_c[:, kvh, bi - 3, :]
                            mm(po[:, 0:128], vv, PT[:, bi, :], start=(bi == 0), stop=(bi == 4), r=[kVt, kvc, kPT], w=[kpo])
                        for bi in range(5):
                            mm(po[:, 128:256], ones_b, PT[:, bi, :], start=(bi == 0), stop=(bi == 4), r=[kob, kPT], w=[kpo])
                        act(rden, po[:, 128:256], AF.Ln, bias=esink[:, hh:hh + 1], scale=1.0, r=[kpo, ksm], w=[krd])
                        act(rden, rden, AF.Exp, scale=-1.0, r=[krd], w=[krd])
                        tt(catT[:, hh, qt * 128:(qt + 1) * 128], po[:, 0:128], rden, ALU.mult, r=[kpo, krd], w=[kcat])
            lba, klba = load_lba(H)
            SA = [A, step_scratch(H)]
            lfb, klfb = H.f32([128, 2, NT, 128])
            kkb, kkkb = H.bf16([128, 2, NT, 128])
            sgw, ksgw = H.f32([128, 2, NT, 128])
            ivb, kivb = H.bf16([128, NT, 128])
            sgb, ksgb = H.f32([128, NT, 128])
            qTh, kqTh = H.bf16([128, G])
            ob, kob_ = H.f32([128, NT, 128])
            ycat, kyc = H.bf16([128, 128])
            osb, kosb = H.f32([128, 128])
            for h in range(8):
                w_ = wbf[h % 2]
                kw_ = kwbf[h % 2]
                for bi, c0 in enumerate([C_FF, C_FB, C_I, C_GG, C_QH]):
                    load_w(w_, kw_, w_in, c0 + h * 128, 128, bi * 128)
                pbs = [bank() for _ in range(4)]
                for t in range(NT):
                    for kc in range(KC):
                        for bi in range(4):
                            mm(pbs[bi][0][:, t * 128:(t + 1) * 128], hT[:, kc, (t + 1) * 128:(t + 2) * 128], w_[:, kc, bi * 128:(bi + 1) * 128],
                               start=(kc == 0), stop=(kc == KC - 1), r=[kw_, khT], w=[pbs[bi][1]])
                for d_ in range(2):
                    act(sgw[:, d_], pbs[d_][0][:, 0:G].rearrange("p (a b) -> p a b", a=NT), AF.Exp, scale=-1.0, r=[pbs[d_][1], ksgw], w=[ksgw])
                hs = slice(h * 128, (h + 1) * 128)
                gate_math(sgw, ksgw, lba[:, 0, :, hs].unsqueeze(2).to_broadcast([128, 2, NT, 128]),
                          lba[:, 1, :, hs].unsqueeze(2).to_broadcast([128, 2, NT, 128]), klba)
                act(lfb, sgw, AF.Ln, r=[ksgw], w=[klfb])
                ts(kkb, sgw, -1.0, 1.0, ALU.mult, ALU.add, r=[ksgw], w=[kkkb])
                cp(ivb, pbs[2][0][:, 0:G].rearrange("p (a b) -> p a b", a=NT), r=[pbs[2][1]], w=[kivb])
                act(sgb, pbs[3][0][:, 0:G].rearrange("p (a b) -> p a b", a=NT), AF.Exp, scale=-1.0, r=[pbs[3][1]], w=[ksgb])
                act(sgb, sgb, AF.Ln, bias=small[:, 22:23], scale=1.0, r=[ksgb, ksm], w=[ksgb])
                act(sgb, sgb, AF.Exp, scale=-1.0, r=[ksgb], w=[ksgb])
                tt(sgb, sgb, pbs[3][0][:, 0:G].rearrange("p (a b) -> p a b", a=NT), ALU.mult, r=[ksgb, pbs[3][1]], w=[ksgb])
                pq, kpq = bank()
                proj_fm(pq[:, 0:G], kpq, w_, kw_, 512, hT, khT, 128, G)
                cp(qTh, pq[:, 0:G], eng='act', r=[kpq], w=[kqTh])
                hsteps = [(1, t) for t in range(NT - 1, -1, -1)] + [(0, t) for t in range(NT)]
                kin_h = [klfb, kkkb, kivb, kqTh]

                def h_front(i, h=h):
                    d_, t = hsteps[i]
                    step_front(d_, h, lfb[:, d_, t, :], kkb[:, d_, t, :], ivb[:, t, :], qTh[:, t * 128:(t + 1) * 128], kin_h, SA[i % 2])

                def h_back(i, h=h):
                    d_, t = hsteps[i]
                    po, kpo = bank()
                    step_back(d_, h, ivb[:, t, :], kin_h, po[:, 0:128], kpo, SA[i % 2])
                    if d_ == 1:
                        cp(ob[:, t, :], po[:, 0:128], r=[kpo], w=[kob_ + str(t)])
                        return
                    tt(osb, po[:, 0:128], ob[:, t, :], ALU.add, r=[kpo, kob_ + str(t)], w=[kosb])
                    p.op('dve', lambda e: e.memset(sstat[:, 4:5], 0.0), reads=[kss], writes=[kss])
                    sq, ksq = A['sq']
                    act(sq[:, 0:128], osb, AF.Square, accum=sstat[:, 4:5], r=[kosb, kss], w=[ksq, kss])
                    act(sstat[:, 5:6], sstat[:, 4:5], AF.Ln, bias=small[:, 20:21], scale=1.0 / 128, r=[kss, ksm], w=[kss])
                    act(sstat[:, 6:7], sstat[:, 5:6], AF.Exp, scale=-0.5, r=[kss], w=[kss])
                    stt(osb, osb, sstat[:, 6:7], ogain, ALU.mult, ALU.mult, r=[kosb, kss, kog], w=[kosb])
                    tt(ycat, osb, sgb[:, t, :], ALU.mult, r=[kosb, ksgb], w=[kyc])
                    pt, kpt = bank_bf()
                    tr(pt[:, 0:128], ycat, identb, r=[kyc, kidb], w=[kpt])
                    cp(catT[:, 8 + h, t * 128:(t + 1) * 128], pt[:, 0:128], eng='act', r=[kpt], w=[kcat])
                h_front(0)
                for i in range(len(hsteps)):
                    if i + 1 < len(hsteps):
                        h_front(i + 1)
                    h_back(i)
            gbc, kgbc = XS, kXS
            p.dma(gbc, modd[2].partition_broadcast(128), reads=['modd'], writes=[kgbc])
            for cg in range(4):
                w_ = wbf[cg % 2]
                kw_ = kwbf[cg % 2]
                load_w(w_, kw_, w_out, cg * 512, 512, 0)
                for t in range(NT):
                    if cg == 0:
                        pass
                    pb, kpb = bank()
                    for kc in range(KC):
                        mm(pb[:], catT[:, kc, t * 128:(t + 1) * 128], w_[:, kc, 0:512], start=(kc == 0), stop=(kc == KC - 1),
                           r=[kcat, kw_], w=[kpb])
                    r0 = g * G + t * 128
                    xa, kxa = A['sq']
                    p.dma(xa, xr[r0:r0 + 128, cg * 512:(cg + 1) * 512], writes=[kxa])
                    tt(A['kn'][0], pb[:], gbc[:, cg * 512:(cg + 1) * 512], ALU.mult, r=[kpb, kgbc], w=[A['kn'][1]])
                    tt(A['kn'][0], A['kn'][0], xa, ALU.add, r=[A['kn'][1], kxa], w=[A['kn'][1]])
                    p.dma(x1s[r0:r0 + 128, cg * 512:(cg + 1) * 512], A['kn'][0], reads=[A['kn'][1]], writes=['x1s'], key=A['kn'][1])

            H = new_phase()
            for t in range(NT):
                r0 = g * G + t * 128
                norm_T(x1s[r0:r0 + 128, :], 2, hT, khT, t * 128)
            s1, ks1 = H.f32([128, NT, 8, 128])
            s2, ks2 = H.f32([128, NT, 8, 128])
            cdiag, kcd = H.bf16([128, NT, 8, 128])
            off_mark = H.off
            qTb, kqTb = H.f32([128, G])
            skT, kskT = H.f32([128, 128])
            t16a, kt16a = H.f32([128, 16])
            t16b, kt16b = H.f32([128, 16])
            scr, kscr = H.f32([128, 256])
            cand, kcand = H.f32([128, 256])
            best, kbest = H.f32([128, 16])
            pst, kpst = H.f32([128, 8])
            for hp in range(16):
                hd, half = hp // 2, hp % 2
                load_w(wbf[hp % 2], kwbf[hp % 2], w_q, hp * 128, 128, 0)
                pb, kpb = bank()
                proj_fm(pb[:, 0:G], kpb, wbf[hp % 2], kwbf[hp % 2], 0, hT, khT, 0, G)
                cp(qTb, pb[:, 0:G], eng='act', r=[kpb], w=[kqTb])
                p.dma(XS[:, 0:128], skd[hp], writes=[kXS])
                pt, kpt = bank()
                tr(pt[:, 0:128], XS[:, 0:128], ident, r=[kXS, kid], w=[kpt])
                cp(skT, pt[:, 0:128], r=[kpt], w=[kskT])
                dst = s1 if half == 0 else s2
                kd = ks1 if half == 0 else ks2
                for t in range(NT):
                    pb2, kpb2 = bank()
                    mm(pb2[:, 0:128], qTb[:, t * 128:(t + 1) * 128], skT, r=[kqTb, kskT], w=[kpb2])
                    cp(dst[:, t, hd, :], pb2[:, 0:128], eng='act', r=[kpb2], w=[kd])
            for t in range(NT):
                for hd in range(8):
                    for (src, t16) in [(s1, t16a), (s2, t16b)]:
                        kt16 = kt16a if t16 is t16a else kt16b
                        p.op('dve', lambda e, src=src, t16=t16, t=t, hd=hd: e.max(out=t16[:, 0:8], in_=src[:, t, hd, :]), reads=[ks1, ks2], writes=[kt16])
                        p.op('dve', lambda e, src=src, t16=t16, t=t, hd=hd: e.match_replace(out=scr[:, 0:128], in_to_replace=t16[:, 0:8], in_values=src[:, t, hd, :], imm_value=-1e30),
                             reads=[ks1, ks2, kt16], writes=[kscr])
                        p.op('dve', lambda e, t16=t16: e.max(out=t16[:, 8:16], in_=scr[:, 0:128]), reads=[kscr, kt16], writes=[kt16])
                    tt(cand.rearrange("p (a b) -> p a b", a=16), t16a.unsqueeze(2).to_broadcast([128, 16, 16]),
                       t16b.unsqueeze(1).to_broadcast([128, 16, 16]), ALU.add, r=[kt16a, kt16b], w=[kcand])
                    p.op('dve', lambda e: e.max(out=best[:, 0:8], in_=cand), reads=[kcand], writes=[kbest])
                    p.op('dve', lambda e: e.match_replace(out=scr, in_to_replace=best[:, 0:8], in_values=cand, imm_value=-1e30),
                         reads=[kcand, kbest], writes=[kscr])
                    p.op('dve', lambda e: e.max(out=best[:, 8:16], in_=scr), reads=[kscr, kbest], writes=[kbest])
                    p.op('dve', lambda e: e.tensor_reduce(out=pst[:, 0:1], in_=best, axis=AX.X, op=ALU.max), reads=[kbest, kpst], writes=[kpst])
                    p.op('dve', lambda e: e.tensor_reduce(out=pst[:, 1:2], in_=best, axis=AX.X, op=ALU.min), reads=[kbest, kpst], writes=[kpst])
                    ts(pst[:, 2:3], pst[:, 0:1], -1.0, None, ALU.mult, r=[kpst], w=[kpst])
                    p.op('dve', lambda e: e.memset(pst[:, 3:4], 0.0), reads=[kpst], writes=[kpst])
                    act(scr[:, 0:16], best, AF.Exp, bias=pst[:, 2:3], scale=1.0, accum=pst[:, 3:4], r=[kbest, kpst], w=[kscr, kpst])
                    tt(pst[:, 4:5], pst[:, 1:2], pst[:, 0:1], ALU.subtract, r=[kpst], w=[kpst])
                    act(pst[:, 4:5], pst[:, 4:5], AF.Exp, r=[kpst], w=[kpst])
                    p.op('dve', lambda e: e.reciprocal(out=pst[:, 5:6], in_=pst[:, 3:4]), reads=[kpst], writes=[kpst])
                    tt(pst[:, 4:5], pst[:, 4:5], pst[:, 5:6], ALU.mult, r=[kpst], w=[kpst])
                    ts(cdiag[:, t, hd, :], ident, pst[:, 4:5], None, ALU.mult, r=[kid, kpst], w=[kcd])
                    ts(s1[:, t, hd, :], s1[:, t, hd, :], pst[:, 1:2], None, ALU.subtract, r=[ks1, kpst], w=[ks1])
            H.off = off_mark
            p.barrier()
            acc, kacc = H.f32([128, NT, D])
            kaccs = [kacc + str(t) for t in range(NT)]
            p.op('pool', lambda e: e.memset(acc, 0.0), writes=kaccs)
            NB = 4
            uTv = [wbfF[0][:, i * 2048:(i + 1) * 2048].rearrange("p (a b) -> p a b", a=KC) for i in range(2)]
            kuTv = ['uTv0', 'uTv1']
            vbv = [wbfF[0][:, 4096 + i * 2048: 4096 + (i + 1) * 2048] for i in range(3)] + \
                  [wbfF[1][:, i * 2048:(i + 1) * 2048] for i in range(5)]
            kvbv = ['vbv%d' % i for i in range(8)]
            Lbr = [(XS[:, i * 1024:(i + 1) * 1024].rearrange("p (a b) -> p a b", a=8), 'Lbr%d' % i) for i in range(2)] + \
                  [(wstF[:, i * 1024:(i + 1) * 1024].rearrange("p (a b) -> p a b", a=8), 'Lbr%d' % (2 + i)) for i in range(2)]
            Xbr = [H.bf16([128, 8, 128]) for _ in range(4)]
            Xmr = [H.bf16([128, 8, 128]) for _ in range(4)]
            gelr = [(xn[:, i * 512:(i + 1) * 512], 'gelr%d' % i) for i in range(4)]
            kGTc = ['gtc%d' % i for i in range(8)]
            p.barrier()
            nblk = CFG['nec'] // NB
            cnt2 = [0]

            def s1_block(blk):
                for c in range(NB):
                    ec = blk * NB + c
                    uT_, kuT_ = uTv[ec % 2], kuTv[ec % 2]
                    vb_, kvb_ = vbv[ec % 8], kvbv[ec % 8]
                    p.dma(uT_, uTd[ec].rearrange("p (a b) -> p a b", a=KC), writes=[kuT_])
                    p.dma(vb_, vd[ec], writes=[kvb_])
                    pa, kpa = bank()
                    for kc in range(KC):
                        mm(pa[:, 0:G], uT_[:, kc, :], hT[:, kc, 0:G], start=(kc == 0), stop=(kc == KC - 1), r=[kuT_, khT], w=[kpa])
                    gel, kgel = gelr[c]
                    act(gel, pa[:, 0:G], AF.Gelu, r=[kpa], w=[kgel])
                for c in range(NB):
                    ec = blk * NB + c
                    gel, kgel = gelr[c]
                    pw, kpw = bank()
                    for t in range(NT):
                        j = cnt2[0] % 4
                        cnt2[0] += 1
                        Lb, kLb = Lbr[j]
                        Xb, kXb = Xbr[j]
                        Xm, kXm = Xmr[j]
                        tt(Lb, s2[:, t], s1[:, t, :, ec:ec + 1].to_broadcast([128, 8, 128]), ALU.add, eng=('pool' if t % 2 == 0 else 'dve'),
                           r=[ks1, ks2], w=[kLb])
                        act(Xb, Lb, AF.Exp, r=[kLb], w=[kXb])
                        stt(Xm, Lb, 0.0, Xb, ALU.is_ge, ALU.mult, r=[kLb, kXb], w=[kXm])
                        for hd in range(8):
                            mm(pw[:, t * 128:(t + 1) * 128], Xm[:, hd, :], cdiag[:, t, hd, :], start=(hd == 0), stop=(hd == 7),
                               r=[kXm, kcd], w=[kpw])
                    gi = (blk % 2) * NB + c
                    GTc = hT[:, 2 * gi:2 * gi + 2, 512:768]
                    tt(GTc, gel.rearrange("p (a b) -> p a b", a=2), pw[:, 0:G].rearrange("p (a b) -> p a b", a=2), ALU.mult,
                       r=[kgel, kpw], w=[kGTc[gi]])

            def s2_block(blk):
                for t in range(NT):
                    for dh in range(2):
                        pvb = []
                        for q in range(2):
                            pvv, kpvv = bank()
                            pvb.append((pvv, kpvv))
                            dg = dh * 2 + q
                            for c in range(NB):
                                ec = blk * NB + c
                                gi = (blk % 2) * NB + c
                                mm(pvv[:], hT[:, 2 * gi + t // 2, 512 + (t % 2) * 128: 512 + (t % 2) * 128 + 128],
                                   vbv[ec % 8][:, dg * 512:(dg + 1) * 512], start=(c == 0), stop=(c == NB - 1),
                                   r=[kGTc[gi], kvbv[ec % 8]], w=[kpvv])
                        for q in range(2):
                            dg = dh * 2 + q
                            pvv, kpvv = pvb[q]
                            tt(acc[:, t, dg * 512:(dg + 1) * 512], acc[:, t, dg * 512:(dg + 1) * 512], pvv[:], ALU.add,
                               r=[kacc + str(t), kpvv], w=[kacc + str(t)])
            for blk in range(nblk):
                s1_block(blk)
                if blk > 0:
                    s2_block(blk - 1)
            s2_block(nblk - 1)
            p.barrier()
            un = [XS, wstF]
            kun = [[kXS], list(kwst)]
            p.dma(un[1], modd[5].partition_broadcast(128), reads=['modd'], writes=kun[1])
            for t in range(NT):
                r0 = g * G + t * 128
                p.dma(XS, x1s[r0:r0 + 128, :], reads=['x1s'], writes=[kXS])
                tt(acc[:, t, :], acc[:, t, :], un[1], ALU.mult, r=[kacc + str(t)] + kun[1], w=[kacc + str(t)])
                tt(acc[:, t, :], acc[:, t, :], XS, ALU.add, r=[kacc + str(t), kXS], w=[kacc + str(t)])
                p.dma(outd[r0:r0 + 128, :], acc[:, t, :], reads=[kacc + str(t)], writes=['outd'], key='outk', final=True)
        print("ops recorded", p.nops, {e: len(v) for e, v in p.ops.items()}, "dsems", len(p.dsem))
        p.emit()
    return nc


def make_in_maps(inputs):
    x = np.asarray(inputs["x"], np.float32)
    f32 = lambda k: np.ascontiguousarray(np.asarray(inputs[k], np.float32))
    in_maps = []
    for core in range(8):
        b, j = core // 4, core % 4
        fl = np.zeros((128, 32), np.float32)
        for m in range(4):
            rf = 1.0 if (j - 3 + m) <= 0 else 0.0
            rb = 1.0 if (j + 3 - m) >= 3 else 0.0
            fl[:, 16 + m] = 1.0 - rf
            fl[:, 20 + m] = 1.0 - rb
        fl[:, 8] = 1.0 if j > 0 else 0.0
        fl[:, 9] = 1.0 if j < 3 else 0.0
        fl[:, 10] = float(j * 64)
        in_maps.append({
            "xr": np.ascontiguousarray(np.roll(x[b], -j * TOK, axis=0)),
            "ctxb": f32("ctx")[b],
            "cvec": np.ascontiguousarray(np.stack([f32("c")[b], f32("c_ctx")])),
            "w_ada": f32("w_ada")[0], "b_ada": f32("b_ada")[0],
            "norm_mix": f32("norm_mix")[0], "norm_ffn": f32("norm_ffn")[0],
            "w_in": f32("w_in")[0], "q_norm": f32("q_norm")[0], "k_norm": f32("k_norm")[0],
            "attn_sink": f32("attn_sink")[0], "lbl": f32("hgrn_lb_logits"),
            "hgrn_norm": f32("hgrn_norm")[0], "w_out": f32("w_out")[0], "w_q": f32("peer_w_q")[0],
            "sk": f32("peer_sub_keys")[0].reshape(16, 128, 128),
            "pu": f32("peer_u")[0], "pv": f32("peer_v")[0],
            "flags": fl,
        })
    return in_maps


def kernel(**inputs):
    nc = build_nc()
    in_maps = make_in_maps(inputs)
    res = run_bass_kernel_spmd(nc, in_maps, core_ids=list(range(8)))
    out = np.zeros((2, SEQ, D), np.float32)
    for core in range(8):
        b, j = core // 4, core % 4
        out[b, j * TOK:(j + 1) * TOK] = res.results[core]["out"]
    return out
```

```python
import numpy as np
from contextlib import ExitStack
import concourse.bass as bass
import concourse.mybir as mybir
from concourse.bass_utils import run_bass_kernel_spmd

F32 = mybir.dt.float32
BF16 = mybir.dt.bfloat16
ALU = mybir.AluOpType
AF = mybir.ActivationFunctionType
AX = mybir.AxisListType

D = 2048
KC = 16
TOK = 4096
SEQ = 16384
G = 512
NT = G // 128
NG = TOK // G
EXT = NT + 2
SGT = 4
NCOL = 6656
C_K, C_V, C_FF, C_FB, C_I, C_QA, C_QH, C_GG = 0, 256, 512, 1536, 2560, 3584, 4608, 5632
EPS = 1e-6
NE = 16384
ENG = ['pe', 'act', 'dve', 'pool', 'sp']
CFG = dict(ng=NG, nec=NE // 128, nslot=3, nsub=TOK // (SGT * 128), mods=6)
PI = float(np.pi)


class Prog:
    def __init__(self, nc, es):
        self.nc = nc
        self.es = es
        self.ops = {e: [] for e in ENG}
        self.esem = {e: es.enter_context(nc.semaphore('sem_' + e)) for e in ENG if e != 'sp'}
        self.cnt = {e: 0 for e in ENG}
        self.known = {e: {} for e in ENG}
        self.bufs = {}
        self.dsem = {}
        self.final = []
        self.floor = {}
        self.nops = 0

    def _deps(self, reads, writes):
        deps = dict(self.floor)

        def add(s, v):
            if deps.get(s, 0) < v:
                deps[s] = v
        for k in reads:
            b = self.bufs.get(k)
            if b and b['w']:
                add(*b['w'])
        for k in writes:
            b = self.bufs.get(k)
            if b:
                if b['w']:
                    add(*b['w'])
                for s, v in b['r'].items():
                    add(s, v)
        return deps

    def _commit(self, reads, writes, ev):
        s, v = ev
        for k in reads:
            b = self.bufs.setdefault(k, {'w': None, 'r': {}})
            if b['r'].get(s, 0) < v:
                b['r'][s] = v
        for k in writes:
            self.bufs[k] = {'w': ev, 'r': {}}

    def _waits(self, eng, deps):
        waits = []
        kn = self.known[eng]
        for s, v in deps.items():
            if eng == 'pe' and s is self.esem['pe']:
                continue
            if kn.get(s, 0) < v:
                kn[s] = v
                waits.append((s, v))
        return waits

    def op(self, eng, fn, reads=(), writes=()):
        deps = self._deps(reads, writes)
        waits = self._waits(eng, deps)
        self.cnt[eng] += 1
        ev = (self.esem[eng], self.cnt[eng])
        self.ops[eng].append((waits, fn, ev[0], 1))
        self._commit(reads, writes, ev)
        self.nops += 1

    def dma(self, out, in_, reads=(), writes=(), key=None, eng='sp', final=False, **kw):
        if key is None:
            key = (list(writes) + list(reads))[0]
        if key not in self.dsem:
            self.dsem[key] = [self.es.enter_context(self.nc.semaphore('dsem%d' % len(self.dsem))), 0]
        ds = self.dsem[key]
        deps = self._deps(reads, writes)
        waits = self._waits(eng, deps)
        ds[1] += 16
        ev = (ds[0], ds[1])
        self.ops[eng].append((waits, lambda e: e.dma_start(out=out, in_=in_, **kw), ds[0], 16))
        self._commit(reads, writes, ev)
        self.nops += 1
        if final:
            self.final.append(ev)

    def barrier(self):
        for e in ENG:
            if e != 'sp' and self.cnt[e] > 0:
                self.floor[self.esem[e]] = self.cnt[e]
        for k, (s, v) in self.dsem.items():
            if v > 0:
                self.floor[s] = v

    def emit(self):
        nc = self.nc
        engmap = {'pe': 'tensor', 'act': 'scalar', 'dve': 'vector', 'pool': 'gpsimd', 'sp': 'sync'}
        with nc.Block() as block:
            for e in ENG:
                ops = self.ops[e]
                fin = self.final if e == 'sp' else []

                def body(eng, ops=ops, fin=fin):
                    for waits, fn, sem, inc in ops:
                        for s, v in waits:
                            eng.wait_ge(s, v)
                        fn(eng).then_inc(sem, inc)
                    for s, v in fin:
                        eng.wait_ge(s, v)
                getattr(block, engmap[e])(body)


class Arena:
    def __init__(self, ap, size, name):
        self.ap, self.size, self.off, self.name, self.n = ap, size, 0, name, 0

    def _shape(self, v, shape):
        if shape[0] < 128:
            v = v[0:shape[0]]
        if len(shape) == 2:
            return v
        if len(shape) == 3:
            return v.rearrange("p (a b) -> p a b", a=shape[1])
        return v.rearrange("p (a b c) -> p a b c", a=shape[1], b=shape[2])

    def _take(self, nf):
        assert self.off + nf <= self.size, (self.name, self.off, nf, self.size)
        v = self.ap[:, self.off:self.off + nf]
        self.off += nf
        self.n += 1
        return v, '%s_%d_%d' % (self.name, self.off, self.n)

    def f32(self, shape):
        n = int(np.prod(shape[1:]))
        v, k = self._take(n)
        return self._shape(v, shape), k

    def bf16(self, shape):
        n = int(np.prod(shape[1:]))
        v, k = self._take((n + 1) // 2)
        v = v.bitcast(BF16)[:, 0:n]
        return self._shape(v, shape), k


def build_nc():
    nc = bass.Bass("TRN2", target_bir_lowering=False)
    di = lambda n, s: nc.dram_tensor(n, s, F32, kind="ExternalInput").ap()
    xr = di("xr", [SEQ, D])
    ctxb = di("ctxb", [256, D])
    cvec = di("cvec", [2, D])
    w_ada = di("w_ada", [D, 6 * D])
    b_ada = di("b_ada", [6 * D])
    norm_mix = di("norm_mix", [D])
    norm_ffn = di("norm_ffn", [D])
    w_in = di("w_in", [D, NCOL])
    q_norm = di("q_norm", [128])
    k_norm = di("k_norm", [128])
    attn_sink = di("attn_sink", [8])
    lbl = di("lbl", [2, 2, 1024])
    hgrn_norm = di("hgrn_norm", [128])
    w_out = di("w_out", [D, D])
    w_q = di("w_q", [D, D])
    skd = di("sk", [16, 128, 128])
    pu = di("pu", [NE, D])
    pv = di("pv", [NE, D])
    flags = di("flags", [128, 32])
    outd = nc.dram_tensor("out", [TOK, D], F32, kind="ExternalOutput").ap()
    modd = nc.dram_tensor("modd", [8, D], F32, kind="Internal").ap()
    lbd = nc.dram_tensor("lbd", [2, 2, 1024], F32, kind="Internal").ap()
    x1s = nc.dram_tensor("x1s", [TOK, D], F32, kind=("ExternalOutput" if CFG.get("dbg") else "Internal")).ap()
    ssave = nc.dram_tensor("ssave", [NG, 128, 1024], F32, kind="Internal").ap()
    w_in_b = nc.dram_tensor("w_in_b", [D, NCOL], BF16, kind="Internal").ap()
    w_out_b = nc.dram_tensor("w_out_b", [D, D], BF16, kind="Internal").ap()
    w_q_b = nc.dram_tensor("w_q_b", [D, D], BF16, kind="Internal").ap()
    uTd = nc.dram_tensor("uTd", [NE // 128, 128, D], BF16, kind="Internal").ap()
    vd = nc.dram_tensor("vd", [NE // 128, 128, D], BF16, kind="Internal").ap()

    with ExitStack() as es:
        PERS = 29960
        PH = 23240
        pers_t = es.enter_context(nc.sbuf_tensor("pers", [128, PERS], F32))
        ph_t = es.enter_context(nc.sbuf_tensor("phase", [128, PH], F32))
        PS = [es.enter_context(nc.psum_tensor("ps%d" % i, [128, 512], F32)) for i in range(8)]
        p = Prog(nc, es)
        PA = Arena(pers_t[:], PERS, 'P')
        ph_n = [0]

        def new_phase():
            p.barrier()
            ph_n[0] += 1
            return Arena(ph_t[:], PH, 'H%d' % ph_n[0])

        psn = [0]

        def bank():
            i = psn[0] % 8
            psn[0] += 1
            return PS[i], 'psb%d' % i

        def bank_bf():
            t, k = bank()
            return t[:].bitcast(BF16), k

        val, kval = PA.f32([128, 128])
        vs, kvs = PA.f32([128, 128])
        vt, kvt = PA.f32([128, 128])
        p.op('pool', lambda e: e.iota(val, pattern=[[1, 128]], base=0, channel_multiplier=-1,
                                      allow_small_or_imprecise_dtypes=True), writes=[kval])
        p.op('pool', lambda e: e.iota(vs, pattern=[[0, 128]], base=0, channel_multiplier=1,
                                      allow_small_or_imprecise_dtypes=True), writes=[kvs])
        p.op('pool', lambda e: e.iota(vt, pattern=[[1, 128]], base=0, channel_multiplier=0,
                                      allow_small_or_imprecise_dtypes=True), writes=[kvt])

        def ts(out, in0, s1, s2, op0, op1=None, eng='dve', r=(), w=()):
            if op1 is None:
                p.op(eng, lambda e: e.tensor_scalar(out=out, in0=in0, scalar1=s1, scalar2=None, op0=op0), reads=r, writes=w)
            else:
                p.op(eng, lambda e: e.tensor_scalar(out=out, in0=in0, scalar1=s1, scalar2=s2, op0=op0, op1=op1), reads=r, writes=w)

        def tt(out, in0, in1, op, eng='dve', r=(), w=()):
            p.op(eng, lambda e: e.tensor_tensor(out=out, in0=in0, in1=in1, op=op), reads=r, writes=w)

        def stt(out, in0, sc, in1, op0, op1, eng='dve', r=(), w=()):
            p.op(eng, lambda e: e.scalar_tensor_tensor(out=out, in0=in0, scalar=sc, in1=in1, op0=op0, op1=op1), reads=r, writes=w)

        def act(out, in_, func, bias=None, scale=1.0, accum=None, r=(), w=()):
            kw = {}
            if bias is not None:
                kw['bias'] = bias
            if accum is not None:
                kw['accum_out'] = accum
            p.op('act', lambda e: e.activation(out=out, in_=in_, func=func, scale=scale, **kw), reads=r, writes=w)

        def cp(out, in_, eng='dve', r=(), w=()):
            if eng == 'act':
                p.op('act', lambda e: e.copy(out=out, in_=in_), reads=r, writes=w)
            else:
                p.op(eng, lambda e: e.tensor_copy(out=out, in_=in_), reads=r, writes=w)

        def mm(out, lhsT, rhs, start=True, stop=True, r=(), w=()):
            p.op('pe', lambda e: e.matmul(out, lhsT=lhsT, rhs=rhs, start=start, stop=stop), reads=r, writes=w)

        def tr(out, in_, idn, r=(), w=()):
            p.op('pe', lambda e: e.transpose(out, in_, idn), reads=r, writes=w)

        ident, kid = PA.f32([128, 128])
        ts(ident, val, 0.0, None, ALU.is_equal, r=[kval], w=[kid])
        identb, kidb = PA.bf16([128, 128])
        cp(identb, ident, r=[kid], w=[kidb])
        c_tmp, kct = PA.f32([128, 128])
        c_tmp2, kct2 = PA.f32([128, 128])
        BM, kbm = PA.f32([128, 128])
        ts(c_tmp, vs, 64.0, None, ALU.is_ge, r=[kvs], w=[kct])
        ts(c_tmp2, vt, 64.0, None, ALU.is_ge, r=[kvt], w=[kct2])
        tt(BM, c_tmp, c_tmp2, ALU.is_equal, r=[kct, kct2], w=[kbm])
        ind, kind = PA.f32([128, 2])
        cp(ind[:, 1:2], c_tmp[:, 0:1], r=[kct], w=[kind])
        ts(ind[:, 0:1], c_tmp[:, 0:1], -1.0, 1.0, ALU.mult, ALU.add, r=[kct, kind], w=[kind])
        TI, TX, TIb_, = {}, {}, {}
        kTI, kTX = {}, {}
        for d_, (opi, opx) in enumerate([(ALU.is_ge, ALU.is_lt), (ALU.is_le, ALU.is_gt)]):
            TI[d_], kTI[d_] = PA.f32([128, 128])
            TX[d_], kTX[d_] = PA.f32([128, 128])
            ts(TI[d_], val, 0.0, None, opi, r=[kval], w=[kTI[d_]])
            tt(TI[d_], TI[d_], BM, ALU.mult, r=[kTI[d_], kbm], w=[kTI[d_]])
            ts(TX[d_], val, 0.0, None, opx, r=[kval], w=[kTX[d_]])
            tt(TX[d_], TX[d_], BM, ALU.mult, r=[kTX[d_], kbm], w=[kTX[d_]])
        TXF, kTXF = {}, {}
        for d_, opx in enumerate([ALU.is_lt, ALU.is_gt]):
            TXF[d_], kTXF[d_] = PA.f32([128, 128])
            ts(TXF[d_], val, 0.0, None, opx, r=[kval], w=[kTXF[d_]])
        fl, kfl = PA.f32([128, 32])
        p.dma(fl, flags, writes=[kfl])
        maskP, kmP = PA.bf16([128, 128])
        maskN, kmN = PA.bf16([128, 128])
        maskP0, kmP0 = PA.bf16([128, 128])
        maskN0, kmN0 = PA.bf16([128, 128])
        ts(maskP, val, 0.0, None, ALU.is_le, r=[kval], w=[kmP])
        ts(maskN, val, 0.0, None, ALU.is_ge, r=[kval], w=[kmN])
        ts(maskP0, val, 0.0, fl[:, 8:9], ALU.is_le, ALU.mult, r=[kval, kfl], w=[kmP0])
        ts(maskN0, val, 0.0, fl[:, 9:10], ALU.is_ge, ALU.mult, r=[kval, kfl], w=[kmN0])
        ones_f, kof = PA.f32([128, 128])
        ones_b, kob = PA.bf16([128, 128])
        p.op('dve', lambda e: e.memset(ones_f, 1.0), writes=[kof])
        p.op('dve', lambda e: e.memset(ones_b, 1.0), writes=[kob])
        PermT, kpm = PA.f32([128, 128])
        p.op('pool', lambda e: e.iota(c_tmp.rearrange("p (a b) -> p a b", a=2), pattern=[[0, 2], [1, 64]], base=0, channel_multiplier=0,
                                      allow_small_or_imprecise_dtypes=True), reads=[kct], writes=[kct])
        ts(c_tmp, c_tmp, 32.0, None, ALU.is_lt, r=[kct], w=[kct])
        ts(c_tmp2, val, -32.0, None, ALU.is_equal, r=[kval], w=[kct2])
        tt(PermT, c_tmp2, c_tmp, ALU.mult, r=[kct, kct2], w=[kpm])
        ts(c_tmp, c_tmp, -1.0, 1.0, ALU.mult, ALU.add, r=[kct], w=[kct])
        ts(c_tmp2, val, 32.0, None, ALU.is_equal, r=[kval], w=[kct2])
        tt(c_tmp2, c_tmp2, c_tmp, ALU.mult, r=[kct, kct2], w=[kct2])
        tt(PermT, PermT, c_tmp2, ALU.add, r=[kpm, kct2], w=[kpm])
        small, ksm = PA.f32([128, 64])
        sgn = small[:, 0:1]
        invf = small[:, 1:2]
        pcol = vs[:, 0:1]
        ts(small[:, 30:31], pcol, 32.0, None, ALU.is_lt, r=[kvs, ksm], w=[ksm])
        ts(small[:, 31:32], pcol, 64.0, None, ALU.is_ge, r=[kvs, ksm], w=[ksm])
        ts(small[:, 32:33], pcol, 96.0, None, ALU.is_lt, r=[kvs, ksm], w=[ksm])
        tt(small[:, 2:3], small[:, 31:32], small[:, 32:33], ALU.mult, r=[ksm], w=[ksm])
        tt(small[:, 2:3], small[:, 2:3], small[:, 30:31], ALU.add, r=[ksm], w=[ksm])
        ts(sgn, small[:, 2:3], -2.0, 1.0, ALU.mult, ALU.add, r=[ksm], w=[ksm])
        ts(small[:, 33:34], pcol, 32.0, None, ALU.is_ge, r=[kvs, ksm], w=[ksm])
        ts(small[:, 34:35], pcol, 96.0, None, ALU.is_ge, r=[kvs, ksm], w=[ksm])
        tt(small[:, 33:34], small[:, 33:34], small[:, 31:32], ALU.add, r=[ksm], w=[ksm])
        tt(small[:, 33:34], small[:, 33:34], small[:, 34:35], ALU.add, r=[ksm], w=[ksm])
        stt(small[:, 3:4], small[:, 33:34], -32.0, pcol, ALU.mult, ALU.add, r=[ksm, kvs], w=[ksm])
        act(invf, small[:, 3:4], AF.Exp, scale=-float(np.log(10000.0)) / 32.0, r=[ksm], w=[ksm])
        qg = small[:, 4:5]
        kg = small[:, 5:6]
        p.dma(qg, q_norm.rearrange("(p o) -> p o", o=1), reads=[ksm], writes=[ksm])
        p.dma(kg, k_norm.rearrange("(p o) -> p o", o=1), reads=[ksm], writes=[ksm])
        esink = small[:, 8:16]
        p.dma(esink, attn_sink.partition_broadcast(128), reads=[ksm], writes=[ksm])
        act(esink, esink, AF.Exp, r=[ksm], w=[ksm])
        ogain, kog = PA.f32([128, 128])
        p.dma(ogain, hgrn_norm.partition_broadcast(128), writes=[kog])
        cols, kcols = PA.f32([128, 8, KC])
        nrm, knrm = PA.f32([128, 2, KC])
        p.dma(nrm[:, 0, :], norm_mix.rearrange("(c p) -> p c", p=128), writes=[knrm], allow_slow_non_contiguous=True)
        p.dma(nrm[:, 1, :], norm_ffn.rearrange("(c p) -> p c", p=128), reads=[knrm], writes=[knrm], allow_slow_non_contiguous=True)
        S_all, kS = PA.f32([128, 16, 128])
        kSh = [[kS + '_%d_%d' % (d_, h) for h in range(8)] for d_ in range(2)]
        Sb_all, _ = PA.bf16([128, 16, 128])
        kSb = [[kS + 'b_%d_%d' % (d_, h) for h in range(8)] for d_ in range(2)]
        S_ctx, kSc = PA.f32([128, 16, 128])
        kcT_c, kkc = PA.bf16([128, 2, 256])
        v_c, kvc = PA.bf16([128, 2, 2, 128])
        hT, khT = PA.bf16([128, KC, 768])
        wbf, kwbf = [], []
        wbfF = []
        for i in range(2):
            a, k = PA.bf16([128, KC * 640])
            wbfF.append(a)
            wbf.append(a.rearrange("p (a b) -> p a b", a=KC))
            kwbf.append(k)
        wstF, _ = PA.f32([128, 2048])
        wst, kwst = [], []
        for i in range(4):
            wst.append(wstF[:, i * 512:(i + 1) * 512])
            kwst.append('wst%d' % i)
        XS, kXS = PA.f32([128, D])
        xn, kxn = PA.bf16([128, D])
        sstat, kss = PA.f32([128, 8])
        wl = [0]

        WB = {}

        def load_w(dst, kdst, wd, col0, ncols, dcol=0):
            src = WB[id(wd)].rearrange("(kc p) c -> p kc c", p=128)[:, :, col0:col0 + ncols]
            p.dma(dst[:, :, dcol:dcol + ncols], src, writes=[kdst])

        def norm_T(rows_ap, ci, dstT, kdst, c0):
            p.dma(XS, rows_ap, writes=[kXS])
            p.op('dve', lambda e: e.memset(sstat[:, 0:1], 0.0), reads=[kss], writes=[kss])
            act(xn, XS, AF.Square, accum=sstat[:, 0:1], r=[kXS, kss], w=[kxn, kss])
            act(sstat[:, 1:2], sstat[:, 0:1], AF.Ln, bias=small[:, 20:21], scale=1.0 / D, r=[kss, ksm], w=[kss])
            act(sstat[:, 2:3], sstat[:, 1:2], AF.Exp, scale=-0.5, r=[kss], w=[kss])
            act(xn, XS, AF.Identity, scale=sstat[:, 2:3], r=[kXS, kss], w=[kxn])
            for half in range(2):
                pb, kpb = bank_bf()
                for q in range(8):
                    kc = half * 8 + q
                    tr(pb[:, q * 128:(q + 1) * 128], xn[:, kc * 128:(kc + 1) * 128], identb, r=[kxn, kidb], w=[kpb])
                for q in range(8):
                    kc = half * 8 + q
                    if q % 2 == 0:
                        act(dstT[:, kc, c0:c0 + 128], pb[:, q * 128:(q + 1) * 128], AF.Identity,
                            bias=cols[:, ci + 1, kc:kc + 1], scale=cols[:, ci, kc:kc + 1], r=[kpb, kcols], w=[kdst])
                    else:
                        ts(dstT[:, kc, c0:c0 + 128], pb[:, q * 128:(q + 1) * 128], cols[:, ci, kc:kc + 1],
                           cols[:, ci + 1, kc:kc + 1], ALU.mult, ALU.add, r=[kpb, kcols], w=[kdst])

        p.op('dve', lambda e: e.memset(small[:, 20:21], EPS), reads=[ksm], writes=[ksm])
        p.op('dve', lambda e: e.memset(small[:, 21:22], -PI), reads=[ksm], writes=[ksm])
        p.op('dve', lambda e: e.memset(small[:, 22:23], 1.0), reads=[ksm], writes=[ksm])

        H = new_phase()
        cv, kcv = H.f32([128, KC, 2])
        p.dma(cv[:, :, 0], cvec[0].rearrange("(c p) -> p c", p=128), writes=[kcv], allow_slow_non_contiguous=True)
        p.dma(cv[:, :, 1], cvec[1].rearrange("(c p) -> p c", p=128), reads=[kcv], writes=[kcv], allow_slow_non_contiguous=True)
        act(cv, cv, AF.Silu, r=[kcv], w=[kcv])
        rep, krep = H.f32([128, 2, KC, 128])
        for v_ in range(2):
            cp(rep[:, v_], cv[:, :, v_].unsqueeze(2).to_broadcast([128, KC, 128]), r=[kcv], w=[krep])
        brow, kbrow = H.f32([128, 512])
        mrow, kmrow = H.f32([128, 512])
        for mi in range(CFG['mods']):
            for cg in range(4):
                col0 = mi * D + cg * 512
                pb0, kpb0 = bank()
                pb1, kpb1 = bank()
                for kc in range(KC):
                    i = wl[0] % 4
                    wl[0] += 1
                    p.dma(wst[i][:, 0:512], w_ada[kc * 128:(kc + 1) * 128, col0:col0 + 512], writes=[kwst[i]])
                    mm(pb0[:], rep[:, 0, kc], wst[i][:, 0:512], start=(kc == 0), stop=(kc == KC - 1), r=[krep, kwst[i]], w=[kpb0])
                    if mi < 2:
                        mm(pb1[:], rep[:, 1, kc], wst[i][:, 0:512], start=(kc == 0), stop=(kc == KC - 1), r=[krep, kwst[i]], w=[kpb1])
                p.dma(brow, b_ada[col0:col0 + 512].partition_broadcast(128), writes=[kbrow])
                tt(mrow, pb0[:], brow, ALU.add, r=[kpb0, kbrow], w=[kmrow])
                p.dma(modd[mi:mi + 1, cg * 512:(cg + 1) * 512], mrow[0:1, :], reads=[kmrow], writes=['modd'], key=kmrow)
                if mi < 2:
                    tt(mrow, pb1[:], brow, ALU.add, r=[kpb1, kbrow], w=[kmrow])
                    p.dma(modd[6 + mi:7 + mi, cg * 512:(cg + 1) * 512], mrow[0:1, :], reads=[kmrow], writes=['modd'], key=kmrow)
        mc, kmc = H.f32([128, 8, KC])
        for mi in range(8):
            p.dma(mc[:, mi, :], modd[mi].rearrange("(c p) -> p c", p=128), reads=['modd', kmc], writes=[kmc], allow_slow_non_contiguous=True)
        for (ci, sci, shi, ni) in [(0, 1, 0, 0), (2, 4, 3, 1), (4, 7, 6, 0)]:
            stt(cols[:, ci, :], mc[:, sci, :], 1.0, nrm[:, ni, :], ALU.add, ALU.mult, r=[kmc, knrm, kcols], w=[kcols])
            cp(cols[:, ci + 1, :], mc[:, shi, :], r=[kmc, kcols], w=[kcols])
        lt_, klt = H.f32([1, 2, 2, 1024])
        p.dma(lt_, lbl.rearrange("(o a) b c -> o a b c", o=1), writes=[klt])
        lo_, klo = H.f32([1, 2, 2, 1024])
        tt(lo_[:, 0], lt_[:, :, 0, :], lt_[:, :, 1, :], ALU.subtract, r=[klt], w=[klo])
        act(lo_[:, 0], lo_[:, 0], AF.Sigmoid, r=[klo], w=[klo])
        ts(lo_[:, 1], lo_[:, 0], -1.0, 1.0, ALU.mult, ALU.add, r=[klo], w=[klo])
        p.dma(lbd.rearrange("(o a) b c -> o a b c", o=1), lo_, reads=[klo], writes=['lbd'], key=klo)

        H = new_phase()
        stg = [H.f32([128, D]) for _ in range(4)]
        ubr = [H.bf16([128, D]) for _ in range(2)]
        uTr = [H.bf16([128, KC, 128]) for _ in range(2)]
        vbr = [H.bf16([128, D]) for _ in range(2)]
        WB[id(w_in)], WB[id(w_out)], WB[id(w_q)] = w_in_b, w_out_b, w_q_b
        wn = 0
        for (wsrc, wdst, ncol_) in [(w_in, w_in_b, NCOL), (w_out, w_out_b, D), (w_q, w_q_b, D)]:
            for kc in range(KC):
                for c0 in range(0, ncol_, 2048):
                    n = min(2048, ncol_ - c0)
                    su, ksu = stg[wn % 4]
                    ub_, kub_ = (ubr + vbr)[wn % 4]
                    p.dma(su[:, 0:n], wsrc[kc * 128:(kc + 1) * 128, c0:c0 + n], writes=[ksu])
                    cp(ub_[:, 0:n], su[:, 0:n], eng=['pool', 'dve', 'act'][wn % 3], r=[ksu], w=[kub_])
                    p.dma(wdst[kc * 128:(kc + 1) * 128, c0:c0 + n], ub_[:, 0:n], reads=[kub_], writes=['wb'], key=kub_)
                    wn += 1
        for ec in range(CFG['nec']):
            i = ec % 2
            su, ksu = stg[i]
            sv, ksv = stg[2 + i]
            p.dma(su, pu[ec * 128:(ec + 1) * 128, :], writes=[ksu])
            p.dma(sv, pv[ec * 128:(ec + 1) * 128, :], writes=[ksv])
            ub_, kub_ = ubr[i]
            cp(ub_, su, eng='pool', r=[ksu], w=[kub_])
            vb_, kvb_ = vbr[i]
            cp(vb_, sv, eng='dve', r=[ksv], w=[kvb_])
            p.dma(vd[ec], vb_, reads=[kvb_], writes=['vd'], key=kvb_)
            uT_, kuT_ = uTr[i]
            for half in range(2):
                pt, kpt = bank_bf()
                for q in range(8):
                    kc = half * 8 + q
                    tr(pt[:, q * 128:(q + 1) * 128], ub_[:, kc * 128:(kc + 1) * 128], identb, r=[kub_, kidb], w=[kpt])
                cp(uT_[:, half * 8:(half + 1) * 8, :], pt[:, 0:1024].rearrange("p (a b) -> p a b", a=8), eng='act', r=[kpt], w=[kuT_])
            p.dma(uTd[ec].rearrange("p (a b) -> p a b", a=KC), uT_, reads=[kuT_], writes=['uTd'], key=kuT_)

        def qk_norm_rope(ps_ap, kps, n, gain, dst, kdst, A, cosv=None, sinv=None, ktab=None):
            sq, ksq = A['sq']
            kn_, kkn = A['kn']
            act(sq[:, 0:n], ps_ap, AF.Square, r=[kps], w=[ksq])
            pb, kpb = bank()
            mm(pb[:, 0:n], ones_f, sq[:, 0:n], r=[kof, ksq], w=[kpb])
            act(sq[:, 0:n], pb[:, 0:n], AF.Ln, bias=small[:, 20:21], scale=1.0 / 128, r=[kpb, ksm], w=[ksq])
            act(sq[:, 0:n], sq[:, 0:n], AF.Exp, scale=-0.5, r=[ksq], w=[ksq])
            if cosv is None:
                stt(dst, ps_ap, gain, sq[:, 0:n], ALU.mult, ALU.mult, r=[kps, ksq, ksm], w=[kdst])
                return
            stt(kn_[:, 0:n], ps_ap, gain, sq[:, 0:n], ALU.mult, ALU.mult, r=[kps, ksq, ksm], w=[kkn])
            pb2, kpb2 = bank()
            mm(pb2[:, 0:n], PermT, kn_[:, 0:n], r=[kpm, kkn], w=[kpb2])
            tt(sq[:, 0:n], pb2[:, 0:n], sinv, ALU.mult, r=[kpb2, ktab, ksq], w=[ksq])
            tt(kn_[:, 0:n], kn_[:, 0:n], cosv, ALU.mult, eng='pool', r=[kkn, ktab], w=[kkn])
            tt(dst, kn_[:, 0:n], sq[:, 0:n], ALU.add, r=[kkn, ksq], w=[kdst])

        def proj_fm(dstps, kps, wv, kw, wc0, src, ksrc, t0, n):
            for kc in range(KC):
                mm(dstps, wv[:, kc, wc0:wc0 + 128], src[:, kc, t0:t0 + n], start=(kc == 0), stop=(kc == KC - 1),
                   r=[kw, ksrc], w=[kps])

        def proj_tm(dstps, kps, wv, kw, wc0, ncols, src, ksrc, t0):
            for kc in range(KC):
                mm(dstps, src[:, kc, t0:t0 + 128], wv[:, kc, wc0:wc0 + ncols], start=(kc == 0), stop=(kc == KC - 1),
                   r=[kw, ksrc], w=[kps])

        def load_lba(H):
            lba, klba = H.f32([128, 2, 2, 1024])
            for a_ in range(2):
                for d_ in range(2):
                    p.dma(lba[:, a_, d_, :], lbd[a_, d_, :].partition_broadcast(128), reads=['lbd', klba], writes=[klba])
            return lba, klba

        def gate_math(sg, ksg, lb_b, oml_b, klba):
            act(sg, sg, AF.Ln, bias=small[:, 22:23], scale=1.0, r=[ksg, ksm], w=[ksg])
            act(sg, sg, AF.Exp, scale=-1.0, r=[ksg], w=[ksg])
            tt(sg, sg, oml_b, ALU.mult, r=[ksg, klba], w=[ksg])
            tt(sg, sg, lb_b, ALU.add, r=[ksg, klba], w=[ksg])

        def state_step(d_, h, lf, kk, iv, kin, A):
            S = S_all[:, d_ * 8 + h, :]
            pbm, kpbm = bank()
            mm(pbm[:, 0:128], TX[d_], lf, r=[kTX[d_]] + kin, w=[kpbm])
            mm(pbm[:, 128:130], lf, ind, r=[kind] + kin, w=[kpbm])
            Eb, kEb = A['Eb']
            ee, kee = A['ee']
            act(Eb, pbm[:, 0:128], AF.Exp, r=[kpbm], w=[kEb])
            act(ee, pbm[:, 128:130], AF.Exp, r=[kpbm], w=[kee])
            for c in ([0, 1] if d_ == 0 else [1, 0]):
                khz, kkhz = A['khz'][c]
                stt(khz, kk, ind[:, c:c + 1], Eb, ALU.mult, ALU.mult, r=[kEb, kind] + kin, w=[kkhz])
                pd, kpd = bank()
                mm(pd[:, 0:128], khz, iv, r=[kkhz] + kin, w=[kpd])
                stt(S, S, ee[:, c:c + 1], pd[:, 0:128], ALU.mult, ALU.add, r=[kSh[d_][h], kee, kpd], w=[kSh[d_][h]])

        def full_step(d_, h, lf, kk, iv, qTt, kin, ops_, kops, A):
            S = S_all[:, d_ * 8 + h, :]
            Sb = Sb_all[:, d_ * 8 + h, :]
            pa, kpa = bank()
            mm(pa[:, 0:128], lf, TI[d_], r=[kTI[d_]] + kin, w=[kpa])
            pbm, kpbm = bank()
            mm(pbm[:, 0:128], TX[d_], lf, r=[kTX[d_]] + kin, w=[kpbm])
            aTs, kaT = A['aTs']
            cp(aTs, pa[:, 0:128], eng='act', r=[kpa], w=[kaT])
            refc = [31, 95] if d_ == 0 else [32, 96]
            endc = [63, 127] if d_ == 0 else [0, 64]
            negr, knr = A['negr']
            Eq, kEq = A['Eq']
            Ek, kEk = A['Ek']
            Ea, kEa = A['Ea']
            ee, kee = A['ee']
            for c in range(2):
                ts(negr[:, c:c + 1], aTs[:, refc[c]:refc[c] + 1], -1.0, None, ALU.mult, r=[kaT, knr], w=[knr])
            for c in range(2):
                sl = slice(64 * c, 64 * c + 64)
                act(Eq[:, sl], pa[:, sl], AF.Exp, bias=negr[:, c:c + 1], scale=1.0, r=[kpa, knr, kEq], w=[kEq])
                act(Ek[:, sl], pa[:, sl], AF.Exp, bias=aTs[:, refc[c]:refc[c] + 1], scale=-1.0, r=[kpa, kaT, kEk], w=[kEk])
                act(ee[:, c:c + 1], aTs[:, endc[c]:endc[c] + 1], AF.Exp, r=[kaT, kee], w=[kee])
            act(Ea, pa[:, 0:128], AF.Exp, r=[kpa], w=[kEa])
            Eb, kEb = A['Eb']
            act(Eb, pbm[:, 0:128], AF.Exp, r=[kpbm], w=[kEb])
            pt, kpt = bank_bf()
            tr(pt[:, 0:128], kk, identb, r=[kidb] + kin, w=[kpt])
            ktT, kktT = A['ktT']
            qtT, kqtT = A['qtT']
            qhT, kqhT = A['qhT']
            tt(ktT, pt[:, 0:128], Ek, ALU.mult, r=[kpt, kEk], w=[kktT])
            tt(qtT, qTt, Eq, ALU.mult, eng='pool', r=[kEq] + kin, w=[kqtT])
            tt(qhT, qTt, Ea, ALU.mult, eng='pool', r=[kEa] + kin, w=[kqhT])
            psc, kpsc = bank()
            mm(psc[:, 0:128], ktT, qtT, r=[kktT, kqtT], w=[kpsc])
            scT, kscT = A['scT']
            tt(scT, psc[:, 0:128], TI[d_], ALU.mult, r=[kpsc, kTI[d_]], w=[kscT])
            for c in range(2):
                khz, kkhz = A['khz'][c]
                stt(khz, kk, ind[:, c:c + 1], Eb, ALU.mult, ALU.mult, r=[kEb, kind] + kin, w=[kkhz])
            mm(ops_, scT, iv, start=True, stop=False, r=[kscT] + kin, w=[kops])
            order = [0, 1] if d_ == 0 else [1, 0]
            for n_, c in enumerate(order):
                mm(ops_[64 * c:64 * c + 64, :], qhT[:, 64 * c:64 * c + 64], Sb, start=False, stop=True,
                   r=[kqhT, kSb[d_][h]], w=[kops])
                khz, kkhz = A['khz'][c]
                pd, kpd = bank()
                mm(pd[:, 0:128], khz, iv, r=[kkhz] + kin, w=[kpd])
                stt(S, S, ee[:, c:c + 1], pd[:, 0:128], ALU.mult, ALU.add, r=[kSh[d_][h], kee, kpd], w=[kSh[d_][h]])
                cp(Sb, S, eng='act', r=[kSh[d_][h]], w=[kSb[d_][h]])

        def step_front(d_, h, lf, kk, iv, qTt, kin, A):
            pa, kpa = bank()
            mm(pa[:, 0:128], lf, TI[d_], r=[kTI[d_]] + kin, w=[kpa])
            pbm, kpbm = bank()
            mm(pbm[:, 0:128], TX[d_], lf, r=[kTX[d_]] + kin, w=[kpbm])
            aTs, kaT = A['aTs']
            cp(aTs, pa[:, 0:128], eng='act', r=[kpa], w=[kaT])
            refc = [31, 95] if d_ == 0 else [32, 96]
            endc = [63, 127] if d_ == 0 else [0, 64]
            negr, knr = A['negr']
            Eq, kEq = A['Eq']
            Ek, kEk = A['Ek']
            Ea, kEa = A['Ea']
            ee, kee = A['ee']
            for c in range(2):
                ts(negr[:, c:c + 1], aTs[:, refc[c]:refc[c] + 1], -1.0, None, ALU.mult, r=[kaT, knr], w=[knr])
            for c in range(2):
                sl = slice(64 * c, 64 * c + 64)
                act(Eq[:, sl], pa[:, sl], AF.Exp, bias=negr[:, c:c + 1], scale=1.0, r=[kpa, knr, kEq], w=[kEq])
                act(Ek[:, sl], pa[:, sl], AF.Exp, bias=aTs[:, refc[c]:refc[c] + 1], scale=-1.0, r=[kpa, kaT, kEk], w=[kEk])
                act(ee[:, c:c + 1], aTs[:, endc[c]:endc[c] + 1], AF.Exp, r=[kaT, kee], w=[kee])
            act(Ea, pa[:, 0:128], AF.Exp, r=[kpa], w=[kEa])
            Eb, kEb = A['Eb']
            act(Eb, pbm[:, 0:128], AF.Exp, r=[kpbm], w=[kEb])
            pt, kpt = bank_bf()
            tr(pt[:, 0:128], kk, identb, r=[kidb] + kin, w=[kpt])
            ktT, kktT = A['ktT']
            qtT, kqtT = A['qtT']
            qhT, kqhT = A['qhT']
            tt(ktT, pt[:, 0:128], Ek, ALU.mult, r=[kpt, kEk], w=[kktT])
            tt(qtT, qTt, Eq, ALU.mult, eng='pool', r=[kEq] + kin, w=[kqtT])
            tt(qhT, qTt, Ea, ALU.mult, eng='pool', r=[kEa] + kin, w=[kqhT])
            psc, kpsc = bank()
            mm(psc[:, 0:128], ktT, qtT, r=[kktT, kqtT], w=[kpsc])
            scT, kscT = A['scT']
            tt(scT, psc[:, 0:128], TI[d_], ALU.mult, r=[kpsc, kTI[d_]], w=[kscT])
            for c in range(2):
                khz, kkhz = A['khz'][c]
                stt(khz, kk, ind[:, c:c + 1], Eb, ALU.mult, ALU.mult, r=[kEb, kind] + kin, w=[kkhz])

        def step_back(d_, h, iv, kin, ops_, kops, A):
            S = S_all[:, d_ * 8 + h, :]
            Sb = Sb_all[:, d_ * 8 + h, :]
            scT, kscT = A['scT']
            qhT, kqhT = A['qhT']
            ee, kee = A['ee']
            mm(ops_, scT, iv, start=True, stop=False, r=[kscT] + kin, w=[kops])
            order = [0, 1] if d_ == 0 else [1, 0]
            for n_, c in enumerate(order):
                mm(ops_[64 * c:64 * c + 64, :], qhT[:, 64 * c:64 * c + 64], Sb, start=False, stop=True,
                   r=[kqhT, kSb[d_][h]], w=[kops])
                khz, kkhz = A['khz'][c]
                pd, kpd = bank()
                mm(pd[:, 0:128], khz, iv, r=[kkhz] + kin, w=[kpd])
                stt(S, S, ee[:, c:c + 1], pd[:, 0:128], ALU.mult, ALU.add, r=[kSh[d_][h], kee, kpd], w=[kSh[d_][h]])
                cp(Sb, S, eng='act', r=[kSh[d_][h]], w=[kSb[d_][h]])

        def step_scratch(H):
            A = {}
            A['Eb'] = H.f32([128, 128])
            A['ee'] = H.f32([128, 2])
            A['khz'] = [H.bf16([128, 128]), H.bf16([128, 128])]
            A['aTs'] = H.f32([128, 128])
            A['negr'] = H.f32([128, 2])
            A['Eq'] = H.f32([128, 128])
            A['Ek'] = H.f32([128, 128])
            A['Ea'] = H.f32([128, 128])
            A['ktT'] = H.bf16([128, 128])
            A['qtT'] = H.bf16([128, 128])
            A['qhT'] = H.bf16([128, 128])
            A['scT'] = H.bf16([128, 128])
            return A

        def hg_scratch(H):
            A = {}
            A['sg'] = H.f32([128, 128])
            A['Eb'] = H.f32([128, 128])
            A['ee'] = H.f32([128, 2])
            A['khz'] = [H.bf16([128, 128]), H.bf16([128, 128])]
            A['aTs'] = H.f32([128, 128])
            A['negr'] = H.f32([128, 2])
            A['Eq'] = H.f32([128, 128])
            A['Ek'] = H.f32([128, 128])
            A['Ea'] = H.f32([128, 128])
            A['ktT'] = H.bf16([128, 128])
            A['qtT'] = H.bf16([128, 128])
            A['qhT'] = H.bf16([128, 128])
            A['scT'] = H.bf16([128, 128])
            A['sq'] = H.f32([128, 512])
            A['kn'] = H.f32([128, 512])
            return A

        def load_lbh(lbh, klbh, h):
            for a in range(2):
                for d_ in range(2):
                    p.dma(lbh[:, a, d_, :], lbd[a, d_, h * 128:(h + 1) * 128].partition_broadcast(128),
                          reads=['lbd', klbh], writes=[klbh])

        def state_heads(d_, ntiles, A):
            lba, klba = A['lba']
            n_ = ntiles

            def quad_front(q):
                lfq, klfq = A['lfq'][q % 2]
                kkq, kkkq = A['kkq'][q % 2]
                ivq, kivq = A['ivq'][q % 2]
                sgq, ksgq = A['sgq'][q % 2]
                load_w(wbf[0], kwbf[0], w_in, (C_FF if d_ == 0 else C_FB) + q * 512, 512, 0)
                load_w(wbf[1], kwbf[1], w_in, C_I + q * 512, 512, 0)
                for t in range(ntiles):
                    pf, kpf = bank()
                    pi_, kpi = bank()
                    for kc in range(KC):
                        mm(pf[:], hT[:, kc, t * 128:(t + 1) * 128], wbf[0][:, kc, 0:512], start=(kc == 0), stop=(kc == KC - 1),
                           r=[kwbf[0], khT], w=[kpf])
                        mm(pi_[:], hT[:, kc, t * 128:(t + 1) * 128], wbf[1][:, kc, 0:512], start=(kc == 0), stop=(kc == KC - 1),
                           r=[kwbf[1], khT], w=[kpi])
                    act(sgq[:, t, :], pf[:], AF.Exp, scale=-1.0, r=[kpf, ksgq], w=[ksgq])
                    cp(ivq[:, t, :], pi_[:], r=[kpi, kivq], w=[kivq])
                sgv = sgq[:, 0:n_, :]
                qs = slice(q * 512, (q + 1) * 512)
                gate_math(sgv, ksgq, lba[:, 0, d_, qs].unsqueeze(1).to_broadcast([128, n_, 512]),
                          lba[:, 1, d_, qs].unsqueeze(1).to_broadcast([128, n_, 512]), klba)
                act(lfq[:, 0:n_, :], sgv, AF.Ln, r=[ksgq], w=[klfq])
                ts(kkq[:, 0:n_, :], sgv, -1.0, 1.0, ALU.mult, ALU.add, r=[ksgq], w=[kkkq])

            def head_tail(h):
                q, hq = h // 4, h % 4
                S = S_all[:, d_ * 8 + h, :]
                lfq, klfb = A['lfq'][q % 2]
                kkq, kkkb = A['kkq'][q % 2]
                ivq, kivb = A['ivq'][q % 2]
                hs = slice(hq * 128, (hq + 1) * 128)
                pa, kpa = bank()
                for t in range(ntiles):
                    mm(pa[:, 0:1], lfq[:, t, hs], ones_f[:, 0:1], start=(t == 0), stop=(t == ntiles - 1),
                       r=[klfb, kof], w=[kpa])
                ee, kee = A['ee2'][h % 2]
                act(ee[:, 0:1], pa[:, 0:1], AF.Exp, r=[kpa], w=[kee])
                pbs_ = []
                for t in range(ntiles):
                    others = [t2 for t2 in range(ntiles) if (t2 > t if d_ == 0 else t2 < t)]
                    pb, kpb = bank()
                    pbs_.append((pb, kpb))
                    mm(pb[:, 0:128], TXF[d_], lfq[:, t, hs], start=True, stop=(len(others) == 0),
                       r=[kTXF[d_], klfb], w=[kpb])
                    for q_, t2 in enumerate(others):
                        mm(pb[:, 0:128], ones_f, lfq[:, t2, hs], start=False, stop=(q_ == len(others) - 1),
                           r=[kof, klfb], w=[kpb])
                pd, kpd = bank()
                for t in range(ntiles):
                    pb, kpb = pbs_[t]
                    Eb, kEb = A['Ebr'][t % 2]
                    khz, kkhz = A['khz4'][t % 4]
                    act(Eb, pb[:, 0:128], AF.Exp, r=[kpb], w=[kEb])
                    tt(khz, kkq[:, t, hs], Eb, ALU.mult, r=[kEb, kkkb], w=[kkhz])
                for t in range(ntiles):
                    khz, kkhz = A['khz4'][t % 4]
                    mm(pd[:, 0:128], khz, ivq[:, t, hs], start=(t == 0), stop=(t == ntiles - 1), r=[kkhz, kivb], w=[kpd])
                stt(S, S, ee[:, 0:1], pd[:, 0:128], ALU.mult, ALU.add, r=[kSh[d_][h], kee, kpd], w=[kSh[d_][h]])

            quad_front(0)
            quad_front(1)
            for h in range(8):
                head_tail(h)

        def state_pass(d_, row0, ntiles, src_d, ci, H, A):
            for t in range(ntiles):
                norm_T(src_d[row0 + t * 128: row0 + (t + 1) * 128, :], ci, hT, khT, t * 128)
            state_heads(d_, ntiles, A)

        def sp_scratch(H, ntiles):
            A = hg_scratch(H)
            A['lba'] = load_lba(H)
            A['sgw'] = H.f32([128, ntiles, 128])
            A['lf'] = H.f32([128, ntiles, 128])
            A['kk'] = H.bf16([128, ntiles, 128])
            A['iv'] = H.bf16([128, ntiles, 128])
            A['Ebr'] = [A['Eb'], H.f32([128, 128])]
            A['lf2'] = [A['lf'], H.f32([128, ntiles, 128])]
            A['kk2'] = [A['kk'], H.bf16([128, ntiles, 128])]
            A['iv2'] = [A['iv'], H.bf16([128, ntiles, 128])]
            A['sg2'] = [A['sgw'], H.f32([128, ntiles, 128])]
            A['khz4'] = A['khz'] + [H.bf16([128, 128]), H.bf16([128, 128])]
            A['lfq'] = [H.f32([128, ntiles, 512]) for _ in range(2)]
            A['kkq'] = [H.bf16([128, ntiles, 512]) for _ in range(2)]
            A['ivq'] = [H.bf16([128, ntiles, 512]) for _ in range(2)]
            A['sgq'] = [H.f32([128, ntiles, 512]) for _ in range(2)]
            A['ee2'] = [A['ee'], H.f32([128, 2])]
            return A

        H = new_phase()
        A = sp_scratch(H, 2)
        for t in range(2):
            norm_T(ctxb[t * 128:(t + 1) * 128, :], 4, hT, khT, t * 128)
        for kvh in range(2):
            load_w(wbf[0], kwbf[0], w_in, C_K + kvh * 128, 128, 0)
            load_w(wbf[0], kwbf[0], w_in, C_V + kvh * 128, 128, 128)
            pb, kpb = bank()
            proj_fm(pb[:, 0:256], kpb, wbf[0], kwbf[0], 0, hT, khT, 0, 256)
            qk_norm_rope(pb[:, 0:256], kpb, 256, kg, kcT_c[:, kvh, :], kkc, A)
            for t in range(2):
                pb, kpb = bank()
                proj_tm(pb[:, 0:128], kpb, wbf[0], kwbf[0], 128, 128, hT, khT, t * 128)
                cp(v_c[:, kvh, t, :], pb[:, 0:128], eng='act', r=[kpb], w=[kvc])
        p.op('dve', lambda e: e.memset(S_all, 0.0), reads=[k for kk_ in kSh for k in kk_], writes=[k for kk_ in kSh for k in kk_])
        for d_ in range(2):
            state_heads(d_, 2, A)
        allS = [k for kk_ in kSh for k in kk_]
        cp(S_ctx, S_all, r=allS, w=[kSc])

        def reset_S(d_, fcol):
            for h in range(8):
                S = S_all[:, d_ * 8 + h, :]
                tt(S, S, S_ctx[:, d_ * 8 + h, :], ALU.subtract, r=[kSh[d_][h], kSc], w=[kSh[d_][h]])
                stt(S, S, fl[:, fcol:fcol + 1], S_ctx[:, d_ * 8 + h, :], ALU.mult, ALU.add,
                    r=[kSh[d_][h], kSc, kfl], w=[kSh[d_][h]])

        for m in range(CFG['nslot']):
            H = new_phase()
            A = sp_scratch(H, SGT)
            reset_S(0, 16 + m)
            for sg_ in range(CFG['nsub']):
                state_pass(0, (m + 1) * TOK + sg_ * SGT * 128, SGT, xr, 0, H, A)
        reset_S(0, 16 + 3)
        for m in range(CFG['nslot']):
            H = new_phase()
            A = sp_scratch(H, SGT)
            reset_S(1, 20 + m)
            for sg_ in range(CFG['nsub'] - 1, -1, -1):
                state_pass(1, (3 - m) * TOK + sg_ * SGT * 128, SGT, xr, 0, H, A)
        reset_S(1, 20 + 3)
        H = new_phase()
        A = sp_scratch(H, NT)
        kbS = [k for k in kSh[1]]
        for g in range(NG - 1, -1, -1):
            p.dma(ssave[g].rearrange("p (h v) -> p h v", h=8), S_all[:, 8:16, :], reads=kbS, writes=['ssave%d' % g], key='ssv')
            if g > 0 and g < CFG['ng']:
                state_pass(1, g * G, NT, xr, 0, H, A)

        for g in range(CFG['ng']):
            H = new_phase()
            A = hg_scratch(H)
            catT, kcat = H.bf16([128, KC, G])
            p.dma(S_all[:, 8:16, :], ssave[g].rearrange("p (h v) -> p h v", h=8), reads=['ssave%d' % g], writes=kbS, key='ssl')
            for h in range(8):
                cp(Sb_all[:, h, :], S_all[:, h, :], eng='act', r=[kSh[0][h]], w=[kSb[0][h]])
                cp(Sb_all[:, 8 + h, :], S_all[:, 8 + h, :], eng='act', r=[kSh[1][h]], w=[kSb[1][h]])
            for t in range(EXT):
                r0 = (g * G - 128 + t * 128) % SEQ
                norm_T(xr[r0:r0 + 128, :], 0, hT, khT, t * 128)
            NX = EXT * 128
            cosT, kcos = H.f32([128, NX])
            sinT, ksin = H.f32([128, NX])
            rope_mark = H.off
            pos, kpos = H.f32([128, NX])
            nrow = NX // 64
            p.op('pool', lambda e, pos=pos, g=g: e.iota(pos[0:64, :].rearrange("p (a b) -> p a b", b=64), pattern=[[1, nrow], [0, 64]],
                                                    base=8 * g - 2, channel_multiplier=0, allow_small_or_imprecise_dtypes=True),
                 writes=[kpos])
            p.op('pool', lambda e, pos=pos: e.iota(pos[64:128, :].rearrange("p (a b) -> p a b", b=64), pattern=[[0, nrow], [1, 64]],
                                               base=0, channel_multiplier=0, allow_small_or_imprecise_dtypes=True),
                 reads=[kpos], writes=[kpos])
            ts(pos[0:64, :], pos[0:64, :], fl[0:64, 10:11], None, ALU.add, r=[kpos, kfl], w=[kpos])
            ts(pos, pos, invf, None, ALU.mult, r=[kpos, ksm], w=[kpos])
            tA, ktA = H.f32([128, NX])
            tB, ktB = H.f32([128, NX])
            tK, ktK = H.f32([128, NX])
            tKi = tK.bitcast(mybir.dt.int32)

            def sin_table(dst, kdst, shift):
                ts(tB, pos, shift, None, ALU.add, r=[kpos], w=[ktB])
                ts(tA, tB, 1.0 / (2 * PI), None, ALU.mult, r=[ktB], w=[ktA])
                cp(tKi, tA, r=[ktA], w=[ktK])
                cp(tA, tKi, r=[ktK], w=[ktA])
                stt(tB, tA, -2 * PI, tB, ALU.mult, ALU.add, r=[ktA, ktB], w=[ktB])
                ts(tA, tB, PI, None, ALU.is_gt, r=[ktB], w=[ktA])
                stt(tB, tA, -2 * PI, tB, ALU.mult, ALU.add, r=[ktA, ktB], w=[ktB])
                ts(tA, tB, -PI, None, ALU.is_lt, r=[ktB], w=[ktA])
                stt(tB, tA, 2 * PI, tB, ALU.mult, ALU.add, r=[ktA, ktB], w=[ktB])
                act(dst, tB, AF.Sin, r=[ktB], w=[kdst])
            sin_table(sinT, ksin, 0.0)
            ts(sinT, sinT, sgn, None, ALU.mult, r=[ksin, ksm], w=[ksin])
            sin_table(cosT, kcos, 0.5 * PI)
            ktab = kcos
            H.off = rope_mark
            p.barrier()
            kTn, kkTn = H.bf16([128, NX])
            Vt, kVt = H.bf16([128, EXT, 128])
            qTn, kqTn = H.bf16([128, G])
            PTr = [H.bf16([128, 5, 128]) for _ in range(2)]
            rdr = [H.f32([128, 128]) for _ in range(2)]
            for kvh in range(2):
                load_w(wbf[0], kwbf[0], w_in, C_K + kvh * 128, 128, 0)
                load_w(wbf[0], kwbf[0], w_in, C_V + kvh * 128, 128, 128)
                for t0 in range(0, NX, 512):
                    n = min(512, NX - t0)
                    pb, kpb = bank()
                    proj_fm(pb[:, 0:n], kpb, wbf[0], kwbf[0], 0, hT, khT, t0, n)
                    qk_norm_rope(pb[:, 0:n], kpb, n, kg, kTn[:, t0:t0 + n], kkTn, A, cosT[:, t0:t0 + n], sinT[:, t0:t0 + n], ktab)
                for t in range(EXT):
                    pb, kpb = bank()
                    proj_tm(pb[:, 0:128], kpb, wbf[0], kwbf[0], 128, 128, hT, khT, t * 128)
                    cp(Vt[:, t, :], pb[:, 0:128], eng='act', r=[kpb], w=[kVt])
                for hq in range(4):
                    hh = kvh * 4 + hq
                    load_w(wbf[1], kwbf[1], w_in, C_QA + hh * 128, 128, 0)
                    pb, kpb = bank()
                    proj_fm(pb[:, 0:G], kpb, wbf[1], kwbf[1], 0, hT, khT, 128, G)
                    qk_norm_rope(pb[:, 0:G], kpb, G, qg, qTn, kqTn, A, cosT[:, 128:128 + G], sinT[:, 128:128 + G], ktab)
                    def att_front(qt, kvh=kvh):
                        PT_, kPT_ = PTr[qt % 2]
                        ps0, kps0 = bank()
                        ps1, kps1 = bank()
                        qs = qTn[:, qt * 128:(qt + 1) * 128]
                        for kb in range(3):
                            mm(ps0[:, kb * 128:(kb + 1) * 128], kTn[:, (qt + kb) * 128:(qt + kb + 1) * 128], qs,
                               r=[kkTn, kqTn], w=[kps0])
                        for cb in range(2):
                            mm(ps1[:, cb * 128:(cb + 1) * 128], kcT_c[:, kvh, cb * 128:(cb + 1) * 128], qs,
                               r=[kkc, kqTn], w=[kps1])
                        sc = float(128 ** -0.5)
                        act(PT_[:, 0:3, :], ps0[:, 0:384].rearrange("p (a b) -> p a b", a=3), AF.Exp, scale=sc, r=[kps0, kPT_], w=[kPT_])
                        act(PT_[:, 3:5, :], ps1[:, 0:256].rearrange("p (a b) -> p a b", a=2), AF.Exp, scale=sc, r=[kps1, kPT_], w=[kPT_])
                        mP = maskP0 if (g == 0 and qt == 0) else maskP
                        mN = maskN0 if (g == NG - 1 and qt == NT - 1) else maskN
                        tt(PT_[:, 0, :], PT_[:, 0, :], mP, ALU.mult, r=[kPT_, kmP, kmP0], w=[kPT_])
                        tt(PT_[:, 2, :], PT_[:, 2, :], mN, ALU.mult, eng='pool', r=[kPT_, kmN, kmN0], w=[kPT_])

                    def att_back(qt, kvh=kvh, hh=hh):
                        PT_, kPT_ = PTr[qt % 2]
                        rden_, krd_ = rdr[qt % 2]
                        po, kpo = bank()
                        for bi in range(5):
                            vv = Vt[:, qt + bi, :] if bi < 3 else v_c[:, kvh, bi - 3, :]
                            mm(po[:, 0:128], vv, PT_[:, bi, :], start=(bi == 0), stop=(bi == 4), r=[kVt, kvc, kPT_], w=[kpo])
                        for bi in range(5):
                            mm(po[:, 128:256], ones_b, PT_[:, bi, :], start=(bi == 0), stop=(bi == 4), r=[kob, kPT_], w=[kpo])
                        act(rden_, po[:, 128:256], AF.Ln, bias=esink[:, hh:hh + 1], scale=1.0, r=[kpo, ksm], w=[krd_])
                        act(rden_, rden_, AF.Exp, scale=-1.0, r=[krd_], w=[krd_])
                        tt(catT[:, hh, qt * 128:(qt + 1) * 128], po[:, 0:128], rden_, ALU.mult, r=[kpo, krd_], w=[kcat])
                    att_front(0)
                    for qt in range(NT):
                        if qt + 1 < NT:
                            att_front(qt + 1)
                        att_back(qt)
            lba, klba = load_lba(H)
            SA = [A, step_scratch(H)]
            lfb, klfb = H.f32([128, 2, NT, 128])
            kkb, kkkb = H.bf16([128, 2, NT, 128])
            sgw, ksgw = H.f32([128, 2, NT, 128])
            ivb, kivb = H.bf16([128, NT, 128])
            sgb, ksgb = H.f32([128, NT, 128])
            qTh, kqTh = H.bf16([128, G])
            ob, kob_ = H.f32([128, NT, 128])
            ycat, kyc = H.bf16([128, 128])
            osb, kosb = H.f32([128, 128])
            for h in range(8):
                w_ = wbf[h % 2]
                kw_ = kwbf[h % 2]
                for bi, c0 in enumerate([C_FF, C_FB, C_I, C_GG, C_QH]):
                    load_w(w_, kw_, w_in, c0 + h * 128, 128, bi * 128)
                pbs = [bank() for _ in range(4)]
                for t in range(NT):
                    for kc in range(KC):
                        for bi in range(4):
                            mm(pbs[bi][0][:, t * 128:(t + 1) * 128], hT[:, kc, (t + 1) * 128:(t + 2) * 128], w_[:, kc, bi * 128:(bi + 1) * 128],
                               start=(kc == 0), stop=(kc == KC - 1), r=[kw_, khT], w=[pbs[bi][1]])
                for d_ in range(2):
                    act(sgw[:, d_], pbs[d_][0][:, 0:G].rearrange("p (a b) -> p a b", a=NT), AF.Exp, scale=-1.0, r=[pbs[d_][1], ksgw], w=[ksgw])
                hs = slice(h * 128, (h + 1) * 128)
                gate_math(sgw, ksgw, lba[:, 0, :, hs].unsqueeze(2).to_broadcast([128, 2, NT, 128]),
                          lba[:, 1, :, hs].unsqueeze(2).to_broadcast([128, 2, NT, 128]), klba)
                act(lfb, sgw, AF.Ln, r=[ksgw], w=[klfb])
                ts(kkb, sgw, -1.0, 1.0, ALU.mult, ALU.add, r=[ksgw], w=[kkkb])
                cp(ivb, pbs[2][0][:, 0:G].rearrange("p (a b) -> p a b", a=NT), r=[pbs[2][1]], w=[kivb])
                act(sgb, pbs[3][0][:, 0:G].rearrange("p (a b) -> p a b", a=NT), AF.Exp, scale=-1.0, r=[pbs[3][1]], w=[ksgb])
                act(sgb, sgb, AF.Ln, bias=small[:, 22:23], scale=1.0, r=[ksgb, ksm], w=[ksgb])
                act(sgb, sgb, AF.Exp, scale=-1.0, r=[ksgb], w=[ksgb])
                tt(sgb, sgb, pbs[3][0][:, 0:G].rearrange("p (a b) -> p a b", a=NT), ALU.mult, r=[ksgb, pbs[3][1]], w=[ksgb])
                pq, kpq = bank()
                proj_fm(pq[:, 0:G], kpq, w_, kw_, 512, hT, khT, 128, G)
                cp(qTh, pq[:, 0:G], eng='act', r=[kpq], w=[kqTh])
                hsteps = [(1, t) for t in range(NT - 1, -1, -1)] + [(0, t) for t in range(NT)]
                kin_h = [klfb, kkkb, kivb, kqTh]

                def h_front(i, h=h):
                    d_, t = hsteps[i]
                    step_front(d_, h, lfb[:, d_, t, :], kkb[:, d_, t, :], ivb[:, t, :], qTh[:, t * 128:(t + 1) * 128], kin_h, SA[i % 2])

                def h_back(i, h=h):
                    d_, t = hsteps[i]
                    po, kpo = bank()
                    step_back(d_, h, ivb[:, t, :], kin_h, po[:, 0:128], kpo, SA[i % 2])
                    if d_ == 1:
                        cp(ob[:, t, :], po[:, 0:128], r=[kpo], w=[kob_ + str(t)])
                        return
                    tt(osb, po[:, 0:128], ob[:, t, :], ALU.add, r=[kpo, kob_ + str(t)], w=[kosb])
                    p.op('dve', lambda e: e.memset(sstat[:, 4:5], 0.0), reads=[kss], writes=[kss])
                    sq, ksq = A['sq']
                    act(sq[:, 0:128], osb, AF.Square, accum=sstat[:, 4:5], r=[kosb, kss], w=[ksq, kss])
                    act(sstat[:, 5:6], sstat[:, 4:5], AF.Ln, bias=small[:, 20:21], scale=1.0 / 128, r=[kss, ksm], w=[kss])
                    act(sstat[:, 6:7], sstat[:, 5:6], AF.Exp, scale=-0.5, r=[kss], w=[kss])
                    stt(osb, osb, sstat[:, 6:7], ogain, ALU.mult, ALU.mult, r=[kosb, kss, kog], w=[kosb])
                    tt(ycat, osb, sgb[:, t, :], ALU.mult, r=[kosb, ksgb], w=[kyc])
                    pt, kpt = bank_bf()
                    tr(pt[:, 0:128], ycat, identb, r=[kyc, kidb], w=[kpt])
                    cp(catT[:, 8 + h, t * 128:(t + 1) * 128], pt[:, 0:128], eng='act', r=[kpt], w=[kcat])
                h_front(0)
                for i in range(len(hsteps)):
                    if i + 1 < len(hsteps):
                        h_front(i + 1)
                    h_back(i)
            gbc, kgbc = XS, kXS
            p.dma(gbc, modd[2].partition_broadcast(128), reads=['modd'], writes=[kgbc])
            for cg in range(4):
                w_ = wbf[cg % 2]
                kw_ = kwbf[cg % 2]
                load_w(w_, kw_, w_out, cg * 512, 512, 0)
                for t in range(NT):
                    if cg == 0:
                        pass
                    pb, kpb = bank()
                    for kc in range(KC):
                        mm(pb[:], catT[:, kc, t * 128:(t + 1) * 128], w_[:, kc, 0:512], start=(kc == 0), stop=(kc == KC - 1),
                           r=[kcat, kw_], w=[kpb])
                    r0 = g * G + t * 128
                    xa, kxa = A['sq']
                    p.dma(xa, xr[r0:r0 + 128, cg * 512:(cg + 1) * 512], writes=[kxa])
                    tt(A['kn'][0], pb[:], gbc[:, cg * 512:(cg + 1) * 512], ALU.mult, r=[kpb, kgbc], w=[A['kn'][1]])
                    tt(A['kn'][0], A['kn'][0], xa, ALU.add, r=[A['kn'][1], kxa], w=[A['kn'][1]])
                    p.dma(x1s[r0:r0 + 128, cg * 512:(cg + 1) * 512], A['kn'][0], reads=[A['kn'][1]], writes=['x1s'], key=A['kn'][1])

            H = new_phase()
            for t in range(NT):
                r0 = g * G + t * 128
                norm_T(x1s[r0:r0 + 128, :], 2, hT, khT, t * 128)
            s1, ks1 = H.f32([128, NT, 8, 128])
            s2, ks2 = H.f32([128, NT, 8, 128])
            cdiag, kcd = H.bf16([128, NT, 8, 128])
            off_mark = H.off
            qTb, kqTb = H.f32([128, G])
            skT, kskT = H.f32([128, 128])
            t16a, kt16a = H.f32([128, 16])
            t16b, kt16b = H.f32([128, 16])
            scr, kscr = H.f32([128, 256])
            cand, kcand = H.f32([128, 256])
            best, kbest = H.f32([128, 16])
            pst, kpst = H.f32([128, 8])
            for hp in range(16):
                hd, half = hp // 2, hp % 2
                load_w(wbf[hp % 2], kwbf[hp % 2], w_q, hp * 128, 128, 0)
                pb, kpb = bank()
                proj_fm(pb[:, 0:G], kpb, wbf[hp % 2], kwbf[hp % 2], 0, hT, khT, 0, G)
                cp(qTb, pb[:, 0:G], eng='act', r=[kpb], w=[kqTb])
                p.dma(XS[:, 0:128], skd[hp], writes=[kXS])
                pt, kpt = bank()
                tr(pt[:, 0:128], XS[:, 0:128], ident, r=[kXS, kid], w=[kpt])
                cp(skT, pt[:, 0:128], r=[kpt], w=[kskT])
                dst = s1 if half == 0 else s2
                kd = ks1 if half == 0 else ks2
                for t in range(NT):
                    pb2, kpb2 = bank()
                    mm(pb2[:, 0:128], qTb[:, t * 128:(t + 1) * 128], skT, r=[kqTb, kskT], w=[kpb2])
                    cp(dst[:, t, hd, :], pb2[:, 0:128], eng='act', r=[kpb2], w=[kd])
            TKB = []
            for i in range(2):
                TKB.append(dict(t16a=H.f32([128, 16]), t16b=H.f32([128, 16]), scr=H.f32([128, 256]), cand=H.f32([128, 256]),
                                best=H.f32([128, 16]), pst=H.f32([128, 8])))

            def tk_chain(t, hd, B):
                t16a_, kta = B['t16a']
                t16b_, ktb = B['t16b']
                scr_, kscr_ = B['scr']
                cand_, kcand_ = B['cand']
                best_, kbest_ = B['best']
                pst_, kpst_ = B['pst']
                kf = 'tk_%d_%d' % (t, hd)
                ops_ = []
                for (src, ksrc, t16, kt16) in [(s1, ks1, t16a_, kta), (s2, ks2, t16b_, ktb)]:
                    ops_.append(lambda src=src, ksrc=ksrc, t16=t16, kt16=kt16: p.op(
                        'dve', lambda e: e.max(out=t16[:, 0:8], in_=src[:, t, hd, :]), reads=[ksrc, kf], writes=[kt16]))
                    ops_.append(lambda src=src, ksrc=ksrc, t16=t16, kt16=kt16: p.op(
                        'dve', lambda e: e.match_replace(out=scr_[:, 0:128], in_to_replace=t16[:, 0:8], in_values=src[:, t, hd, :], imm_value=-1e30),
                        reads=[ksrc, kf, kt16], writes=[kscr_]))
                    ops_.append(lambda t16=t16, kt16=kt16: p.op(
                        'dve', lambda e: e.max(out=t16[:, 8:16], in_=scr_[:, 0:128]), reads=[kscr_, kt16], writes=[kt16]))
                ops_.append(lambda: tt(cand_.rearrange("p (a b) -> p a b", a=16), t16a_.unsqueeze(2).to_broadcast([128, 16, 16]),
                                       t16b_.unsqueeze(1).to_broadcast([128, 16, 16]), ALU.add, r=[kta, ktb], w=[kcand_]))
                ops_.append(lambda: p.op('dve', lambda e: e.max(out=best_[:, 0:8], in_=cand_), reads=[kcand_], writes=[kbest_]))
                ops_.append(lambda: p.op('dve', lambda e: e.match_replace(out=scr_, in_to_replace=best_[:, 0:8], in_values=cand_, imm_value=-1e30),
                                         reads=[kcand_, kbest_], writes=[kscr_]))
                ops_.append(lambda: p.op('dve', lambda e: e.max(out=best_[:, 8:16], in_=scr_), reads=[kscr_, kbest_], writes=[kbest_]))
                ops_.append(lambda: p.op('dve', lambda e: e.tensor_reduce(out=pst_[:, 0:1], in_=best_, axis=AX.X, op=ALU.max), reads=[kbest_, kpst_], writes=[kpst_]))
                ops_.append(lambda: p.op('dve', lambda e: e.tensor_reduce(out=pst_[:, 1:2], in_=best_, axis=AX.X, op=ALU.min), reads=[kbest_, kpst_], writes=[kpst_]))
                ops_.append(lambda: ts(pst_[:, 2:3], pst_[:, 0:1], -1.0, None, ALU.mult, r=[kpst_], w=[kpst_]))
                ops_.append(lambda: p.op('dve', lambda e: e.memset(pst_[:, 3:4], 0.0), reads=[kpst_], writes=[kpst_]))
                ops_.append(lambda: act(scr_[:, 0:16], best_, AF.Exp, bias=pst_[:, 2:3], scale=1.0, accum=pst_[:, 3:4], r=[kbest_, kpst_], w=[kscr_, kpst_]))
                ops_.append(lambda: tt(pst_[:, 4:5], pst_[:, 1:2], pst_[:, 0:1], ALU.subtract, r=[kpst_], w=[kpst_]))
                ops_.append(lambda: act(pst_[:, 4:5], pst_[:, 4:5], AF.Exp, r=[kpst_], w=[kpst_]))
                ops_.append(lambda: p.op('dve', lambda e: e.reciprocal(out=pst_[:, 5:6], in_=pst_[:, 3:4]), reads=[kpst_], writes=[kpst_]))
                ops_.append(lambda: tt(pst_[:, 4:5], pst_[:, 4:5], pst_[:, 5:6], ALU.mult, r=[kpst_], w=[kpst_]))
                ops_.append(lambda: ts(cdiag[:, t, hd, :], ident, pst_[:, 4:5], None, ALU.mult, r=[kid, kpst_], w=[kcd + kf]))
                ops_.append(lambda: ts(s1[:, t, hd, :], s1[:, t, hd, :], pst_[:, 1:2], None, ALU.subtract, r=[ks1, kf, kpst_], w=[kf]))
                return ops_

            for t in range(NT):
                for hd in range(0, 8, 2):
                    ca = tk_chain(t, hd, TKB[0])
                    cb = tk_chain(t, hd + 1, TKB[1])
                    for fa, fb in zip(ca, cb):
                        fa()
                        fb()
            H.off = off_mark
            p.barrier()
            acc, kacc = H.f32([128, NT, D])
            kaccs = [kacc + str(t) for t in range(NT)]
            p.op('pool', lambda e: e.memset(acc, 0.0), writes=kaccs)
            NB = 4
            uTv = [wbfF[0][:, i * 2048:(i + 1) * 2048].rearrange("p (a b) -> p a b", a=KC) for i in range(2)]
            kuTv = ['uTv0', 'uTv1']
            vbv = [wbfF[0][:, 4096 + i * 2048: 4096 + (i + 1) * 2048] for i in range(3)] + \
                  [wbfF[1][:, i * 2048:(i + 1) * 2048] for i in range(5)]
            kvbv = ['vbv%d' % i for i in range(8)]
            Lbr = [(XS[:, i * 1024:(i + 1) * 1024].rearrange("p (a b) -> p a b", a=8), 'Lbr%d' % i) for i in range(2)] + \
                  [(wstF[:, i * 1024:(i + 1) * 1024].rearrange("p (a b) -> p a b", a=8), 'Lbr%d' % (2 + i)) for i in range(2)]
            Xbr = [H.bf16([128, 8, 128]) for _ in range(4)]
            Xmr = [H.bf16([128, 8, 128]) for _ in range(4)]
            gelr = [(xn[:, i * 512:(i + 1) * 512], 'gelr%d' % i) for i in range(4)]
            kGTc = ['gtc%d' % i for i in range(8)]
            p.barrier()
            nblk = CFG['nec'] // NB
            cnt2 = [0]

            def s1_block(blk):
                for c in range(NB):
                    ec = blk * NB + c
                    uT_, kuT_ = uTv[ec % 2], kuTv[ec % 2]
                    vb_, kvb_ = vbv[ec % 8], kvbv[ec % 8]
                    p.dma(uT_, uTd[ec].rearrange("p (a b) -> p a b", a=KC), writes=[kuT_])
                    p.dma(vb_, vd[ec], writes=[kvb_])
                    pa, kpa = bank()
                    for kc in range(KC):
                        mm(pa[:, 0:G], uT_[:, kc, :], hT[:, kc, 0:G], start=(kc == 0), stop=(kc == KC - 1), r=[kuT_, khT], w=[kpa])
                    gel, kgel = gelr[c]
                    act(gel, pa[:, 0:G], AF.Gelu, r=[kpa], w=[kgel])
                for c in range(NB):
                    ec = blk * NB + c
                    gel, kgel = gelr[c]
                    pw, kpw = bank()
                    for t in range(NT):
                        j = cnt2[0] % 4
                        cnt2[0] += 1
                        Lb, kLb = Lbr[j]
                        Xb, kXb = Xbr[j]
                        Xm, kXm = Xmr[j]
                        tt(Lb, s2[:, t], s1[:, t, :, ec:ec + 1].to_broadcast([128, 8, 128]), ALU.add, eng=('pool' if t % 2 == 0 else 'dve'),
                           r=[ks1, ks2], w=[kLb])
                        act(Xb, Lb, AF.Exp, r=[kLb], w=[kXb])
                        stt(Xm, Lb, 0.0, Xb, ALU.is_ge, ALU.mult, r=[kLb, kXb], w=[kXm])
                        for hd in range(8):
                            mm(pw[:, t * 128:(t + 1) * 128], Xm[:, hd, :], cdiag[:, t, hd, :], start=(hd == 0), stop=(hd == 7),
                               r=[kXm, kcd], w=[kpw])
                    gi = (blk % 2) * NB + c
                    GTc = hT[:, 2 * gi:2 * gi + 2, 512:768]
                    tt(GTc, gel.rearrange("p (a b) -> p a b", a=2), pw[:, 0:G].rearrange("p (a b) -> p a b", a=2), ALU.mult,
                       r=[kgel, kpw], w=[kGTc[gi]])

            def s2_block(blk):
                for t in range(NT):
                    for dh in range(2):
                        pvb = []
                        for q in range(2):
                            pvv, kpvv = bank()
                            pvb.append((pvv, kpvv))
                            dg = dh * 2 + q
                            for c in range(NB):
                                ec = blk * NB + c
                                gi = (blk % 2) * NB + c
                                mm(pvv[:], hT[:, 2 * gi + t // 2, 512 + (t % 2) * 128: 512 + (t % 2) * 128 + 128],
                                   vbv[ec % 8][:, dg * 512:(dg + 1) * 512], start=(c == 0), stop=(c == NB - 1),
                                   r=[kGTc[gi], kvbv[ec % 8]], w=[kpvv])
                        for q in range(2):
                            dg = dh * 2 + q
                            pvv, kpvv = pvb[q]
                            tt(acc[:, t, dg * 512:(dg + 1) * 512], acc[:, t, dg * 512:(dg + 1) * 512], pvv[:], ALU.add,
                               r=[kacc + str(t), kpvv], w=[kacc + str(t)])
            for blk in range(nblk):
                s1_block(blk)
                if blk > 0:
                    s2_block(blk - 1)
            s2_block(nblk - 1)
            p.barrier()
            un = [XS, wstF]
            kun = [[kXS], list(kwst)]
            p.dma(un[1], modd[5].partition_broadcast(128), reads=['modd'], writes=kun[1])
            for t in range(NT):
                r0 = g * G + t * 128
                p.dma(XS, x1s[r0:r0 + 128, :], reads=['x1s'], writes=[kXS])
                tt(acc[:, t, :], acc[:, t, :], un[1], ALU.mult, r=[kacc + str(t)] + kun[1], w=[kacc + str(t)])
                tt(acc[:, t, :], acc[:, t, :], XS, ALU.add, r=[kacc + str(t), kXS], w=[kacc + str(t)])
                p.dma(outd[r0:r0 + 128, :], acc[:, t, :], reads=[kacc + str(t)], writes=['outd'], key='outk', final=True)
        print("ops recorded", p.nops, {e: len(v) for e, v in p.ops.items()}, "dsems", len(p.dsem))
        p.emit()
    return nc


def make_in_maps(inputs):
    x = np.asarray(inputs["x"], np.float32)
    f32 = lambda k: np.ascontiguousarray(np.asarray(inputs[k], np.float32))
    in_maps = []
    for core in range(8):
        b, j = core // 4, core % 4
        fl = np.zeros((128, 32), np.float32)
        for m in range(4):
            rf = 1.0 if (j - 3 + m) <= 0 else 0.0
            rb = 1.0 if (j + 3 - m) >= 3 else 0.0
            fl[:, 16 + m] = 1.0 - rf
            fl[:, 20 + m] = 1.0 - rb
        fl[:, 8] = 1.0 if j > 0 else 0.0
        fl[:, 9] = 1.0 if j < 3 else 0.0
        fl[:, 10] = float(j * 64)
        in_maps.append({
            "xr": np.ascontiguousarray(np.roll(x[b], -j * TOK, axis=0)),
            "ctxb": f32("ctx")[b],
            "cvec": np.ascontiguousarray(np.stack([f32("c")[b], f32("c_ctx")])),
            "w_ada": f32("w_ada")[0], "b_ada": f32("b_ada")[0],
            "norm_mix": f32("norm_mix")[0], "norm_ffn": f32("norm_ffn")[0],
            "w_in": f32("w_in")[0], "q_norm": f32("q_norm")[0], "k_norm": f32("k_norm")[0],
            "attn_sink": f32("attn_sink")[0], "lbl": f32("hgrn_lb_logits"),
            "hgrn_norm": f32("hgrn_norm")[0], "w_out": f32("w_out")[0], "w_q": f32("peer_w_q")[0],
            "sk": f32("peer_sub_keys")[0].reshape(16, 128, 128),
            "pu": f32("peer_u")[0], "pv": f32("peer_v")[0],
            "flags": fl,
        })
    return in_maps


def kernel(**inputs):
    nc = build_nc()
    in_maps = make_in_maps(inputs)
    res = run_bass_kernel_spmd(nc, in_maps, core_ids=list(range(8)))
    out = np.zeros((2, SEQ, D), np.float32)
    for core in range(8):
        b, j = core // 4, core % 4
        out[b, j * TOK:(j + 1) * TOK] = res.results[core]["out"]
    return out
```
